# Optimizing a Trainium2 kernel written in Bass

```python
import math
import jax, jax.numpy as jnp
from jax import lax
import numpy as np

D_MODEL = 1024
BATCH = 8
SEQ = 2048
DEPTH = 1

HEAD_DIM = 64
MIX_WIDTH = D_MODEL
ATTN_WIDTH = MIX_WIDTH // 2
RET_WIDTH = MIX_WIDTH - ATTN_WIDTH
N_ATTN_HEADS = ATTN_WIDTH // HEAD_DIM
N_RET_HEADS = RET_WIDTH // HEAD_DIM
PROJ_WIDTH = 3 * ATTN_WIDTH + 4 * RET_WIDTH
ATTN_BRANCHES = ((128, 1), (512, 4), (2048, 16))
ATTN_BLOCK = 64
N_BUCKETS = 32
REL_MAX_DIST = 1024
RET_CHUNK = 128
ROPE_BASE = 10000.0
N_GROUPS = 4
EXPERTS_PER_GROUP = 4
N_EXPERTS = N_GROUPS * EXPERTS_PER_GROUP
EXPERT_FF = D_MODEL // 2
TOP_K_INNER = 2
EPS = 1e-6
NEG_INF = -1e30

kernel_name = 'hybrid_dilated_attn_retention_hier_moe'


def _rms_norm(x, gain):
    xf = x.astype(jnp.float32)
    y = xf * lax.rsqrt(jnp.mean(xf * xf, axis=-1, keepdims=True) + EPS)
    return (y * gain.astype(jnp.float32)).astype(x.dtype)


def _to_heads(t, n_heads):
    b, s, _ = t.shape
    return t.reshape(b, s, n_heads, -1).transpose(0, 2, 1, 3)


def _from_heads(t):
    b, h, s, hd = t.shape
    return t.transpose(0, 2, 1, 3).reshape(b, s, h * hd)


def _t5_bucket(rel):
    half = N_BUCKETS // 2
    max_exact = half // 2
    offset = jnp.where(rel > 0, half, 0)
    n = jnp.abs(rel)
    nf = jnp.maximum(n, 1).astype(jnp.float32)
    large = max_exact + (jnp.log(nf / max_exact) / math.log(REL_MAX_DIST / max_exact)
                         * (half - max_exact)).astype(jnp.int32)
    large = jnp.minimum(large, half - 1)
    return offset + jnp.where(n < max_exact, n, large)


def _dilated_window_branch(q, k, v, rel_bias, window, dilation):
    b, h, s, hd = q.shape
    radius = (window // 2) // dilation
    sub_len = s // dilation
    n_blk = -(-sub_len // ATTN_BLOCK)
    pad_len = n_blk * ATTN_BLOCK - sub_len

    def to_sub(t):
        return t.reshape(b, h, sub_len, dilation, hd).transpose(0, 1, 3, 2, 4)

    qs = jnp.pad(to_sub(q), ((0, 0), (0, 0), (0, 0), (0, pad_len), (0, 0)))
    qs = qs.reshape(b, h, dilation, n_blk, ATTN_BLOCK, hd)

    def neighbour_blocks(t):
        tp = jnp.pad(to_sub(t), ((0, 0), (0, 0), (0, 0), (ATTN_BLOCK, pad_len + ATTN_BLOCK), (0, 0)))
        tp = tp.reshape(b, h, dilation, n_blk + 2, ATTN_BLOCK, hd)
        return jnp.concatenate([tp[:, :, :, :-2], tp[:, :, :, 1:-1], tp[:, :, :, 2:]], axis=4)

    kb = neighbour_blocks(k)
    vb = neighbour_blocks(v)
    logits = jnp.einsum('bhrnqd,bhrnkd->bhrnqk', qs, kb).astype(jnp.float32)

    rel_sub = jnp.arange(3 * ATTN_BLOCK)[None, :] - ATTN_BLOCK - jnp.arange(ATTN_BLOCK)[:, None]
    bias = rel_bias[_t5_bucket(rel_sub * dilation)].astype(jnp.float32)
    bias = jnp.transpose(bias, (2, 0, 1))
    key_pos = (jnp.arange(n_blk)[:, None] - 1) * ATTN_BLOCK + jnp.arange(3 * ATTN_BLOCK)[None, :]
    valid = (jnp.abs(rel_sub) <= radius)[None] & ((key_pos >= 0) & (key_pos < sub_len))[:, None, :]
    logits = jnp.where(valid, logits + bias[None, :, None, None], NEG_INF)

    m = jnp.max(logits, axis=-1, keepdims=True)
    p = jnp.exp(logits - m)
    denom = jnp.sum(p, axis=-1, keepdims=True)
    out = jnp.einsum('bhrnqk,bhrnkd->bhrnqd', p, vb.astype(jnp.float32)) / denom
    lse = (m + jnp.log(denom))[..., 0]

    def from_sub(t):
        tail = t.shape[5:]
        t = t.reshape((b, h, dilation, n_blk * ATTN_BLOCK) + tail)[:, :, :, :sub_len]
        t = jnp.moveaxis(t, 2, 3)
        return t.reshape((b, h, s) + tail)

    return from_sub(out), from_sub(lse)


def _dilated_attention(q, k, v, rel_bias):
    outs, lses = [], []
    for window, dilation in ATTN_BRANCHES:
        o, l = _dilated_window_branch(q, k, v, rel_bias, window, dilation)
        outs.append(o)
        lses.append(l)
    w = jax.nn.softmax(jnp.stack(lses, axis=0), axis=0)
    return jnp.sum(w[..., None] * jnp.stack(outs, axis=0), axis=0)


def _rotary(t, pos):
    half = t.shape[-1] // 2
    inv = ROPE_BASE ** (-jnp.arange(half, dtype=jnp.float32) / half)
    ang = pos[:, None] * inv[None, :]
    cos, sin = jnp.cos(ang), jnp.sin(ang)
    t1, t2 = t[..., :half], t[..., half:]
    return jnp.concatenate([t1 * cos - t2 * sin, t1 * sin + t2 * cos], axis=-1)


def _chunk_retention(q, k, v, log_g, include_diag):
    b, h, s, hd = q.shape
    nc = s // RET_CHUNK
    qc = q.reshape(b, h, nc, RET_CHUNK, hd)
    kc = k.reshape(b, h, nc, RET_CHUNK, hd)
    vc = v.reshape(b, h, nc, RET_CHUNK, v.shape[-1])
    idx = jnp.arange(RET_CHUNK, dtype=jnp.float32)
    rel = idx[:, None] - idx[None, :]
    mask = (rel >= 0) if include_diag else (rel > 0)
    decay = jnp.where(mask[None], jnp.exp(log_g[:, None, None] * jnp.maximum(rel, 0.0)[None]), 0.0)
    scores = jnp.einsum('bhncd,bhnkd->bhnck', qc, kc) * decay[None, :, None]
    intra = jnp.einsum('bhnck,bhnke->bhnce', scores, vc)
    k_dec = jnp.exp(log_g[:, None] * (RET_CHUNK - 1 - idx)[None])
    kv = jnp.einsum('bhnkd,bhnke->bhnde', kc * k_dec[None, :, None, :, None], vc)
    chunk_decay = jnp.exp(log_g * RET_CHUNK)[None, :, None, None]

    def step(state, kv_n):
        return state * chunk_decay + kv_n, state

    init = jnp.zeros((b, h, hd, v.shape[-1]), jnp.float32)
    _, states = lax.scan(step, init, jnp.moveaxis(kv, 2, 0))
    states = jnp.moveaxis(states, 0, 2)
    q_dec = jnp.exp(log_g[:, None] * (idx + 1.0)[None])
    cross = jnp.einsum('bhncd,bhnde->bhnce', qc * q_dec[None, :, None, :, None], states)
    return (intra + cross).reshape(b, h, s, v.shape[-1])


def _retention(q, k, v, g, decay_fwd, decay_bwd):
    s, hd = q.shape[2], q.shape[3]
    pos = jnp.arange(s, dtype=jnp.float32)
    qf = _rotary(q.astype(jnp.float32), pos)
    kf = _rotary(k.astype(jnp.float32), pos) * (hd ** -0.5)
    vf = v.astype(jnp.float32)
    log_g_f = -jnp.exp(decay_fwd.astype(jnp.float32))
    log_g_b = -jnp.exp(decay_bwd.astype(jnp.float32))
    y_f = _chunk_retention(qf, kf, vf, log_g_f, True)
    flip = lambda t: jnp.flip(t, axis=2)
    y_b = flip(_chunk_retention(flip(qf), flip(kf), flip(vf), log_g_b, False))
    y = y_f + y_b
    y = y * lax.rsqrt(jnp.mean(y * y, axis=-1, keepdims=True) + EPS)
    y = _from_heads(y) * jax.nn.silu(g.astype(jnp.float32))
    return y.astype(g.dtype)


def _hier_moe(xn, w_g, b_g, w_e, b_e, w1, w3, w2):
    b, s, d = xn.shape
    t = xn.reshape(b * s, d)
    group_logits = (t @ w_g).astype(jnp.float32) + b_g.astype(jnp.float32)
    group_probs = jax.nn.softmax(group_logits, axis=-1)
    p_group, g_idx = lax.top_k(group_probs, 1)
    expert_logits = jnp.einsum('td,gde->tge', t, w_e).astype(jnp.float32) + b_e.astype(jnp.float32)
    chosen = jnp.take_along_axis(expert_logits, g_idx[:, :, None], axis=1)[:, 0]
    top_vals, top_idx = lax.top_k(chosen, TOP_K_INNER)
    top_p = jax.nn.softmax(top_vals, axis=-1)
    inner = jnp.sum(jax.nn.one_hot(top_idx, EXPERTS_PER_GROUP) * top_p[..., None], axis=1)
    gates = jax.nn.one_hot(g_idx[:, 0], N_GROUPS)[:, :, None] * (p_group[:, :, None] * inner[:, None, :])
    out = jnp.zeros((b * s, d), jnp.float32)
    for grp in range(N_GROUPS):
        sl = slice(grp * EXPERTS_PER_GROUP, (grp + 1) * EXPERTS_PER_GROUP)
        hidden = jax.nn.silu(jnp.einsum('td,edf->tef', t, w1[sl])) * jnp.einsum('td,edf->tef', t, w3[sl])
        hidden = hidden.astype(jnp.float32) * gates[:, grp, :, None]
        out = out + jnp.einsum('tef,efd->td', hidden, w2[sl].astype(jnp.float32))
    return out.reshape(b, s, d).astype(xn.dtype)


def setup_inputs(seed: int = 0) -> dict:
    key = jax.random.key(seed)
    ks = jax.random.split(key, 20)
    f32 = jnp.float32
    nrm = lambda k, shape, scale: jax.random.normal(k, shape, f32) * scale
    heads = jnp.arange(N_RET_HEADS, dtype=f32)
    base_decay = jnp.log(-jnp.log1p(-(2.0 ** (-5.0 - heads))))
    return {
        'x': nrm(ks[0], (BATCH, SEQ, D_MODEL), 1.0),
        'w_in': nrm(ks[1], (DEPTH, D_MODEL, PROJ_WIDTH), D_MODEL ** -0.5),
        'w_out': nrm(ks[2], (DEPTH, MIX_WIDTH, D_MODEL), MIX_WIDTH ** -0.5),
        'norm_mix': 1.0 + nrm(ks[3], (DEPTH, D_MODEL), 0.02),
        'norm_ffn': 1.0 + nrm(ks[4], (DEPTH, D_MODEL), 0.02),
        'norm_final': 1.0 + nrm(ks[5], (D_MODEL,), 0.02),
        'attn_out_gain': 1.0 + nrm(ks[6], (DEPTH, ATTN_WIDTH), 0.02),
        'rel_bias': nrm(ks[7], (N_BUCKETS, N_ATTN_HEADS), 0.2),
        'ret_decay_fwd': base_decay[None] + nrm(ks[8], (DEPTH, N_RET_HEADS), 0.05),
        'ret_decay_bwd': base_decay[None] + nrm(ks[9], (DEPTH, N_RET_HEADS), 0.05),
        'router_group_w': nrm(ks[10], (DEPTH, D_MODEL, N_GROUPS), D_MODEL ** -0.5),
        'router_group_b': nrm(ks[11], (DEPTH, N_GROUPS), 0.01),
        'router_expert_w': nrm(ks[12], (DEPTH, N_GROUPS, D_MODEL, EXPERTS_PER_GROUP), D_MODEL ** -0.5),
        'router_expert_b': nrm(ks[13], (DEPTH, N_GROUPS, EXPERTS_PER_GROUP), 0.01),
        'expert_w1': nrm(ks[14], (DEPTH, N_EXPERTS, D_MODEL, EXPERT_FF), D_MODEL ** -0.5),
        'expert_w3': nrm(ks[15], (DEPTH, N_EXPERTS, D_MODEL, EXPERT_FF), D_MODEL ** -0.5),
        'expert_w2': nrm(ks[16], (DEPTH, N_EXPERTS, EXPERT_FF, D_MODEL), EXPERT_FF ** -0.5),
    }


def reference(x, w_in, w_out, norm_mix, norm_ffn, norm_final, attn_out_gain, rel_bias,
              ret_decay_fwd, ret_decay_bwd, router_group_w, router_group_b,
              router_expert_w, router_expert_b, expert_w1, expert_w3, expert_w2):
    a = ATTN_WIDTH
    r = RET_WIDTH
    split_at = [a, 2 * a, 3 * a, 3 * a + r, 3 * a + 2 * r, 3 * a + 3 * r]
    h = x
    for layer in range(DEPTH):
        xn = _rms_norm(h, norm_mix[layer])
        proj = xn @ w_in[layer]
        aq, ak, av, rq, rk, rv, rg = jnp.split(proj, split_at, axis=-1)
        attn = _dilated_attention(_to_heads(aq, N_ATTN_HEADS) * (HEAD_DIM ** -0.5),
                                  _to_heads(ak, N_ATTN_HEADS), _to_heads(av, N_ATTN_HEADS), rel_bias)
        attn = _rms_norm(_from_heads(attn).astype(x.dtype), attn_out_gain[layer])
        ret = _retention(_to_heads(rq, N_RET_HEADS), _to_heads(rk, N_RET_HEADS),
                         _to_heads(rv, N_RET_HEADS), rg, ret_decay_fwd[layer], ret_decay_bwd[layer])
        h = h + jnp.concatenate([attn, ret], axis=-1) @ w_out[layer]
        hn = _rms_norm(h, norm_ffn[layer])
        h = h + _hier_moe(hn, router_group_w[layer], router_group_b[layer],
                          router_expert_w[layer], router_expert_b[layer],
                          expert_w1[layer], expert_w3[layer], expert_w2[layer])
    return _rms_norm(h, norm_final)
```

```python
import math
from contextlib import ExitStack

import numpy as np
import concourse.bass as bass
import concourse.mybir as mybir
from concourse.bass_utils import run_bass_kernel_spmd

F32, BF16 = mybir.dt.float32, mybir.dt.bfloat16
AF = mybir.ActivationFunctionType
ALU = mybir.AluOpType
AX = mybir.AxisListType

S = 2048
D = 1024
NT = S // 128
PAD = 256
SP_ = S + 2 * PAD
EPS = 1e-6
NEG = -30000.0
PROJ_W = 3584
N_BUCKETS = 32
REL_MAX_DIST = 1024
ROPE_BASE = 10000.0

SAME_SYNC = True
MOE_CAP = 384
FORCE_FALLBACK = False


class Buf:
    __slots__ = ("name", "w", "r")

    def __init__(self, name=""):
        self.name = name
        self.w = None
        self.r = {}


class Eng:
    def __init__(self, name, sem):
        self.name = name
        self.sem = sem
        self.count = 0
        self.waited = {}
        self.queue = []


class Tracker:
    def __init__(self, nc, es, prefix=""):
        self.nc = nc
        self.es = es
        self.prefix = prefix
        self.sems = {}
        self.engs = {}
        for name in ("pe", "act", "dve", "pool", "sp"):
            sem = es.enter_context(nc.semaphore("sem_" + prefix + name))
            self.sems[name] = sem
            self.engs[name] = Eng(name, sem)
        self.dma_counts = {}
        self.ndma = 0
        self.capture = None

    def dma_sem(self, name=None):
        name = name or ("dma%d" % self.ndma)
        self.ndma += 1
        sem = self.es.enter_context(self.nc.semaphore("sem_" + self.prefix + name))
        self.sems[name] = sem
        self.dma_counts[name] = 0
        return name

    def _deps(self, eng, reads, writes):
        deps = {}

        def add(k, v):
            if v > deps.get(k, 0):
                deps[k] = v
        for b in reads:
            if b.w is not None:
                add(*b.w)
        for b in writes:
            if b.w is not None:
                add(*b.w)
            for k, v in b.r.items():
                add(k, v)
        waits = []
        for k, v in deps.items():
            if k == eng.name and (eng.name in ("pe", "sp") or not SAME_SYNC):
                continue
            if eng.waited.get(k, 0) >= v:
                continue
            eng.waited[k] = v
            waits.append((k, v))
        return waits

    def op(self, engname, fn, reads=(), writes=()):
        if self.capture is not None:
            self.capture.append(("op", engname, fn, list(reads), list(writes)))
            return None
        eng = self.engs[engname]
        waits = self._deps(eng, reads, writes)
        eng.count += 1
        tok = (engname, eng.count)
        eng.queue.append((waits, fn, (engname, 1)))
        for b in reads:
            b.r[engname] = eng.count
        for b in writes:
            b.w = tok
            b.r = {}
        return tok

    def dma(self, qname, semname, fn, reads=(), writes=(), n=1):
        if self.capture is not None:
            self.capture.append(("dma", qname, semname, fn, list(reads), list(writes), n))
            return None
        eng = self.engs[qname]
        waits = self._deps(eng, reads, writes)
        self.dma_counts[semname] += 16 * n
        val = self.dma_counts[semname]
        eng.queue.append((waits, fn, (semname, 16)))
        for b in reads:
            b.r[semname] = val
        for b in writes:
            b.w = (semname, val)
            b.r = {}
        return (semname, val)

    def begin_capture(self):
        self.capture = []

    def end_capture(self):
        items, self.capture = self.capture, None
        return items

    def replay(self, *lists):
        merged = []
        for li, items in enumerate(lists):
            n = len(items)
            for k, it in enumerate(items):
                merged.append(((k + 0.5) / max(n, 1), li, k, it))
        merged.sort(key=lambda t: (t[0], t[1], t[2]))
        for _, _, _, it in merged:
            if it[0] == "op":
                self.op(it[1], it[2], it[3], it[4])
            else:
                self.dma(it[1], it[2], it[3], it[4], it[5], it[6])

    def barrier(self):
        for eng in self.engs.values():
            for k, other in self.engs.items():
                if k == eng.name:
                    continue
                if other.count > eng.waited.get(k, 0):
                    eng.waited[k] = other.count
                    eng.queue.append(([(k, other.count)], None, None))
            for k, v in self.dma_counts.items():
                if v > eng.waited.get(k, 0):
                    eng.waited[k] = v
                    eng.queue.append(([(k, v)], None, None))

    def emit(self, cond_ap=None):
        nc = self.nc
        tr = self
        with nc.Block() as block:
            def run(engname):
                def body(e):
                    if cond_ap is None:
                        inner(e)
                    else:
                        with e.register("r_cond_" + engname) as r:
                            e.reg_load(r, cond_ap)
                            with e.If_ne(r, 0):
                                inner(e)

                def inner(e):
                    eng = tr.engs[engname]
                    for waits, fn, inc in eng.queue:
                        for k, v in waits:
                            e.wait_ge(tr.sems[k], v)
                        if fn is None:
                            continue
                        res = fn(e)
                        if inc[1] == 16:
                            for ins in res:
                                ins.then_inc(tr.sems[inc[0]], 16)
                        else:
                            res.then_inc(tr.sems[inc[0]], 1)
                    eng.queue = []
                return body
            block.tensor(run("pe"))
            block.scalar(run("act"))
            block.vector(run("dve"))
            block.gpsimd(run("pool"))
            block.sync(run("sp"))


def _t5_bucket_np(rel):
    half = N_BUCKETS // 2
    max_exact = half // 2
    offset = np.where(rel > 0, half, 0)
    n = np.abs(rel)
    nf = np.maximum(n, 1).astype(np.float32)
    large = max_exact + (np.log(nf / np.float32(max_exact)) / np.float32(math.log(REL_MAX_DIST / max_exact))
                         * np.float32(half - max_exact)).astype(np.int32)
    large = np.minimum(large, half - 1)
    return offset + np.where(n < max_exact, n, large)


TYPES = [(1, -64), (1, 64), (4, -64), (4, 64), (16, 0)]
HANK_W = 255


def _const_onehot():
    oh = np.zeros((33, 5, 256), np.float32)
    for ti, (dil, off) in enumerate(TYPES):
        for u in range(HANK_W):
            rel = u - 127 + off
            if abs(rel) <= 64:
                b = int(_t5_bucket_np(np.array([rel * dil]))[0])
                oh[b, ti, u] = 1.0
            else:
                oh[32, ti, u] = NEG
        oh[32, ti, 255] = NEG
    return oh.reshape(33, 5 * 256)


def _const_rope():
    half = 32
    inv = (np.float32(ROPE_BASE) ** (-np.arange(half, dtype=np.float32) / np.float32(half))).astype(np.float32)
    pos = np.arange(S, dtype=np.float32)
    ang = (pos[:, None] * inv[None, :]).astype(np.float32)
    cos = np.cos(ang).astype(np.float32)
    sin = np.sin(ang).astype(np.float32)
    cosT = np.zeros((128, S), np.float32)
    sinT = np.zeros((128, S), np.float32)
    for p in range(128):
        d = p % 64
        j = d % 32
        cosT[p] = cos[:, j]
        sinT[p] = -sin[:, j] if d < 32 else sin[:, j]
    return cosT, sinT


def _const_relm():
    BIG = 1.0e9
    t = np.zeros((128, 514), np.float32)
    c = np.arange(128, dtype=np.float32)
    p = np.arange(128, dtype=np.float32)
    t[:, 0:128] = (c + 1.0)[None, :]
    t[:, 128:256] = (128.0 - c)[None, :]
    rel = c[None, :] - p[:, None]
    t[:, 256:384] = np.where(rel >= 0, rel, BIG)
    t[:, 384:512] = np.where(rel < 0, -rel, BIG)
    t[:, 512] = 127.0 - p
    t[:, 513] = p
    return t


def build_program(dbg=None, stop_after=None):
    dbg = dbg or []
    nc = bass.Bass("TRN2", target_bir_lowering=False)

    def din(name, shape, dt=F32):
        return nc.dram_tensor(name, list(shape), dt, kind="ExternalInput").ap()

    x_d = din("x", [S, D])
    win_d = din("w_in", [D, PROJ_W + 1024])
    wout_d = din("w_out", [D, D])
    nmix_d = din("norm_mix_t", [128, 8])
    nffn_d = din("norm_ffn_t", [128, 8])
    nfin_d = din("norm_final", [1, D])
    again_d = din("attn_gain_t", [64, 8])
    relb_d = din("rel_bias", [32, 8])
    rdec_d = din("ret_decay", [1, 16])
    wr_d = din("w_router", [D, 20])
    br_d = din("b_router", [1, 20])
    w1_d = din("w1", [16, D, 512])
    w3_d = din("w3", [16, D, 512])
    w2_d = din("w2", [16, 512, D])
    c_ident_d = din("c_ident", [128, 128])
    c_oh_d = din("c_onehot", [33, 5 * 256])
    c_cos_d = din("c_cos", [128, S])
    c_sin_d = din("c_sin", [128, S])
    c_relm_d = din("c_relm", [128, 514])
    nffr_d = din("norm_ffn_row", [1, D])
    c_ustr_d = din("c_ustr", [128, 128])
    c_iota_d = din("c_iota16", [128, 17])
    out_d = nc.dram_tensor("out", [S, D], F32, kind="ExternalOutput").ap()
    hank_d = nc.dram_tensor("hank_scratch", [8, 5 * 256], F32, kind="Internal").ap()

    dbg_outs = {}

    with ExitStack() as es:
        tr = Tracker(nc, es)

        def sb(name, shape, dt, stack=es):
            return stack.enter_context(nc.sbuf_tensor(name, list(shape), dt))

        def ps(name, shape, dt, stack=es):
            return stack.enter_context(nc.psum_tensor(name, list(shape), dt))

        ident_f = sb("ident_f", [128, 128], F32)
        ident_b = sb("ident_b", [128, 128], BF16)
        ones_f = sb("ones_f", [128, 128], F32)
        ones_b = sb("ones_b", [128, 128], BF16)
        nmix = sb("nmix", [128, 8], F32)
        nffn = sb("nffn", [128, 8], F32)
        again = sb("again", [64, 8], F32)
        xnT = sb("xnT", [128, 8, SP_], BF16)
        mixT = sb("mixT", [128, 8, S], BF16)
        attnT = mixT
        ssh = sb("ssh", [128, NT, 8], F32)

        B_const = Buf("const")
        B_xnT = Buf("xnT")
        B_attnT = Buf("attnT")
        B_retT = Buf("retT")
        B_ssh = Buf("ssh")

        def dump(name, ap, shape, dt=F32):
            o = nc.dram_tensor("dbg_" + name, list(shape), dt, kind="ExternalOutput").ap()
            dbg_outs[name] = o
            sem = tr.dma_sem()
            tr.barrier()
            tr.dma("sp", sem, lambda e: [e.dma_start(out=o, in_=ap)])

        s_c = tr.dma_sem("c0")
        tr.dma("sp", s_c, lambda e: [
            e.dma_start(out=ident_f[:], in_=c_ident_d[:, :]),
            e.dma_start(out=nmix[:], in_=nmix_d[:, :]),
            e.dma_start(out=nffn[:], in_=nffn_d[:, :]),
            e.dma_start(out=again[:], in_=again_d[:, :]),
        ], writes=[B_const], n=4)
        tr.op("pool", lambda e: e.memset(ones_f[:], 1.0), writes=[B_const])
        tr.op("pool", lambda e: e.memset(ones_b[:], 1.0), writes=[B_const])
        tr.op("dve", lambda e: e.tensor_copy(ident_b[:], ident_f[:]), reads=[B_const], writes=[B_const])
        tr.op("pool", lambda e: e.memset(xnT[:, :, 0:PAD], 0.0), writes=[B_xnT])
        tr.op("pool", lambda e: e.memset(xnT[:, :, PAD + S:SP_], 0.0), writes=[B_xnT])

        def rms_to_featmajor(stack, src_loader, gain_t, dstT, B_dst, tagp):
            pass

        with ExitStack() as ph:
            xt = [sb("xt%d" % i, [128, D], F32, ph) for i in range(2)]
            xs = [sb("xs%d" % i, [128, D], BF16, ph) for i in range(2)]
            junk = sb("junkA", [128, D], BF16, ph)
            ssq = [sb("ssq%d" % i, [128, 1], F32, ph) for i in range(2)]
            rstd = [sb("rstd%d" % i, [128, 1], F32, ph) for i in range(2)]
            tp = [ps("tpA%d" % i, [128, 8, 128], BF16, ph) for i in range(2)]
            B_xt = [Buf() for _ in range(2)]
            B_xs = [Buf() for _ in range(2)]
            B_ssq = [Buf() for _ in range(2)]
            B_rstd = [Buf() for _ in range(2)]
            B_tp = [Buf() for _ in range(2)]
            B_junk = Buf()
            s_x = [tr.dma_sem() for _ in range(2)]
            for i in range(NT):
                sl = i % 2
                tr.dma("sp", s_x[sl], lambda e, i=i, sl=sl: [e.dma_start(out=xt[sl][:], in_=x_d[i * 128:(i + 1) * 128, :])],
                       writes=[B_xt[sl]])
                tr.op("act", lambda e, sl=sl: e.activation(out=junk[:], in_=xt[sl][:], func=AF.Square, accum_out=ssq[sl][:]),
                      reads=[B_xt[sl]], writes=[B_junk, B_ssq[sl]])
                tr.op("act", lambda e, sl=sl: e.activation(out=rstd[sl][:], in_=ssq[sl][:], func=AF.Sqrt, scale=1.0 / D, bias=EPS),
                      reads=[B_ssq[sl]], writes=[B_rstd[sl]])
                tr.op("dve", lambda e, sl=sl: e.reciprocal(rstd[sl][:], rstd[sl][:]), reads=[B_rstd[sl]], writes=[B_rstd[sl]])
                tr.op("dve", lambda e, sl=sl: e.tensor_scalar(xs[sl][:], xt[sl][:], rstd[sl][:, 0:1], None, ALU.mult),
                      reads=[B_xt[sl], B_rstd[sl]], writes=[B_xs[sl]])

                def tps(e, sl=sl):
                    last = None
                    for k in range(8):
                        last = e.transpose(tp[sl][:, k, :], xs[sl][:, k * 128:(k + 1) * 128], ident_b[:])
                    return last
                tr.op("pe", tps, reads=[B_xs[sl], B_const], writes=[B_tp[sl]])
                tr.op("dve", lambda e, i=i, sl=sl: e.tensor_tensor(
                    xnT[:, :, PAD + i * 128:PAD + (i + 1) * 128], tp[sl][:, :, :],
                    nmix[:, :].unsqueeze(2).broadcast_to([128, 8, 128]), ALU.mult),
                    reads=[B_tp[sl], B_const], writes=[B_xnT])
            if "xnT" in dbg:
                dump("xnT", xnT[:, :, PAD:PAD + S], [128, 8, S], BF16)
            tr.barrier()
            tr.emit()

        if stop_after == "A":
            tr.barrier(); tr.emit()
            return nc, dbg_outs

        pa = es.enter_context(ExitStack())
        Tt = sb("Tt", [128, 8, 5, 128], BF16, pa)
        B_Tt = Buf("Tt")
        with ExitStack() as ph:
            relb = sb("relb", [33, 8], F32, ph)
            oh = sb("oh", [33, 5 * 256], F32, ph)
            Fsb = sb("Fsb", [8, 5 * 256], F32, ph)
            Tp = sb("Tp", [128, 8, 5, 128], F32, ph)
            Fps = ps("Fps", [8, 3, 512], F32, ph)
            B_relb, B_oh, B_F, B_Fps, B_hank, B_Tp = Buf(), Buf(), Buf(), Buf(), Buf(), Buf()
            s_t = tr.dma_sem("t0")
            tr.op("pool", lambda e: e.memset(relb[32:33, :], 1.0), writes=[B_relb])
            tr.dma("sp", s_t, lambda e: [e.dma_start(out=relb[0:32, :], in_=relb_d[:, :]),
                                         e.dma_start(out=oh[:], in_=c_oh_d[:, :])], writes=[B_relb, B_oh], n=2)

            def fmm(e):
                last = None
                for c in range(3):
                    w = 512 if c < 2 else 256
                    last = e.matmul(Fps[0:8, c, 0:w], relb[0:33, 0:8], oh[0:33, c * 512:c * 512 + w], start=True, stop=True)
                return last
            tr.op("pe", fmm, reads=[B_relb, B_oh], writes=[B_Fps])
            tr.op("dve", lambda e: e.tensor_copy(Fsb[:, 0:1024], Fps[0:8, 0:2, :].rearrange("p a b -> p (a b)")),
                  reads=[B_Fps], writes=[B_F])
            tr.op("dve", lambda e: e.tensor_copy(Fsb[:, 1024:1280], Fps[0:8, 2, 0:256]), reads=[B_Fps], writes=[B_F])
            s_h = tr.dma_sem("hank")
            tr.dma("sp", s_h, lambda e: [e.dma_start(out=hank_d[:, :], in_=Fsb[:])], reads=[B_F], writes=[B_hank])
            tr.dma("sp", s_h, lambda e: [e.dma_start(out=Tp[:, h, :, :], in_=bass.AP(hank_d.tensor, h * 5 * 256, [[1, 128], [256, 5], [1, 128]]))
                                         for h in range(8)], reads=[B_hank], writes=[B_Tp], n=8)
            tr.op("dve", lambda e: e.tensor_copy(Tt[:], Tp[:, :, :, ::-1]), reads=[B_Tp], writes=[B_Tt])
            if "Tt" not in dbg:
                tr.op("act", lambda e: e.activation(out=Tt[:], in_=Tt[:], func=AF.Exp), reads=[B_Tt], writes=[B_Tt])
            if "Tt" in dbg:
                dump("Tt", Tt[:], [128, 8, 5, 128], BF16)
            tr.barrier()
            tr.emit()

        with ExitStack() as ph:
            wq = sb("wq", [128, 8, 256], BF16, ph)
            wk = sb("wk", [128, 8, 256], BF16, ph)
            wv = sb("wv", [128, 8, 256], BF16, ph)
            qT = sb("qT", [128, 2, S], BF16, ph)
            kT = sb("kT", [128, 2, SP_], BF16, ph)
            Vd1 = sb("Vd1", [128, 17, 4, 65], BF16, ph)
            Vd4 = sb("Vd4", [128, 20, 4, 65], BF16, ph)
            Vd16 = sb("Vd16", [128, 16, 4, 65], BF16, ph)
            acc = sb("acc", [128, 4, S], F32, ph)
            vT = acc[:, :, :].rearrange("p a b -> p (a b)")[:, 0:SP_].bitcast(BF16).rearrange("p (c t) -> p c t", c=2)
            Pt = [sb("Pt%d" % i, [128, 2, 2, 128], BF16, ph) for i in range(3)]
            Pt2 = [sb("Pt2_%d" % i, [128, 2, 2, 128], BF16, ph) for i in range(3)]
            rd = [sb("rd%d" % i, [64, 512], F32, ph) for i in range(2)]
            a32 = [sb("a32_%d" % i, [64, 512], F32, ph) for i in range(2)]
            sq = [sb("sq%d" % i, [64, 512], BF16, ph) for i in range(2)]
            S2 = [ps("S2_%d" % i, [128, 2, 512], F32, ph) for i in range(2)]
            pp = [S2[0][:, 0, :], S2[0][:, 1, :]]
            Ops = [ps("Ops%d" % i, [65, 4, 128], F32, ph) for i in range(2)]
            tpV = [S2[1][:, i, 0:128].bitcast(BF16).rearrange("p (c t) -> p c t", c=2) for i in range(2)]
            bcps = [Ops[i][0:64, :, :].rearrange("p a b -> p (a b)") for i in range(2)]
            ssps = ps("ssps", [128, 4], F32, ph)
            B_wq, B_wk, B_wv, B_qT, B_kT = Buf(), Buf(), Buf(), Buf(), Buf()
            B_V = {"d1": [Buf() for _ in range(17)], "d4": [Buf() for _ in range(20)], "d16": [Buf() for _ in range(16)]}
            B_acc = Buf()
            B_Pt = [Buf() for _ in range(3)]
            B_Pt2 = [Buf() for _ in range(3)]
            B_bank = [[Buf(), Buf()], [Buf(), Buf()]]
            B_pp = B_bank[0]
            B_Sps = B_bank[1]
            B_Ops = [Buf() for _ in range(2)]
            B_rd = [Buf() for _ in range(2)]
            B_ssps = Buf()
            B_a32 = [Buf() for _ in range(2)]
            B_sq = [Buf() for _ in range(2)]
            s_w = [tr.dma_sem() for _ in range(3)]

            tr.op("pool", lambda e: e.memset(kT[:, :, 0:PAD], 0.0), writes=[B_kT])
            tr.op("pool", lambda e: e.memset(kT[:, :, PAD + S:SP_], 0.0), writes=[B_kT])
            allV = B_V["d1"] + B_V["d4"] + B_V["d16"]
            tr.op("pool", lambda e: e.memset(Vd1[:, :, :, 64:65], 1.0), writes=B_V["d1"])
            tr.op("pool", lambda e: e.memset(Vd4[:, :, :, 64:65], 1.0), writes=B_V["d4"])
            tr.op("pool", lambda e: e.memset(Vd16[:, :, :, 64:65], 1.0), writes=B_V["d16"])
            tr.op("pool", lambda e: e.memset(Vd1[0:64, 0, :, 64:65], 0.0), writes=[B_V["d1"][0]])
            tr.op("pool", lambda e: e.memset(Vd1[64:128, 16, :, 64:65], 0.0), writes=[B_V["d1"][16]])
            for r in range(4):
                tr.op("pool", lambda e, r=r: e.memset(Vd4[0:64, r * 5, :, 64:65], 0.0), writes=[B_V["d4"][r * 5]])
                tr.op("pool", lambda e, r=r: e.memset(Vd4[64:128, r * 5 + 4, :, 64:65], 0.0), writes=[B_V["d4"][r * 5 + 4]])

            ppc = [0]
            evc = [0]

            def evac_copy(dst_fn, src_fn, reads, writes, scale=None):
                evc[0] += 1
                if scale is not None or evc[0] % 2 == 0:
                    sc = 1.0 if scale is None else scale
                    tr.op("act", lambda e: e.activation(out=dst_fn(), in_=src_fn(), func=AF.Copy, scale=sc), reads=reads, writes=writes)
                else:
                    tr.op("dve", lambda e: e.tensor_copy(dst_fn(), src_fn()), reads=reads, writes=writes)

            def attn_epilogue(hg):
                items = [(hh, tcq) for hh in range(4) for tcq in range(4)]

                def stage_a(ii):
                    hh, tcq = items[ii]
                    cs = slice(tcq * 512, (tcq + 1) * 512)
                    al = ii % 2
                    tr.op("pe", lambda e: e.matmul(bcps[al], ones_f[64:65, 0:64], acc[64:65, hh, cs], start=True, stop=True),
                          reads=[B_acc, B_const], writes=[B_Ops[al]])
                    tr.op("act", lambda e: e.activation(out=rd[al][:, :], in_=bcps[al], func=AF.Ln), reads=[B_Ops[al]], writes=[B_rd[al]])
                    tr.op("act", lambda e: e.activation(out=rd[al][:, :], in_=rd[al][:, :], func=AF.Exp, scale=-1.0), reads=[B_rd[al]], writes=[B_rd[al]])

                def stage_b(ii):
                    hh, tcq = items[ii]
                    h = 4 * hg + hh
                    cs = slice(tcq * 512, (tcq + 1) * 512)
                    al = ii % 2
                    tr.op("dve", lambda e: e.tensor_tensor(a32[al][:, :], acc[0:64, hh, cs], rd[al][:, :], ALU.mult),
                          reads=[B_acc, B_rd[al]], writes=[B_a32[al]])
                    tr.op("act", lambda e: e.activation(out=attnT[0:64, h, cs], in_=a32[al][:, :], func=AF.Copy, scale=again[0:64, h:h + 1]),
                          reads=[B_a32[al], B_const], writes=[B_attnT])
                    tr.op("act", lambda e: e.activation(out=sq[al][:, :], in_=a32[al][:, :], func=AF.Square),
                          reads=[B_a32[al]], writes=[B_sq[al]])

                    def ssmm(e):
                        last = None
                        for tt in range(4):
                            last = e.matmul(ssps[:, tt:tt + 1], sq[al][0:64, tt * 128:(tt + 1) * 128], ones_b[0:64, 0:1],
                                            start=True, stop=True)
                        return last
                    tr.op("pe", ssmm, reads=[B_sq[al], B_const], writes=[B_ssps])
                    tr.op("dve", lambda e: e.tensor_copy(ssh[:, 4 * tcq:4 * tcq + 4, h], ssps[:, 0:4]),
                          reads=[B_ssps], writes=[B_ssh])
                stage_a(0)
                for ii in range(len(items)):
                    if ii + 1 < len(items):
                        stage_a(ii + 1)
                    stage_b(ii)

            for hg in range(2):
                for wi, (wt, Bw, c0) in enumerate([(wq, B_wq, 0), (wk, B_wk, 512), (wv, B_wv, 1024)]):
                    src = win_d[:, c0 + hg * 256:c0 + hg * 256 + 256].rearrange("(k p) c -> p k c", p=128)
                    tr.dma("pool", s_w[wi], lambda e, wt=wt, src=src: [e.dma_start(out=wt[:], in_=src)], writes=[Bw])
                for (wt, Bw, dstT, Bd, off, scale) in [(wq, B_wq, qT, B_qT, 0, 0.125), (wk, B_wk, kT, B_kT, PAD, None)]:
                    for c2 in range(2):
                        for tc in range(4):
                            sl = ppc[0] % 2
                            ppc[0] += 1

                            def mm(e, wt=wt, c2=c2, tc=tc, sl=sl):
                                last = None
                                for k in range(8):
                                    last = e.matmul(pp[sl], wt[:, k, c2 * 128:(c2 + 1) * 128],
                                                    xnT[:, k, PAD + tc * 512:PAD + (tc + 1) * 512], start=(k == 0), stop=(k == 7))
                                return last
                            tr.op("pe", mm, reads=[Bw, B_xnT], writes=[B_pp[sl]])
                            evac_copy(lambda dstT=dstT, c2=c2, tc=tc, off=off: dstT[:, c2, off + tc * 512:off + (tc + 1) * 512],
                                      lambda sl=sl: pp[sl], [B_pp[sl]], [Bd], scale=scale)
                if hg > 0:
                    attn_epilogue(hg - 1)
                tr.op("pool", lambda e: e.memset(vT[:, :, 0:PAD], 0.0), writes=[B_acc])
                tr.op("pool", lambda e: e.memset(vT[:, :, PAD + S:SP_], 0.0), writes=[B_acc])
                for c2 in range(2):
                    for tc in range(4):
                        sl = ppc[0] % 2
                        ppc[0] += 1

                        def mmvt(e, c2=c2, tc=tc, sl=sl):
                            last = None
                            for k in range(8):
                                last = e.matmul(pp[sl], wv[:, k, c2 * 128:(c2 + 1) * 128],
                                                xnT[:, k, PAD + tc * 512:PAD + (tc + 1) * 512], start=(k == 0), stop=(k == 7))
                            return last
                        tr.op("pe", mmvt, reads=[B_wv, B_xnT], writes=[B_pp[sl]])
                        evac_copy(lambda c2=c2, tc=tc: vT[:, c2, PAD + tc * 512:PAD + (tc + 1) * 512],
                                  lambda sl=sl: pp[sl], [B_pp[sl]], [B_acc])
                vt = []
                for j in range(17):
                    vt.append(("d1", j, Vd1, (PAD + 128 * j - 64, 1)))
                for r in range(4):
                    for j in range(5):
                        vt.append(("d4", r * 5 + j, Vd4, (PAD + r + 4 * (128 * j - 64), 4)))
                for r in range(16):
                    vt.append(("d16", r, Vd16, (PAD + r, 16)))
                for vi, (dn, ti, Vt, (st, step)) in enumerate(vt):
                    tl = vi % 2

                    def tpv(e, st=st, step=step, tl=tl):
                        last = None
                        for c2 in range(2):
                            last = e.transpose(tpV[tl][:, c2, :], vT[:, c2, st:st + 127 * step + 1:step], ident_b[:])
                        return last
                    tr.op("pe", tpv, reads=[B_acc, B_const], writes=[B_Sps[tl]])
                    evac_copy(lambda Vt=Vt, ti=ti: Vt[:, ti, :, 0:64],
                              lambda tl=tl: tpV[tl][:, :, :].rearrange("p c (h d) -> p (c h) d", h=2),
                              [B_Sps[tl]], [B_V[dn][ti]])

                units = []
                for i in range(16):
                    units.append(dict(q=(128 * i, 1), acc=(128 * i, 1), first=True,
                                      keys=[("d1", i, Vd1, (PAD + 128 * i - 64, 1), 0), ("d1", i + 1, Vd1, (PAD + 128 * (i + 1) - 64, 1), 1)]))
                for r in range(4):
                    for i in range(4):
                        units.append(dict(q=(r + 512 * i, 4), acc=(r + 512 * i, 4), first=False,
                                          keys=[("d4", r * 5 + i, Vd4, (PAD + r + 4 * (128 * i - 64), 4), 2),
                                                ("d4", r * 5 + i + 1, Vd4, (PAD + r + 4 * (128 * (i + 1) - 64), 4), 3)]))
                for r in range(16):
                    units.append(dict(q=(r, 16), acc=(r, 16), first=False, keys=[("d16", r, Vd16, (PAD + r, 16), 4)]))
                steps = []
                for ui, u in enumerate(units):
                    for ki, key in enumerate(u["keys"]):
                        steps.append((ui, u, ki, key))

                def emit_qk(si, hg=hg):
                    ui, u, ki, (dn, ti, Vt, (kst, kstep), ty) = steps[si]
                    sl = si % 2
                    qst, qstep = u["q"]

                    def f(e):
                        last = None
                        for hh in range(4):
                            c2, par = hh // 2, hh % 2
                            base = 64 * par
                            last = e.matmul(S2[sl][:, par, c2 * 128:(c2 + 1) * 128], kT[base:base + 64, c2, kst:kst + 127 * kstep + 1:kstep],
                                            qT[base:base + 64, c2, qst:qst + 127 * qstep + 1:qstep], start=True, stop=True)
                        return last
                    tr.op("pe", f, reads=[B_kT, B_qT], writes=B_bank[sl])
                    pl = si % 3
                    tr.op("act", lambda e: e.activation(out=Pt[pl][:].rearrange("p a j c -> p a (j c)"), in_=S2[sl][:, :, 0:256], func=AF.Exp),
                          reads=B_bank[sl], writes=[B_Pt[pl]])
                    tr.op("pool" if si % 2 == 0 else "dve", lambda e: e.tensor_tensor(
                        Pt2[pl][:], Pt[pl][:], Tt[:, 4 * hg:4 * hg + 4, ty, :].rearrange("p (j a) c -> p a j c", a=2), ALU.mult),
                          reads=[B_Pt[pl], B_Tt], writes=[B_Pt2[pl]])

                def emit_pv(si, hg=hg):
                    ui, u, ki, (dn, ti, Vt, (kst, kstep), ty) = steps[si]
                    pl = si % 3
                    ol = ui % 2
                    nk = len(u["keys"])

                    def f(e):
                        last = None
                        for hh in range(4):
                            last = e.matmul(Ops[ol][0:65, hh, :], Vt[:, ti, hh, 0:65], Pt2[pl][:, hh % 2, hh // 2, :],
                                            start=(ki == 0 and hh == 0), stop=(ki == nk - 1), skip_group_check=True)
                        return last
                    tr.op("pe", f, reads=[B_V[dn][ti], B_Pt2[pl]], writes=[B_Ops[ol]])
                    if ki == nk - 1:
                        ast, astep = u["acc"]
                        dst = lambda: acc[0:65, :, ast:ast + 127 * astep + 1:astep]
                        if u["first"]:
                            tr.op("dve", lambda e: e.tensor_copy(dst(), Ops[ol][0:65, :, :]), reads=[B_Ops[ol]], writes=[B_acc])
                        else:
                            tr.op("dve", lambda e: e.tensor_tensor(dst(), dst(), Ops[ol][0:65, :, :], ALU.add),
                                  reads=[B_Ops[ol], B_acc], writes=[B_acc])

                emit_qk(0)
                emit_qk(1)
                for si in range(len(steps)):
                    if si + 2 < len(steps):
                        emit_qk(si + 2)
                    emit_pv(si)

                if "acc" in dbg and hg == 0:
                    dump("acc", acc[0:65, :, :], [65, 4, S], F32)
                if "qk" in dbg and hg == 0:
                    dump("qT", qT[:], [128, 2, S], BF16)
                    dump("kT", kT[:], [128, 2, SP_], BF16)
                if "v1" in dbg and hg == 0:
                    dump("Vd1", Vd1[:], [128, 17, 4, 65], BF16)
                if "v4" in dbg and hg == 0:
                    dump("Vd4", Vd4[:], [128, 20, 4, 65], BF16)
                    dump("Vd16", Vd16[:], [128, 16, 4, 65], BF16)

            attn_epilogue(1)
            if "attnT" in dbg:
                dump("attnT", mixT[0:64, :, :], [64, 8, S], BF16)
                dump("ssh", ssh[:], [128, NT, 8], F32)
            tr.barrier()
            tr.emit()
        pa.close()
        if stop_after == "C":
            tr.barrier(); tr.emit()
            return nc, dbg_outs
        B_retT = Buf("retT")

        with ExitStack() as ph:
            crel = sb("crel", [128, 514], F32, ph)
            dec = sb("dec", [128, 16], F32, ph)
            lg = sb("lg", [128, 16], F32, ph)
            lgPf = sb("lgPf", [128, 4], F32, ph)
            lgPb = sb("lgPb", [128, 4], F32, ph)
            QDF = sb("QDF", [128, 4, 128], F32, ph)
            QDB = sb("QDB", [128, 4, 128], F32, ph)
            DT = sb("DT", [128, 8, 128], F32, ph)
            DT2 = sb("DT2", [128, 8, 128], F32, ph)
            KDF = sb("KDF", [128, 8], F32, ph)
            KDB = sb("KDB", [128, 8], F32, ph)
            GCf = sb("GCf", [128, 4], F32, ph)
            GCb = sb("GCb", [128, 4], F32, ph)
            cosT = sb("cosT", [128, S], F32, ph)
            sinT = sb("sinT", [128, S], F32, ph)
            w2x = sb("w2x", [128, 8, 256], BF16, ph)
            qsw = [sb("qsw%d" % i, [128, 512], F32, ph) for i in range(2)]
            B_qsw = [Buf(), Buf()]
            wrv = sb("wrv", [128, 8, 256], BF16, ph)
            wrg = sb("wrg", [128, 8, 256], BF16, ph)
            rqT = sb("rqT", [128, 2, S], BF16, ph)
            rkT = sb("rkT", [128, 2, S], BF16, ph)
            Vt = sb("Vt", [128, NT, 256], BF16, ph)
            sgT = sb("sgT", [64, 4, S], BF16, ph)
            Ktok = sb("Ktok", [128, NT, 2, 128], BF16, ph)
            Sfb = sb("Sfb", [128, NT, 2, 64], BF16, ph)
            Sbb = sb("Sbb", [128, NT, 2, 64], BF16, ph)
            KV = sb("KV", [128, NT, 2, 64], F32, ph)
            B_KV = [Buf() for _ in range(NT)]
            Srf = sb("Srf", [128, 2, 64], F32, ph)
            Srb = sb("Srb", [128, 2, 64], F32, ph)
            t1 = [sb("t1_%d" % i, [128, 512], F32, ph) for i in range(2)]
            t2 = [sb("t2_%d" % i, [128, 512], F32, ph) for i in range(2)]
            Vs = [sb("Vs%d" % i, [128, 256], BF16, ph) for i in range(2)]
            Pm = [sb("Pm%d" % i, [128, 2, 2, 128], BF16, ph) for i in range(2)]
            Qf = [sb("Qf%d" % i, [128, 2, 128], BF16, ph) for i in range(2)]
            Qb = [sb("Qb%d" % i, [128, 2, 128], BF16, ph) for i in range(2)]
            sqyL = [sb("sqy%d" % i, [64, 512], BF16, ph) for i in range(2)]
            rs = sb("rs", [64, 512], F32, ph)
            rs2 = sb("rs2", [64, 512], F32, ph)
            ty = sb("ty", [64, 512], F32, ph)
            S4 = [ps("rS4_%d" % i, [128, 512], F32, ph) for i in range(4)]
            pp = S4[0:2]
            yps = ps("yps", [64, 4, 128], F32, ph)
            ssb = ps("ssb", [64, 512], F32, ph)
            G1 = ps("rG1", [128, 512], F32, ph)
            kvps = [ps("kvps0", [128, 2, 128], F32, ph), G1[:, 0:256].rearrange("p (a b) -> p a b", a=2)]
            ypsL = [yps, G1[0:64, :].rearrange("p (a b) -> p a b", a=4)]
            tpK = kvps[0][:, 0, :].bitcast(BF16).rearrange("p (c t) -> p c t", c=2)
            B_crel, B_dec, B_lg, B_tab, B_rope = Buf(), Buf(), Buf(), Buf(), Buf()
            B_w2x, B_wrv, B_wrg, B_rqT, B_rkT, B_sgT = Buf(), Buf(), Buf(), Buf(), Buf(), Buf()
            B_Vt = [Buf() for _ in range(NT)]
            B_Ktok = [Buf() for _ in range(NT)]
            B_Sfb = [Buf() for _ in range(NT)]
            B_Sbb = [Buf() for _ in range(NT)]
            B_Srf, B_Srb = Buf(), Buf()
            B_t1 = [Buf() for _ in range(2)]
            B_t2 = [Buf() for _ in range(2)]
            B_Vs = [Buf() for _ in range(2)]
            B_Pm = [Buf() for _ in range(2)]
            B_Qf = [Buf() for _ in range(2)]
            B_Qb = [Buf() for _ in range(2)]
            B_rs, B_ty, B_rs2 = Buf(), Buf(), Buf()
            B_sqyL = [Buf(), Buf()]
            B_S4 = [Buf() for _ in range(4)]
            B_pp = B_S4[0:2]
            B_yps, B_ssb = Buf(), Buf()
            B_kvps = [Buf(), Buf()]
            B_tpK = B_kvps[0]
            B_ypsL = [B_yps, B_kvps[1]]

            s_r = tr.dma_sem("r0")
            tr.dma("sp", s_r, lambda e: [
                e.dma_start(out=crel[:], in_=c_relm_d[:, :]),
                e.dma_start(out=dec[:], in_=rdec_d.partition_broadcast(128)),
                e.dma_start(out=cosT[:], in_=c_cos_d[:, :]),
                e.dma_start(out=sinT[:], in_=c_sin_d[:, :]),
            ], writes=[B_crel, B_dec, B_rope], n=4)
            tr.op("act", lambda e: e.activation(out=lg[:], in_=dec[:], func=AF.Exp), reads=[B_dec], writes=[B_lg])
            tr.op("dve", lambda e: e.tensor_scalar(lg[:], lg[:], -1.0, None, ALU.mult), reads=[B_lg], writes=[B_lg])
            tr.op("dve", lambda e: e.tensor_copy(lgPf[0:64, :], lg[0:64, 0:8:2]), reads=[B_lg], writes=[B_tab])
            tr.op("dve", lambda e: e.tensor_copy(lgPf[64:128, :], lg[64:128, 1:8:2]), reads=[B_lg], writes=[B_tab])
            tr.op("dve", lambda e: e.tensor_copy(lgPb[0:64, :], lg[0:64, 8:16:2]), reads=[B_lg], writes=[B_tab])
            tr.op("dve", lambda e: e.tensor_copy(lgPb[64:128, :], lg[64:128, 9:16:2]), reads=[B_lg], writes=[B_tab])
            for gp in range(4):
                tr.op("act", lambda e, gp=gp: e.activation(out=QDF[:, gp, :], in_=crel[:, 0:128], func=AF.Exp, scale=lgPf[:, gp:gp + 1]),
                      reads=[B_tab, B_crel], writes=[B_tab])
                tr.op("act", lambda e, gp=gp: e.activation(out=QDB[:, gp, :], in_=crel[:, 128:256], func=AF.Exp, scale=lgPb[:, gp:gp + 1]),
                      reads=[B_tab, B_crel], writes=[B_tab])
            for h in range(8):
                tr.op("act", lambda e, h=h: e.activation(out=DT[:, h, :], in_=crel[:, 256:384], func=AF.Exp, scale=lg[:, h:h + 1]),
                      reads=[B_lg, B_crel], writes=[B_tab])
                tr.op("act", lambda e, h=h: e.activation(out=DT2[:, h, :], in_=crel[:, 384:512], func=AF.Exp, scale=lg[:, 8 + h:9 + h]),
                      reads=[B_lg, B_crel], writes=[B_tab])
            tr.op("dve", lambda e: e.tensor_tensor(DT[:], DT[:], DT2[:], ALU.add), reads=[B_tab], writes=[B_tab])
            tr.op("act", lambda e: e.activation(out=KDF[:], in_=lg[:, 0:8], func=AF.Exp, scale=crel[:, 512:513]), reads=[B_lg, B_crel], writes=[B_tab])
            tr.op("act", lambda e: e.activation(out=KDB[:], in_=lg[:, 8:16], func=AF.Exp, scale=crel[:, 513:514]), reads=[B_lg, B_crel], writes=[B_tab])
            tr.op("act", lambda e: e.activation(out=GCf[:], in_=lgPf[:], func=AF.Exp, scale=128.0), reads=[B_tab], writes=[B_tab])
            tr.op("act", lambda e: e.activation(out=GCb[:], in_=lgPb[:], func=AF.Exp, scale=128.0), reads=[B_tab], writes=[B_tab])

            s_rw = [tr.dma_sem() for _ in range(3)]
            ppc = [0]
            if "rtab" in dbg:
                dump("DT", DT[:], [128, 8, 128], F32)
                dump("QDF", QDF[:], [128, 4, 128], F32)
                dump("KDF", KDF[:], [128, 8], F32)
                dump("GCf", GCf[:], [128, 4], F32)
            CUT = int(stop_after[1]) if (stop_after and stop_after[0] == "D" and len(stop_after) == 2) else 9
            for hg in (range(2) if stop_after != "D0" else []):
                for (c_plain, dstT, Bd, kscale) in [(1536, rqT, B_rqT, 1.0), (2048, rkT, B_rkT, 0.125)]:
                    srcA = win_d[:, c_plain + hg * 256:c_plain + hg * 256 + 256].rearrange("(k p) c -> p k c", p=128)
                    tr.dma("pool", s_rw[0], lambda e, srcA=srcA: [e.dma_start(out=w2x[:], in_=srcA)], writes=[B_w2x])
                    for pr in range(2):
                        for tc in range(4):
                            cs = slice(tc * 512, (tc + 1) * 512)
                            tl = ppc[0] % 2
                            ppc[0] += 1

                            def mmq(e, pr=pr, tc=tc, tl=tl):
                                last = None
                                for k in range(8):
                                    last = e.matmul(pp[tl][:, :], w2x[:, k, pr * 128:(pr + 1) * 128],
                                                    xnT[:, k, PAD + tc * 512:PAD + (tc + 1) * 512], start=(k == 0), stop=(k == 7))
                                return last
                            tr.op("pe", mmq, reads=[B_w2x, B_xnT], writes=[B_pp[tl]])
                            for (dst0, src0) in [(0, 32), (32, 0), (64, 96), (96, 64)]:
                                tr.op("act", lambda e, tl=tl, dst0=dst0, src0=src0: e.activation(
                                    out=qsw[tl][dst0:dst0 + 32, :], in_=pp[tl][src0:src0 + 32, :], func=AF.Copy),
                                    reads=[B_pp[tl]], writes=[B_qsw[tl]])
                            tr.op("dve", lambda e, cs=cs, tl=tl, kscale=kscale: e.scalar_tensor_tensor(t1[tl][:], pp[tl][:, :], kscale, cosT[:, cs], ALU.mult, ALU.mult),
                                  reads=[B_pp[tl], B_rope, B_qsw[tl]], writes=[B_t1[tl]])
                            tr.op("dve", lambda e, cs=cs, tl=tl, kscale=kscale: e.scalar_tensor_tensor(t2[tl][:], qsw[tl][:, :], kscale, sinT[:, cs], ALU.mult, ALU.mult),
                                  reads=[B_qsw[tl], B_rope], writes=[B_t2[tl]])
                            tr.op("pool", lambda e, cs=cs, tl=tl, dstT=dstT, pr=pr: e.tensor_tensor(dstT[:, pr, cs], t1[tl][:], t2[tl][:], ALU.add),
                                  reads=[B_t1[tl], B_t2[tl]], writes=[Bd])
                if CUT < 2:
                    continue
                srcV = win_d[:, 2560 + hg * 256:2560 + hg * 256 + 256].rearrange("(k p) c -> p k c", p=128)
                srcG = win_d[:, 3072 + hg * 256:3072 + hg * 256 + 256].rearrange("(k p) c -> p k c", p=128)
                tr.dma("pool", s_rw[1], lambda e, srcV=srcV: [e.dma_start(out=wrv[:], in_=srcV)], writes=[B_wrv])
                tr.dma("pool", s_rw[2], lambda e, srcG=srcG: [e.dma_start(out=wrg[:], in_=srcG)], writes=[B_wrg])
                for n in range(NT):
                    sl = ppc[0] % 2
                    ppc[0] += 1

                    def mmv(e, n=n, sl=sl):
                        last = None
                        for k in range(8):
                            last = e.matmul(pp[sl][:, 0:256], xnT[:, k, PAD + n * 128:PAD + (n + 1) * 128], wrv[:, k, :],
                                            start=(k == 0), stop=(k == 7))
                        return last
                    tr.op("pe", mmv, reads=[B_wrv, B_xnT], writes=[B_pp[sl]])
                    tr.op("act", lambda e, n=n, sl=sl: e.activation(out=Vt[:, n, :], in_=pp[sl][:, 0:256], func=AF.Copy),
                          reads=[B_pp[sl]], writes=[B_Vt[n]])
                for hh in range(4):
                    for tc in range(4):
                        sl = ppc[0] % 2
                        ppc[0] += 1

                        def mmg(e, hh=hh, tc=tc, sl=sl):
                            last = None
                            for k in range(8):
                                last = e.matmul(pp[sl][0:64, :], wrg[:, k, hh * 64:(hh + 1) * 64],
                                                xnT[:, k, PAD + tc * 512:PAD + (tc + 1) * 512], start=(k == 0), stop=(k == 7))
                            return last
                        tr.op("pe", mmg, reads=[B_wrg, B_xnT], writes=[B_pp[sl]])
                        tr.op("act", lambda e, hh=hh, tc=tc, sl=sl: e.activation(out=sgT[0:64, hh, tc * 512:(tc + 1) * 512], in_=pp[sl][0:64, :], func=AF.Silu),
                              reads=[B_pp[sl]], writes=[B_sgT])
                if CUT < 3:
                    continue
                for n in range(NT):
                    def tpk(e, n=n):
                        last = None
                        for pr in range(2):
                            last = e.transpose(tpK[:, pr, :], rkT[:, pr, n * 128:(n + 1) * 128], ident_b[:])
                        return last
                    tr.op("pe", tpk, reads=[B_rkT, B_const], writes=[B_tpK])
                    tr.op("dve", lambda e, n=n: e.tensor_copy(Ktok[:, n, :, :], tpK[:, :, :]), reads=[B_tpK], writes=[B_Ktok[n]])
                if CUT < 4:
                    continue
                dirs = [(list(range(NT)), KDF, GCf, Srf, B_Srf, Sfb, B_Sfb), (list(range(NT - 1, -1, -1)), KDB, GCb, Srb, B_Srb, Sbb, B_Sbb)]
                for di, (order, KD, GC, Srun, B_Srun, Sbf, B_Sbf) in enumerate(dirs):
                    for n in order:
                        vl = ppc[0] % 2
                        ppc[0] += 1
                        tr.op("pool", lambda e, n=n, vl=vl, KD=KD, hg=hg: e.tensor_tensor(
                            Vs[vl][:, :].rearrange("p (h d) -> p h d", h=4), Vt[:, n, :].rearrange("p (h d) -> p h d", h=4),
                            KD[:, 4 * hg:4 * hg + 4].unsqueeze(2).broadcast_to([128, 4, 64]), ALU.mult),
                            reads=[B_Vt[n], B_tab], writes=[B_Vs[vl]])

                        def mmkv(e, n=n, vl=vl):
                            last = None
                            for pr in range(2):
                                last = e.matmul(kvps[vl][:, pr, :], Ktok[:, n, pr, :], Vs[vl][:, pr * 128:(pr + 1) * 128], start=True, stop=True)
                            return last
                        tr.op("pe", mmkv, reads=[B_Ktok[n], B_Vs[vl]], writes=[B_kvps[vl]])
                        tr.op("act", lambda e, n=n, vl=vl: e.activation(out=KV[0:64, n, :, :], in_=kvps[vl][0:64, :, 0:64], func=AF.Copy),
                              reads=[B_kvps[vl]], writes=[B_KV[n]])
                        tr.op("dve", lambda e, n=n, vl=vl: e.tensor_copy(KV[64:128, n, :, :], kvps[vl][64:128, :, 64:128]),
                              reads=[B_kvps[vl]], writes=[B_KV[n]])
                    tr.op("pool", lambda e, Srun=Srun: e.memset(Srun[:], 0.0), writes=[B_Srun])
                    for n in order:
                        tr.op("act", lambda e, n=n, Srun=Srun, Sbf=Sbf: e.activation(out=Sbf[:, n, :, :], in_=Srun[:, :, :], func=AF.Copy),
                              reads=[B_Srun], writes=[B_Sbf[n]])
                        for pr in range(2):
                            tr.op("dve", lambda e, n=n, pr=pr, Srun=Srun, GC=GC, hg=hg: e.scalar_tensor_tensor(
                                Srun[:, pr, :], Srun[:, pr, :], GC[:, 2 * hg + pr:2 * hg + pr + 1], KV[:, n, pr, :], ALU.mult, ALU.add),
                                reads=[B_KV[n], B_Srun, B_tab], writes=[B_Srun])
                if CUT < 5:
                    continue
                def stage_a(n, hg=hg):
                    cs = slice(n * 128, (n + 1) * 128)
                    sl = n % 2
                    tr.op("pool", lambda e: e.tensor_tensor(Qf[sl][:], rqT[:, :, cs], QDF[:, 2 * hg:2 * hg + 2, :], ALU.mult),
                          reads=[B_rqT, B_tab], writes=[B_Qf[sl]])
                    tr.op("pool", lambda e: e.tensor_tensor(Qb[sl][:], rqT[:, :, cs], QDB[:, 2 * hg:2 * hg + 2, :], ALU.mult),
                          reads=[B_rqT, B_tab], writes=[B_Qb[sl]])

                    def mms(e):
                        last = None
                        for hh in range(4):
                            pr, par = hh // 2, hh % 2
                            base = 64 * par
                            last = e.matmul(S4[2 * sl + par][:, pr * 128:(pr + 1) * 128], rkT[base:base + 64, pr, cs],
                                            rqT[base:base + 64, pr, cs], start=True, stop=True)
                        return last
                    tr.op("pe", mms, reads=[B_rkT, B_rqT], writes=[B_S4[2 * sl], B_S4[2 * sl + 1]])
                    for par in range(2):
                        tr.op("dve", lambda e, par=par: e.tensor_tensor(
                            Pm[sl][:, par, :, :], S4[2 * sl + par][:, 0:256].rearrange("p (j c) -> p j c", j=2),
                            DT[:, 4 * hg + par:4 * hg + 4:2, :], ALU.mult),
                            reads=[B_S4[2 * sl + par], B_tab], writes=[B_Pm[sl]])

                def stage_b(n, hg=hg):
                    sl = n % 2
                    yb = ypsL[sl]

                    def mmy(e):
                        last = None
                        for hh in range(4):
                            pr, base = hh // 2, 64 * (hh % 2)
                            e.matmul(yb[0:64, hh, :], Vt[:, n, hh * 64:(hh + 1) * 64], Pm[sl][:, hh % 2, hh // 2, :], start=True, stop=False)
                            e.matmul(yb[0:64, hh, :], Sfb[base:base + 64, n, pr, :], Qf[sl][base:base + 64, pr, :], start=False, stop=False)
                            last = e.matmul(yb[0:64, hh, :], Sbb[base:base + 64, n, pr, :], Qb[sl][base:base + 64, pr, :], start=False, stop=True)
                        return last
                    tr.op("pe", mmy, reads=[B_Vt[n], B_Pm[sl], B_Sfb[n], B_Sbb[n], B_Qf[sl], B_Qb[sl]], writes=[B_ypsL[sl]])
                    tr.op("act", lambda e: e.activation(out=sqyL[sl][:], in_=yb[0:64, :, :].rearrange("p a b -> p (a b)"), func=AF.Square),
                          reads=[B_ypsL[sl]], writes=[B_sqyL[sl]])

                def stage_c(n, hg=hg):
                    cs = slice(n * 128, (n + 1) * 128)
                    sl = n % 2
                    yb = ypsL[sl]
                    tr.op("pe", lambda e: e.matmul(ssb[:, :], ones_b[0:64, 0:64], sqyL[sl][:, :], start=True, stop=True),
                          reads=[B_sqyL[sl], B_const], writes=[B_ssb])
                    tr.op("act", lambda e: e.activation(out=rs[:], in_=ssb[:, :], func=AF.Ln, scale=1.0 / 64, bias=EPS),
                          reads=[B_ssb], writes=[B_rs])
                    tr.op("act", lambda e: e.activation(out=rs2[:], in_=rs[:], func=AF.Exp, scale=-0.5), reads=[B_rs], writes=[B_rs2])
                    tr.op("dve", lambda e: e.tensor_tensor(ty[:], yb[0:64, :, :].rearrange("p a b -> p (a b)"), rs2[:], ALU.mult),
                          reads=[B_ypsL[sl], B_rs2], writes=[B_ty])
                    tr.op("dve", lambda e: e.tensor_tensor(mixT[64:128, 4 * hg:4 * hg + 4, cs], ty[:, :].rearrange("p (a b) -> p a b", a=4),
                                                           sgT[0:64, :, cs], ALU.mult),
                          reads=[B_ty, B_sgT], writes=[B_retT])
                if CUT >= 9:
                    stage_a(0)
                    stage_a(1)
                    stage_b(0)
                    for n in range(NT):
                        if n + 2 < NT:
                            stage_a(n + 2)
                        if n + 1 < NT:
                            stage_b(n + 1)
                        stage_c(n)
            if "retT" in dbg:
                dump("retT", mixT[64:128, :, :], [64, 8, S], BF16)
            tr.barrier()
            tr.emit()
        if stop_after and stop_after[0] == "D":
            tr.barrier(); tr.emit()
            return nc, dbg_outs

        CAP = MOE_CAP
        NSL = 16 * CAP
        BIGIDX = 1.0e6
        xe_d = nc.dram_tensor("xe_scratch", [NSL + 128, D], BF16, kind="Internal").ap()
        y_d = nc.dram_tensor("y_scratch", [NSL + 128, D], F32, kind="Internal").ap()
        hres = sb("hres", [128, NT, D], F32)
        gates = sb("gates", [128, NT, 16], F32)
        OH1 = sb("OH1", [128, NT, 16], F32)
        OH2 = sb("OH2", [128, NT, 16], F32)
        A12 = sb("A12", [128, 2, NT], F32)
        AV = sb("AV", [128, 2, NT], F32)
        DI = sb("DI", [128, 2, NT], mybir.dt.int32)
        flag = sb("flag", [1, 2], mybir.dt.int32)
        B_hres = [Buf() for _ in range(NT)]
        B_gates = Buf()
        B_OH, B_A, B_AV, B_DI, B_flag, B_xe, B_yd = Buf(), Buf(), Buf(), Buf(), Buf(), Buf(), Buf()
        hnT = xnT
        BIGM = 1.0e4
        with ExitStack() as ph:
            hn_d = nc.dram_tensor("hn_scratch", [S, D], BF16, kind="Internal").ap()
            hn_tok = [sb("hn_tok%d" % i, [128, D], BF16, ph) for i in range(2)]
            hn_ld = [sb("hn_ld%d" % i, [128, D], BF16, ph) for i in range(2)]
            B_hnl = [Buf() for _ in range(2)]
            B_hnd = Buf()
            s_hn = [tr.dma_sem() for _ in range(2)]
            s_hl = [tr.dma_sem() for _ in range(2)]
            wo = sb("wo", [128, 8, D], BF16, ph)
            wr32 = sb("wr32", [128, 8, 20], F32, ph)
            brB = sb("brB", [128, 20], F32, ph)
            nffnB = sb("nffnB", [128, D], F32, ph)
            ustr = sb("ustr", [128, 128], F32, ph)
            iota16 = sb("iota16", [128, 17], F32, ph)
            zt = sb("zt", [128, D], BF16, ph)
            xt = [sb("ext%d" % i, [128, D], F32, ph) for i in range(2)]
            hsL = [sb("hs%d" % i, [128, D], F32, ph) for i in range(2)]
            junk = sb("junkE", [128, D], BF16, ph)
            hn32L = [sb("hn32_%d" % i, [128, 8, 128], F32, ph) for i in range(2)]
            sm = [sb("smallE%d" % i, [128, 24], F32, ph) for i in range(2)]
            Lg = [sb("Lg%d" % i, [128, 20], F32, ph) for i in range(2)]
            elm = [sb("elm%d" % i, [128, 16], F32, ph) for i in range(2)]
            elm2 = [sb("elm2%d" % i, [128, 16], F32, ph) for i in range(2)]
            ohg = [sb("ohg%d" % i, [128, 4], F32, ph) for i in range(2)]
            eg = [sb("eg%d" % i, [128, 4], F32, ph) for i in range(2)]
            Mk = sb("Mk", [128, NT, 16], F32, ph)
            Pos = sb("Pos", [128, NT, 16], F32, ph)
            Tot = sb("Tot", [128, NT, 16], F32, ph)
            Off = sb("Off", [128, NT, 16], F32, ph)
            tmpR = sb("tmpR", [128, NT, 16], F32, ph)
            SL = sb("SL", [128, 2, NT], F32, ph)
            EI = sb("EI", [128, 2, NT], F32, ph)
            VL = sb("VL", [128, 2, NT], F32, ph)
            DF = sb("DF", [128, 2, NT], F32, ph)
            ovs = sb("ovs", [128, 4], F32, ph)
            oA = [ps("oA%d" % i, [128, 512], F32, ph) for i in range(2)]
            oR = [ps("oR%d" % i, [128, 512], F32, ph) for i in range(2)]
            tp32 = ps("tp32", [128, 8, 128], F32, ph)
            lgpsL = [ps("lgps%d" % i, [128, 20], F32, ph) for i in range(2)]
            B_wo, B_wr, B_junk = Buf(), Buf(), Buf()
            B_hsL = [Buf(), Buf()]
            B_hn32L = [Buf(), Buf()]
            B_lgpsL = [Buf(), Buf()]
            B_sm = [Buf() for _ in range(2)]
            B_g = [Buf() for _ in range(2)]
            B_hnt = [Buf() for _ in range(2)]
            B_xt = [Buf() for _ in range(2)]
            B_oA = [Buf() for _ in range(2)]
            B_oR = [Buf() for _ in range(2)]
            B_tp32, B_r = Buf(), Buf()
            B_zt = Buf()
            s_e = tr.dma_sem("e0")
            s_ex = [tr.dma_sem() for _ in range(2)]
            tr.dma("pool", s_e, lambda e: [
                e.dma_start(out=wo[0:64, :, :], in_=wout_d[0:512, :].rearrange("(h p) c -> p h c", p=64)),
                e.dma_start(out=wo[64:128, :, :], in_=wout_d[512:1024, :].rearrange("(h p) c -> p h c", p=64))], writes=[B_wo], n=2)
            s_e2 = tr.dma_sem("e1")
            tr.dma("sp", s_e2, lambda e: [
                e.dma_start(out=wr32[:], in_=wr_d[:, :].rearrange("(k p) c -> p k c", p=128)),
                e.dma_start(out=brB[:], in_=br_d.partition_broadcast(128)),
                e.dma_start(out=nffnB[:], in_=nffr_d.partition_broadcast(128)),
                e.dma_start(out=ustr[:], in_=c_ustr_d[:, :]),
                e.dma_start(out=iota16[:], in_=c_iota_d[:, :])], writes=[B_wr], n=5)
            tr.op("pool", lambda e: e.memset(zt[:], 0.0), writes=[B_zt])
            s_z = tr.dma_sem("z0")
            nz = NSL // 128 + 1
            def tile_stages(i):
                cs = slice(i * 128, (i + 1) * 128)
                sl = i % 2
                SM = sm[sl]
                hs = hsL[sl]
                hn32 = hn32L[sl]
                lgps = lgpsL[sl]
                B_hs, B_hn32, B_lgps = B_hsL[sl], B_hn32L[sl], B_lgpsL[sl]
                tr.begin_capture()
                tr.dma("sp", s_ex[sl], lambda e, i=i, sl=sl: [e.dma_start(out=xt[sl][:], in_=x_d[i * 128:(i + 1) * 128, :])], writes=[B_xt[sl]])
                for half in range(2):
                    def mmo(e, cs=cs, half=half):
                        last = None
                        for h in range(8):
                            e.matmul(oA[half][:, :], mixT[0:64, h, cs], wo[0:64, h, half * 512:(half + 1) * 512], start=(h == 0), stop=(h == 7))
                            last = e.matmul(oR[half][:, :], mixT[64:128, h, cs], wo[64:128, h, half * 512:(half + 1) * 512], start=(h == 0), stop=(h == 7))
                        return last
                    tr.op("pe", mmo, reads=[B_attnT, B_retT, B_wo], writes=[B_oA[half], B_oR[half]])
                S_ = [B_sm[sl]]
                tr.op("dve", lambda e, i=i, SM=SM: e.tensor_reduce(SM[:, 0:1], ssh[:, i, :], AX.X, ALU.add), reads=[B_ssh], writes=S_)
                tr.op("act", lambda e, SM=SM: e.activation(out=SM[:, 1:2], in_=SM[:, 0:1], func=AF.Sqrt, scale=1.0 / 512, bias=EPS), reads=S_, writes=S_)
                tr.op("dve", lambda e, SM=SM: e.reciprocal(SM[:, 2:3], SM[:, 1:2]), reads=S_, writes=S_)
                for half in range(2):
                    hsl = slice(half * 512, (half + 1) * 512)
                    tr.op("dve", lambda e, i=i, half=half, hsl=hsl, sl=sl, SM=SM: e.scalar_tensor_tensor(
                        hres[:, i, hsl], oA[half][:, :], SM[:, 2:3], xt[sl][:, hsl], ALU.mult, ALU.add),
                        reads=[B_oA[half], B_sm[sl], B_xt[sl]], writes=[B_hres[i]])
                    tr.op("dve", lambda e, i=i, half=half, hsl=hsl: e.tensor_tensor(hres[:, i, hsl], hres[:, i, hsl], oR[half][:, :], ALU.add),
                          reads=[B_oR[half], B_hres[i]], writes=[B_hres[i]])
                tr.op("act", lambda e, i=i, SM=SM: e.activation(out=junk[:], in_=hres[:, i, :], func=AF.Square, accum_out=SM[:, 3:4]),
                      reads=[B_hres[i]], writes=[B_junk] + S_)
                tr.op("act", lambda e, SM=SM: e.activation(out=SM[:, 4:5], in_=SM[:, 3:4], func=AF.Sqrt, scale=1.0 / D, bias=EPS), reads=S_, writes=S_)
                tr.op("dve", lambda e, SM=SM: e.reciprocal(SM[:, 5:6], SM[:, 4:5]), reads=S_, writes=S_)
                tr.op("dve", lambda e, i=i, SM=SM: e.tensor_scalar(hs[:], hres[:, i, :], SM[:, 5:6], None, ALU.mult), reads=[B_hres[i]] + S_, writes=[B_hs])
                tr.op("pool", lambda e, sl=sl: e.tensor_tensor(hn_tok[sl][:], hs[:], nffnB[:], ALU.mult), reads=[B_hs, B_wr], writes=[B_hnt[sl]])
                tr.dma("sp", s_hn[sl], lambda e, i=i, sl=sl: [e.dma_start(out=hn_d[i * 128:(i + 1) * 128, :], in_=hn_tok[sl][:])],
                       reads=[B_hnt[sl]], writes=[B_hnd])

                st1 = tr.end_capture()
                tr.begin_capture()

                def tps32(e):
                    last = None
                    for k in range(8):
                        last = e.transpose(tp32[:, k, :], hs[:, k * 128:(k + 1) * 128], ident_f[:])
                    return last
                tr.op("pe", tps32, reads=[B_hs, B_const], writes=[B_tp32])
                tr.op("dve", lambda e: e.tensor_tensor(hn32[:], tp32[:, :, :], nffn[:, :].unsqueeze(2).broadcast_to([128, 8, 128]), ALU.mult),
                      reads=[B_tp32, B_const], writes=[B_hn32])
                tr.op("pool", lambda e, cs=cs: e.tensor_copy(hnT[:, :, PAD + cs.start:PAD + cs.stop], hn32[:]), reads=[B_hn32], writes=[B_xnT])

                def mmr(e):
                    last = None
                    for k in range(8):
                        last = e.matmul(lgps[:, :], hn32[:, k, :], wr32[:, k, :], start=(k == 0), stop=(k == 7))
                    return last
                tr.op("pe", mmr, reads=[B_hn32, B_wr], writes=[B_lgps])
                G = [B_g[sl]]
                L_, E1, E2, OG, EG = Lg[sl], elm[sl], elm2[sl], ohg[sl], eg[sl]
                o1 = lambda i=i: OH1[:, i, :]
                o2 = lambda i=i: OH2[:, i, :]
                tr.op("dve", lambda e, L_=L_: e.tensor_tensor(L_[:], lgps[:, :], brB[:], ALU.add), reads=[B_lgps, B_wr], writes=G)
                tr.op("dve", lambda e, L_=L_, SM=SM: e.tensor_reduce(SM[:, 8:9], L_[:, 0:4], AX.X, ALU.max), reads=G, writes=G)
                tr.op("dve", lambda e, SM=SM: e.tensor_scalar(SM[:, 9:10], SM[:, 8:9], -1.0, None, ALU.mult), reads=G, writes=G)
                tr.op("act", lambda e, L_=L_, SM=SM, EG=EG: e.activation(out=EG[:], in_=L_[:, 0:4], func=AF.Exp, bias=SM[:, 9:10], scale=1.0, accum_out=SM[:, 10:11]),
                      reads=G, writes=G)
                tr.op("dve", lambda e, SM=SM: e.reciprocal(SM[:, 11:12], SM[:, 10:11]), reads=G, writes=G)
                tr.op("dve", lambda e, L_=L_, SM=SM, OG=OG: e.tensor_tensor(OG[:], L_[:, 0:4], SM[:, 8:9].broadcast_to([128, 4]), ALU.is_equal), reads=G, writes=G)
                tr.op("dve", lambda e, OG=OG: e.tensor_scalar(OG[:], OG[:], BIGM, -BIGM, ALU.mult, ALU.add), reads=G, writes=G)
                tr.op("dve", lambda e, L_=L_, OG=OG, E1=E1: e.tensor_tensor(E1[:, :].rearrange("p (g j) -> p g j", g=4), L_[:, 4:20].rearrange("p (g j) -> p g j", g=4),
                                                                           OG[:, :].unsqueeze(2).broadcast_to([128, 4, 4]), ALU.add), reads=G, writes=G)
                tr.op("dve", lambda e, SM=SM, E1=E1: e.tensor_reduce(SM[:, 12:13], E1[:], AX.X, ALU.max), reads=G, writes=G)
                tr.op("dve", lambda e, SM=SM, E1=E1, o1=o1: e.tensor_tensor(o1(), E1[:], SM[:, 12:13].broadcast_to([128, 16]), ALU.is_equal), reads=G, writes=G + [B_OH])
                tr.op("dve", lambda e, E1=E1, E2=E2, o1=o1: e.scalar_tensor_tensor(E2[:], o1(), -BIGM, E1[:], ALU.mult, ALU.add), reads=G + [B_OH], writes=G)
                tr.op("dve", lambda e, SM=SM, E2=E2: e.tensor_reduce(SM[:, 13:14], E2[:], AX.X, ALU.max), reads=G, writes=G)
                tr.op("dve", lambda e, SM=SM, E2=E2, o2=o2: e.tensor_tensor(o2(), E2[:], SM[:, 13:14].broadcast_to([128, 16]), ALU.is_equal), reads=G, writes=G + [B_OH])
                tr.op("dve", lambda e, SM=SM: e.tensor_tensor(SM[:, 14:15], SM[:, 13:14], SM[:, 12:13], ALU.subtract), reads=G, writes=G)
                tr.op("act", lambda e, SM=SM: e.activation(out=SM[:, 15:16], in_=SM[:, 14:15], func=AF.Exp), reads=G, writes=G)
                tr.op("dve", lambda e, SM=SM: e.tensor_scalar(SM[:, 16:17], SM[:, 15:16], 1.0, None, ALU.add), reads=G, writes=G)
                tr.op("dve", lambda e, SM=SM: e.reciprocal(SM[:, 17:18], SM[:, 16:17]), reads=G, writes=G)
                tr.op("dve", lambda e, SM=SM, i=i: e.tensor_tensor(A12[:, 0, i:i + 1], SM[:, 17:18], SM[:, 11:12], ALU.mult), reads=G, writes=G + [B_A])
                tr.op("dve", lambda e, SM=SM, i=i: e.tensor_tensor(A12[:, 1, i:i + 1], A12[:, 0, i:i + 1], SM[:, 15:16], ALU.mult), reads=G + [B_A], writes=[B_A])
                st2 = tr.end_capture()
                return st1, st2

            stages = [tile_stages(i) for i in range(NT)]
            tr.replay(stages[0][0])
            for i in range(NT):
                if i + 1 < NT:
                    tr.replay(stages[i + 1][0], stages[i][1])
                else:
                    tr.replay(stages[i][1])

            R = [B_r]
            tr.op("dve", lambda e: e.tensor_tensor(Mk[:], OH1[:], OH2[:], ALU.add), reads=[B_OH], writes=R)
            mk2 = lambda t: t[:, :, :].rearrange("p a b -> p (a b)")
            with ExitStack() as ph2:
                pw, tt, ovp = oA[0], oA[1], oR[0]
                B_pw, B_tt, B_ovp = B_oA[0], B_oA[1], B_oR[0]
                tr.op("pe", lambda e: e.matmul(pw[:, 0:256], ustr[:, :], mk2(Mk), start=True, stop=True), reads=R + [B_wr], writes=[B_pw])
                tr.op("pe", lambda e: e.matmul(tt[:, 0:256], ones_f[:, :], mk2(Mk), start=True, stop=True), reads=R + [B_const], writes=[B_tt])
                tr.op("dve", lambda e: e.tensor_copy(mk2(Tot), tt[:, 0:256]), reads=[B_tt], writes=R)
                tr.op("pool", lambda e: e.memset(Off[:, 0, :], 0.0), writes=R)
                for i in range(1, NT):
                    tr.op("dve", lambda e, i=i: e.tensor_tensor(Off[:, i, :], Off[:, i - 1, :], Tot[:, i - 1, :], ALU.add), reads=R, writes=R)
                tr.op("dve", lambda e: e.tensor_tensor(mk2(Pos), pw[:, 0:256], mk2(Off), ALU.add), reads=[B_pw] + R, writes=R)
                for a, OHa in enumerate([OH1, OH2]):
                    tr.op("dve", lambda e, OHa=OHa: e.tensor_tensor(tmpR[:], OHa[:], Pos[:], ALU.mult), reads=R + [B_OH], writes=R)
                    tr.op("dve", lambda e, a=a: e.tensor_reduce(SL[:, a, :], tmpR[:], AX.X, ALU.add), reads=R, writes=R)
                    tr.op("dve", lambda e, OHa=OHa: e.tensor_tensor(tmpR[:], OHa[:], iota16[:, 0:16].unsqueeze(1).broadcast_to([128, NT, 16]), ALU.mult),
                          reads=R + [B_OH, B_wr], writes=R)
                    tr.op("dve", lambda e, a=a: e.tensor_reduce(EI[:, a, :], tmpR[:], AX.X, ALU.add), reads=R, writes=R)
                fl = lambda t: t[:, :, :].rearrange("p a b -> p (a b)")
                tr.op("dve", lambda e: e.tensor_scalar(fl(VL), fl(SL), float(CAP), None, ALU.is_lt), reads=R, writes=R)
                tr.op("dve", lambda e: e.scalar_tensor_tensor(fl(DF), fl(EI), float(CAP), fl(SL), ALU.mult, ALU.add), reads=R, writes=R)
                tr.op("dve", lambda e: e.tensor_scalar(ovs[:, 3:4], iota16[:, 16:17], float(NSL), None, ALU.add), reads=R + [B_wr], writes=R)
                tr.op("dve", lambda e: e.tensor_scalar(fl(DF), fl(DF), ovs[:, 3:4], None, ALU.subtract), reads=R, writes=R)
                tr.op("dve", lambda e: e.tensor_tensor(fl(DF), fl(DF), fl(VL), ALU.mult), reads=R, writes=R)
                tr.op("dve", lambda e: e.tensor_scalar(fl(DF), fl(DF), ovs[:, 3:4], None, ALU.add), reads=R, writes=R)
                tr.op("dve", lambda e: e.tensor_copy(fl(DI), fl(DF)), reads=R, writes=[B_DI])
                tr.op("dve", lambda e: e.tensor_tensor(fl(AV), fl(A12), fl(VL), ALU.mult), reads=R + [B_A], writes=[B_AV])
                tr.op("dve", lambda e: e.tensor_tensor(fl(DF), fl(A12), fl(AV), ALU.subtract), reads=R + [B_A, B_AV], writes=R)
                tr.op("dve", lambda e: e.tensor_tensor(gates[:], OH1[:], DF[:, 0, :].unsqueeze(2).broadcast_to([128, NT, 16]), ALU.mult),
                      reads=R + [B_OH], writes=[B_gates])
                tr.op("dve", lambda e: e.tensor_tensor(tmpR[:], OH2[:], DF[:, 1, :].unsqueeze(2).broadcast_to([128, NT, 16]), ALU.mult),
                      reads=R + [B_OH], writes=R)
                tr.op("dve", lambda e: e.tensor_tensor(gates[:], gates[:], tmpR[:], ALU.add), reads=R + [B_gates], writes=[B_gates])
                tr.op("dve", lambda e: e.tensor_scalar(fl(VL), fl(VL), -1.0, 1.0, ALU.mult, ALU.add), reads=R, writes=R)
                tr.op("dve", lambda e: e.tensor_reduce(ovs[:, 0:1], fl(VL), AX.X, ALU.add), reads=R, writes=R)
                tr.op("pe", lambda e: e.matmul(ovp[0:1, 0:1], ovs[:, 0:1], ones_f[:, 0:1], start=True, stop=True), reads=R + [B_const], writes=[B_ovp])
                tr.op("dve", lambda e: e.tensor_copy(ovs[0:1, 2:3], ovp[0:1, 0:1]), reads=[B_ovp], writes=R)
                if FORCE_FALLBACK:
                    tr.op("dve", lambda e: e.tensor_scalar(ovs[0:1, 2:3], ovs[0:1, 2:3], 1.0, None, ALU.add), reads=R, writes=R)
                tr.op("dve", lambda e: e.tensor_copy(flag[0:1, 0:1], ovs[0:1, 2:3]), reads=R, writes=[B_flag])
                s_sc = tr.dma_sem("scat")
                for i in range(NT):
                    sl = i % 2
                    tr.dma("sp", s_hl[sl], lambda e, i=i, sl=sl: [e.dma_start(out=hn_ld[sl][:], in_=hn_d[i * 128:(i + 1) * 128, :])],
                           reads=[B_hnd], writes=[B_hnl[sl]])
                    for a in range(2):
                        tr.dma("pool", s_sc, lambda e, i=i, a=a, sl=sl: [e.indirect_dma_start(
                            out=xe_d[:, :], out_offset=bass.IndirectOffsetOnAxis(ap=DI[:, a, i:i + 1], axis=0),
                            in_=hn_ld[sl][:, :], in_offset=None)],
                            reads=[B_DI, B_hnl[sl]], writes=[B_xe])
                if "route" in dbg:
                    dump("DI", DI[:], [128, 2, NT], mybir.dt.int32)
                    dump("AV", AV[:], [128, 2, NT], F32)
                    dump("gres", gates[:], [128, NT, 16], F32)
                    dump("flag", flag[:], [1, 2], mybir.dt.int32)
                if "hres" in dbg:
                    dump("hres", hres[:], [128, NT, D], F32)
                tr.barrier()
                tr.emit()
        if stop_after == "E":
            tr.barrier(); tr.emit()
            return nc, dbg_outs

        NS3 = CAP // 128
        with ExitStack() as ph:
            w1b = [sb("w1b%d" % i, [128, 8, 512], BF16, ph) for i in range(2)]
            w3b = [sb("w3b%d" % i, [128, 8, 512], BF16, ph) for i in range(2)]
            w2b = [sb("w2b%d" % i, [128, 4, D], BF16, ph) for i in range(2)]
            Xt = [mixT[:, 0:NS3, 0:D], mixT[:, NS3:2 * NS3, 0:D]]
            XT = [mixT[:, :, D:D + CAP], sb("XT1", [128, 8, CAP], BF16, ph)]
            hidT = mixT[:, 0:4, D + CAP:D + 2 * CAP]
            sil = [sb("sil%d" % i, [128, CAP], F32, ph) for i in range(2)]
            ysb = [sb("ysb%d" % i, [128, D], F32, ph) for i in range(2)]
            tpx = [ps("tpx%d" % i, [128, 8, 128], BF16, ph) for i in range(2)]
            h1p = [ps("h1p%d" % i, [128, 512], F32, ph)[:, 0:CAP] for i in range(2)]
            h3p = [ps("h3p%d" % i, [128, 512], F32, ph)[:, 0:CAP] for i in range(2)]
            yp = [ps("yp%d" % i, [128, 512], F32, ph) for i in range(2)]
            B_w = [Buf() for _ in range(2)]
            B_Xt = [Buf() for _ in range(2)]
            B_XT = [Buf() for _ in range(2)]
            B_hidT = Buf()
            B_sil = [Buf() for _ in range(2)]
            B_ysb = [Buf() for _ in range(2)]
            B_tpx = [Buf() for _ in range(2)]
            B_h1 = [Buf() for _ in range(2)]
            B_h3 = [Buf() for _ in range(2)]
            B_yp = [Buf() for _ in range(2)]
            s_m = [tr.dma_sem() for _ in range(2)]
            s_xl = [tr.dma_sem() for _ in range(2)]
            s_ys = [tr.dma_sem() for _ in range(2)]
            cnt = [0, 0, 0, 0]

            def load_w(ex):
                ws = ex % 2
                tr.dma("pool", s_m[ws], lambda e: [
                    e.dma_start(out=w1b[ws][:], in_=w1_d[ex].rearrange("(p k) f -> p k f", k=8)),
                    e.dma_start(out=w3b[ws][:], in_=w3_d[ex].rearrange("(p k) f -> p k f", k=8)),
                    e.dma_start(out=w2b[ws][:], in_=w2_d[ex].rearrange("(p k) d -> p k d", k=4))], writes=[B_w[ws]], n=3)

            def load_x(ex):
                xs_ = ex % 2
                tr.dma("sp", s_xl[xs_], lambda e: [e.dma_start(out=Xt[xs_], in_=xe_d[ex * CAP:(ex + 1) * CAP, :].rearrange("(s p) d -> p s d", p=128))],
                       reads=[B_xe], writes=[B_Xt[xs_]])

            def transposes(ex):
                xs_ = ex % 2
                for s3 in range(NS3):
                    tl = cnt[0] % 2
                    cnt[0] += 1

                    def tpf(e, s3=s3, tl=tl):
                        last = None
                        for k in range(8):
                            last = e.transpose(tpx[tl][:, k, :], Xt[xs_][:, s3, k:D:8], ident_b[:])
                        return last
                    tr.op("pe", tpf, reads=[B_Xt[xs_], B_const], writes=[B_tpx[tl]])
                    if cnt[0] % 2 == 0:
                        tr.op("act", lambda e, s3=s3, tl=tl: e.activation(out=XT[xs_][:, :, s3 * 128:(s3 + 1) * 128], in_=tpx[tl][:, :, :], func=AF.Copy),
                              reads=[B_tpx[tl]], writes=[B_XT[xs_]])
                    else:
                        tr.op("dve", lambda e, s3=s3, tl=tl: e.tensor_copy(XT[xs_][:, :, s3 * 128:(s3 + 1) * 128], tpx[tl][:, :, :]),
                              reads=[B_tpx[tl]], writes=[B_XT[xs_]])

            load_w(0)
            load_x(0)
            load_x(1)
            transposes(0)
            for ex in range(16):
                ws = ex % 2
                xs_ = ex % 2
                if ex + 1 < 16:
                    load_w(ex + 1)
                for ffc in range(4):
                    bl = cnt[1] % 2
                    sl2 = cnt[1] % 2
                    cnt[1] += 1

                    def mm13(e, ffc=ffc, wt=None, dst=None, ws=ws, xs_=xs_):
                        last = None
                        for k in range(8):
                            last = e.matmul(dst[:, :], wt[ws][:, k, ffc:512:4], XT[xs_][:, k, :], start=(k == 0), stop=(k == 7))
                        return last
                    tr.op("pe", lambda e, ffc=ffc, bl=bl, ws=ws, xs_=xs_, f=mm13: f(e, ffc, w1b, h1p[bl], ws, xs_), reads=[B_w[ws], B_XT[xs_]], writes=[B_h1[bl]])
                    tr.op("pe", lambda e, ffc=ffc, bl=bl, ws=ws, xs_=xs_, f=mm13: f(e, ffc, w3b, h3p[bl], ws, xs_), reads=[B_w[ws], B_XT[xs_]], writes=[B_h3[bl]])
                    tr.op("act", lambda e, sl2=sl2, bl=bl: e.activation(out=sil[sl2][:], in_=h1p[bl][:, :], func=AF.Silu), reads=[B_h1[bl]], writes=[B_sil[sl2]])
                    tr.op("dve", lambda e, sl2=sl2, ffc=ffc, bl=bl: e.tensor_tensor(hidT[:, ffc, :], sil[sl2][:], h3p[bl][:, :], ALU.mult),
                          reads=[B_sil[sl2], B_h3[bl]], writes=[B_hidT])
                if ex + 1 < 16:
                    transposes(ex + 1)
                if ex + 2 < 16:
                    load_x(ex + 2)
                for s3 in range(NS3):
                    yl = cnt[2] % 2
                    cnt[2] += 1
                    for half in range(2):
                        ol = cnt[3] % 2
                        cnt[3] += 1

                        def mm2(e, s3=s3, half=half, ol=ol, ws=ws):
                            last = None
                            for ffc in range(4):
                                last = e.matmul(yp[ol][:, :], hidT[:, ffc, s3 * 128:(s3 + 1) * 128],
                                                w2b[ws][:, ffc, half * 512:(half + 1) * 512], start=(ffc == 0), stop=(ffc == 3))
                            return last
                        tr.op("pe", mm2, reads=[B_hidT, B_w[ws]], writes=[B_yp[ol]])
                        if half == 0:
                            tr.op("act", lambda e, yl=yl, ol=ol: e.activation(out=ysb[yl][:, 0:512], in_=yp[ol][:, :], func=AF.Copy),
                                  reads=[B_yp[ol]], writes=[B_ysb[yl]])
                        else:
                            tr.op("dve", lambda e, yl=yl, ol=ol: e.tensor_copy(ysb[yl][:, 512:1024], yp[ol][:, :]),
                                  reads=[B_yp[ol]], writes=[B_ysb[yl]])
                    r0 = ex * CAP + s3 * 128
                    tr.dma("sp", s_ys[yl], lambda e, yl=yl, r0=r0: [e.dma_start(out=y_d[r0:r0 + 128, :], in_=ysb[yl][:])],
                           reads=[B_ysb[yl]], writes=[B_yd])
            tr.barrier()
            tr.emit()

        with ExitStack() as ph:
            tr2 = Tracker(nc, ph, prefix="fb_")
            w1b = [sb("fw1b%d" % i, [128, 8, 512], BF16, ph) for i in range(2)]
            w3b = [sb("fw3b%d" % i, [128, 8, 512], BF16, ph) for i in range(2)]
            w2b = [sb("fw2b%d" % i, [128, 4, D], BF16, ph) for i in range(2)]
            hid = [sb("fhid%d" % i, [128, 4, 512], BF16, ph) for i in range(2)]
            sil = [sb("fsil%d" % i, [128, 512], F32, ph) for i in range(2)]
            h1p = [ps("fh1p%d" % i, [128, 512], F32, ph) for i in range(2)]
            h3p = [ps("fh3p%d" % i, [128, 512], F32, ph) for i in range(2)]
            op_ = [ps("fop%d" % i, [128, 512], F32, ph) for i in range(4)]
            B_w = [Buf() for _ in range(2)]
            B_hid = [Buf() for _ in range(2)]
            B_sil = [Buf() for _ in range(2)]
            B_h1 = [Buf() for _ in range(2)]
            B_h3 = [Buf() for _ in range(2)]
            B_op = [Buf() for _ in range(4)]
            F_hres = [Buf() for _ in range(NT)]
            s_m = [tr2.dma_sem() for _ in range(2)]
            cnt = [0, 0, 0]
            for ex in range(16):
                ws = ex % 2
                tr2.dma("pool", s_m[ws], lambda e, ex=ex, ws=ws: [
                    e.dma_start(out=w1b[ws][:], in_=w1_d[ex].rearrange("(k p) f -> p k f", p=128)),
                    e.dma_start(out=w3b[ws][:], in_=w3_d[ex].rearrange("(k p) f -> p k f", p=128)),
                    e.dma_start(out=w2b[ws][:], in_=w2_d[ex].rearrange("(k p) d -> p k d", p=128))], writes=[B_w[ws]], n=3)
                for tc in range(4):
                    hsl = cnt[0] % 2
                    cnt[0] += 1
                    for ffc in range(4):
                        bl = cnt[1] % 2
                        cnt[1] += 1

                        def mm13(e, ws=ws, tc=tc, ffc=ffc, wt=None, dst=None):
                            last = None
                            for k in range(8):
                                last = e.matmul(dst[:, :], wt[ws][:, k, ffc * 128:(ffc + 1) * 128],
                                                hnT[:, k, PAD + tc * 512:PAD + (tc + 1) * 512], start=(k == 0), stop=(k == 7))
                            return last
                        tr2.op("pe", lambda e, ws=ws, tc=tc, ffc=ffc, bl=bl: mm13(e, ws, tc, ffc, w1b, h1p[bl]), reads=[B_w[ws]], writes=[B_h1[bl]])
                        tr2.op("pe", lambda e, ws=ws, tc=tc, ffc=ffc, bl=bl: mm13(e, ws, tc, ffc, w3b, h3p[bl]), reads=[B_w[ws]], writes=[B_h3[bl]])
                        tr2.op("act", lambda e, bl=bl: e.activation(out=sil[bl][:], in_=h1p[bl][:, :], func=AF.Silu), reads=[B_h1[bl]], writes=[B_sil[bl]])
                        tr2.op("dve", lambda e, bl=bl, hsl=hsl, ffc=ffc: e.tensor_tensor(hid[hsl][:, ffc, :], sil[bl][:], h3p[bl][:, :], ALU.mult),
                               reads=[B_sil[bl], B_h3[bl]], writes=[B_hid[hsl]])
                    for tt in range(4):
                        tile_i = tc * 4 + tt
                        for half in range(2):
                            ol = cnt[2] % 4
                            cnt[2] += 1

                            def mm2(e, ws=ws, hsl=hsl, tt=tt, half=half, ol=ol):
                                last = None
                                for ffc in range(4):
                                    last = e.matmul(op_[ol][:, :], hid[hsl][:, ffc, tt * 128:(tt + 1) * 128],
                                                    w2b[ws][:, ffc, half * 512:(half + 1) * 512], start=(ffc == 0), stop=(ffc == 3))
                                return last
                            tr2.op("pe", mm2, reads=[B_hid[hsl], B_w[ws]], writes=[B_op[ol]])
                            tr2.op("dve", lambda e, tile_i=tile_i, half=half, ol=ol, ex=ex: e.scalar_tensor_tensor(
                                hres[:, tile_i, half * 512:(half + 1) * 512], op_[ol][:, :], gates[:, tile_i, ex:ex + 1],
                                hres[:, tile_i, half * 512:(half + 1) * 512], ALU.mult, ALU.add),
                                reads=[B_op[ol], F_hres[tile_i]], writes=[F_hres[tile_i]])
            tr2.barrier()
            tr.barrier()
            tr.emit()
            tr2.emit(cond_ap=flag[0:1, 0:1])

        with ExitStack() as ph:
            nfB = sb("nfB", [128, D], F32, ph)
            y12 = [[sb("y%d_%d" % (a, i), [128, D], F32, ph) for a in range(2)] for i in range(2)]
            ot = [sb("ot%d" % i, [128, D], F32, ph) for i in range(2)]
            junk = sb("junkG", [128, D], BF16, ph)
            smg = sb("smG", [128, 2, 4], F32, ph)
            B_nf, B_junk = Buf(), Buf()
            B_y = [[Buf() for a in range(2)] for i in range(2)]
            B_ot = [Buf() for _ in range(2)]
            B_smg = [Buf() for _ in range(2)]
            s_g = tr.dma_sem("g0")
            s_o = [tr.dma_sem() for _ in range(2)]
            s_ga = [[tr.dma_sem() for a in range(2)] for i in range(2)]
            tr.dma("sp", s_g, lambda e: [e.dma_start(out=nfB[:], in_=nfin_d.partition_broadcast(128))], writes=[B_nf])
            for sl in range(2):
                for a in range(2):
                    tr.op("pool", lambda e, sl=sl, a=a: e.memset(y12[sl][a][:], 0.0), writes=[B_y[sl][a]])
            s_yz = tr.dma_sem("yz")
            tr.dma("sp", s_yz, lambda e: [e.dma_start(out=y_d[NSL:NSL + 128, :], in_=y12[0][0][:])], reads=[B_y[0][0]], writes=[B_yd])
            for i in range(NT):
                sl = i % 2
                for a in range(2):
                    tr.dma("pool", s_ga[sl][a], lambda e, i=i, a=a, sl=sl: [e.indirect_dma_start(
                        out=y12[sl][a][:, :], out_offset=None, in_=y_d[:, :],
                        in_offset=bass.IndirectOffsetOnAxis(ap=DI[:, a, i:i + 1], axis=0))],
                        reads=[B_DI, B_yd], writes=[B_y[sl][a]])
                    tr.op("dve", lambda e, i=i, a=a, sl=sl: e.scalar_tensor_tensor(hres[:, i, :], y12[sl][a][:], AV[:, a, i:i + 1], hres[:, i, :], ALU.mult, ALU.add),
                          reads=[B_y[sl][a], B_AV, B_hres[i]], writes=[B_hres[i]])
                tr.op("act", lambda e, i=i, sl=sl: e.activation(out=junk[:], in_=hres[:, i, :], func=AF.Square, accum_out=smg[:, sl, 0:1]),
                      reads=[B_hres[i]], writes=[B_junk, B_smg[sl]])
                tr.op("act", lambda e, sl=sl: e.activation(out=smg[:, sl, 1:2], in_=smg[:, sl, 0:1], func=AF.Sqrt, scale=1.0 / D, bias=EPS),
                      reads=[B_smg[sl]], writes=[B_smg[sl]])
                tr.op("dve", lambda e, sl=sl: e.reciprocal(smg[:, sl, 2:3], smg[:, sl, 1:2]), reads=[B_smg[sl]], writes=[B_smg[sl]])
                tr.op("dve", lambda e, i=i, sl=sl: e.scalar_tensor_tensor(ot[sl][:], hres[:, i, :], smg[:, sl, 2:3], nfB[:], ALU.mult, ALU.mult),
                      reads=[B_hres[i], B_smg[sl], B_nf], writes=[B_ot[sl]])
                tr.dma("sp", s_o[sl], lambda e, i=i, sl=sl: [e.dma_start(out=out_d[i * 128:(i + 1) * 128, :], in_=ot[sl][:])], reads=[B_ot[sl]])
            tr.barrier()
            tr.emit()

        tr.barrier()
        tr.emit()
    return nc, dbg_outs


def make_inputs(inputs):
    w_in = np.asarray(inputs["w_in"][0], np.float32)
    a = 512

    def swap_halves(w):
        w4 = w.reshape(D, 8, 2, 32)
        return np.ascontiguousarray(w4[:, :, ::-1, :]).reshape(D, 512)
    rq = w_in[:, 3 * a:3 * a + 512]
    rk = w_in[:, 3 * a + 512:3 * a + 1024]
    w_in_ext = np.ascontiguousarray(np.concatenate([w_in, swap_halves(rq), swap_halves(rk)], axis=1))
    cosT, sinT = _const_rope()
    shared = {
        "w_in": w_in_ext,
        "w_out": np.ascontiguousarray(inputs["w_out"][0], np.float32),
        "norm_mix_t": np.ascontiguousarray(np.asarray(inputs["norm_mix"][0], np.float32).reshape(8, 128).T),
        "norm_ffn_t": np.ascontiguousarray(np.asarray(inputs["norm_ffn"][0], np.float32).reshape(8, 128).T),
        "norm_final": np.ascontiguousarray(np.asarray(inputs["norm_final"], np.float32).reshape(1, D)),
        "attn_gain_t": np.ascontiguousarray(np.asarray(inputs["attn_out_gain"][0], np.float32).reshape(8, 64).T),
        "rel_bias": np.ascontiguousarray(inputs["rel_bias"], np.float32),
        "ret_decay": np.ascontiguousarray(np.concatenate([np.asarray(inputs["ret_decay_fwd"][0], np.float32),
                                                          np.asarray(inputs["ret_decay_bwd"][0], np.float32)]).reshape(1, 16)),
        "w_router": np.ascontiguousarray(np.concatenate([
            np.asarray(inputs["router_group_w"][0], np.float32),
            np.asarray(inputs["router_expert_w"][0], np.float32).transpose(1, 0, 2).reshape(D, 16)], axis=1)),
        "b_router": np.ascontiguousarray(np.concatenate([
            np.asarray(inputs["router_group_b"][0], np.float32).reshape(4),
            np.asarray(inputs["router_expert_b"][0], np.float32).reshape(16)]).reshape(1, 20)),
        "w1": np.ascontiguousarray(inputs["expert_w1"][0], np.float32),
        "w3": np.ascontiguousarray(inputs["expert_w3"][0], np.float32),
        "w2": np.ascontiguousarray(inputs["expert_w2"][0], np.float32),
        "c_ident": np.eye(128, dtype=np.float32),
        "c_onehot": _const_onehot(),
        "c_cos": cosT,
        "c_sin": sinT,
        "c_relm": _const_relm(),
        "norm_ffn_row": np.ascontiguousarray(np.asarray(inputs["norm_ffn"][0], np.float32).reshape(1, D)),
        "c_ustr": np.triu(np.ones((128, 128), np.float32), k=1),
        "c_iota16": np.ascontiguousarray(np.concatenate([np.broadcast_to(np.arange(16, dtype=np.float32)[None, :], (128, 16)),
                                                         np.arange(128, dtype=np.float32)[:, None]], axis=1)),
    }
    x = np.asarray(inputs["x"], np.float32)
    in_maps = []
    for b in range(8):
        m = dict(shared)
        m["x"] = np.ascontiguousarray(x[b])
        in_maps.append(m)
    return in_maps


def kernel(**inputs):
    nc, _ = build_program()
    in_maps = make_inputs(inputs)
    res = run_bass_kernel_spmd(nc, in_maps, core_ids=list(range(8)))
    return np.stack([np.asarray(r["out"], np.float32) for r in res.results], axis=0)
```

```python
import math
from contextlib import ExitStack

import numpy as np
import concourse.bass as bass
import concourse.mybir as mybir
from concourse.bass_utils import run_bass_kernel_spmd

F32, BF16 = mybir.dt.float32, mybir.dt.bfloat16
AF = mybir.ActivationFunctionType
ALU = mybir.AluOpType
AX = mybir.AxisListType

S = 2048
D = 1024
NT = S // 128
PAD = 256
SP_ = S + 2 * PAD
EPS = 1e-6
NEG = -30000.0
PROJ_W = 3584
N_BUCKETS = 32
REL_MAX_DIST = 1024
ROPE_BASE = 10000.0

SAME_SYNC = True
MOE_CAP = 384
FORCE_FALLBACK = False


class Buf:
    __slots__ = ("name", "w", "r")

    def __init__(self, name=""):
        self.name = name
        self.w = None
        self.r = {}


class Eng:
    def __init__(self, name, sem):
        self.name = name
        self.sem = sem
        self.count = 0
        self.waited = {}
        self.queue = []


class Tracker:
    def __init__(self, nc, es, prefix=""):
        self.nc = nc
        self.es = es
        self.prefix = prefix
        self.sems = {}
        self.engs = {}
        for name in ("pe", "act", "dve", "pool", "sp"):
            sem = es.enter_context(nc.semaphore("sem_" + prefix + name))
            self.sems[name] = sem
            self.engs[name] = Eng(name, sem)
        self.dma_counts = {}
        self.ndma = 0
        self.capture = None

    def dma_sem(self, name=None):
        name = name or ("dma%d" % self.ndma)
        self.ndma += 1
        sem = self.es.enter_context(self.nc.semaphore("sem_" + self.prefix + name))
        self.sems[name] = sem
        self.dma_counts[name] = 0
        return name

    def _deps(self, eng, reads, writes):
        deps = {}

        def add(k, v):
            if v > deps.get(k, 0):
                deps[k] = v
        for b in reads:
            if b.w is not None:
                add(*b.w)
        for b in writes:
            if b.w is not None:
                add(*b.w)
            for k, v in b.r.items():
                add(k, v)
        waits = []
        for k, v in deps.items():
            if k == eng.name and (eng.name in ("pe", "sp") or not SAME_SYNC):
                continue
            if eng.waited.get(k, 0) >= v:
                continue
            eng.waited[k] = v
            waits.append((k, v))
        return waits

    def op(self, engname, fn, reads=(), writes=()):
        if self.capture is not None:
            self.capture.append(("op", engname, fn, list(reads), list(writes)))
            return None
        eng = self.engs[engname]
        waits = self._deps(eng, reads, writes)
        eng.count += 1
        tok = (engname, eng.count)
        eng.queue.append((waits, fn, (engname, 1)))
        for b in reads:
            b.r[engname] = eng.count
        for b in writes:
            b.w = tok
            b.r = {}
        return tok

    def dma(self, qname, semname, fn, reads=(), writes=(), n=1):
        if self.capture is not None:
            self.capture.append(("dma", qname, semname, fn, list(reads), list(writes), n))
            return None
        eng = self.engs[qname]
        waits = self._deps(eng, reads, writes)
        self.dma_counts[semname] += 16 * n
        val = self.dma_counts[semname]
        eng.queue.append((waits, fn, (semname, 16)))
        for b in reads:
            b.r[semname] = val
        for b in writes:
            b.w = (semname, val)
            b.r = {}
        return (semname, val)

    def begin_capture(self):
        self.capture = []

    def end_capture(self):
        items, self.capture = self.capture, None
        return items

    def replay(self, *lists):
        merged = []
        for li, items in enumerate(lists):
            n = len(items)
            for k, it in enumerate(items):
                merged.append(((k + 0.5) / max(n, 1), li, k, it))
        merged.sort(key=lambda t: (t[0], t[1], t[2]))
        for _, _, _, it in merged:
            if it[0] == "op":
                self.op(it[1], it[2], it[3], it[4])
            else:
                self.dma(it[1], it[2], it[3], it[4], it[5], it[6])

    def barrier(self):
        for eng in self.engs.values():
            for k, other in self.engs.items():
                if k == eng.name:
                    continue
                if other.count > eng.waited.get(k, 0):
                    eng.waited[k] = other.count
                    eng.queue.append(([(k, other.count)], None, None))
            for k, v in self.dma_counts.items():
                if v > eng.waited.get(k, 0):
                    eng.waited[k] = v
                    eng.queue.append(([(k, v)], None, None))

    def emit(self, cond_ap=None):
        nc = self.nc
        tr = self
        with nc.Block() as block:
            def run(engname):
                def body(e):
                    if cond_ap is None:
                        inner(e)
                    else:
                        with e.register("r_cond_" + engname) as r:
                            e.reg_load(r, cond_ap)
                            with e.If_ne(r, 0):
                                inner(e)

                def inner(e):
                    eng = tr.engs[engname]
                    for waits, fn, inc in eng.queue:
                        for k, v in waits:
                            e.wait_ge(tr.sems[k], v)
                        if fn is None:
                            continue
                        res = fn(e)
                        if inc[1] == 16:
                            for ins in res:
                                ins.then_inc(tr.sems[inc[0]], 16)
                        else:
                            res.then_inc(tr.sems[inc[0]], 1)
                    eng.queue = []
                return body
            block.tensor(run("pe"))
            block.scalar(run("act"))
            block.vector(run("dve"))
            block.gpsimd(run("pool"))
            block.sync(run("sp"))


def _t5_bucket_np(rel):
    half = N_BUCKETS // 2
    max_exact = half // 2
    offset = np.where(rel > 0, half, 0)
    n = np.abs(rel)
    nf = np.maximum(n, 1).astype(np.float32)
    large = max_exact + (np.log(nf / np.float32(max_exact)) / np.float32(math.log(REL_MAX_DIST / max_exact))
                         * np.float32(half - max_exact)).astype(np.int32)
    large = np.minimum(large, half - 1)
    return offset + np.where(n < max_exact, n, large)


TYPES = [(1, -64), (1, 64), (4, -64), (4, 64), (16, 0)]
HANK_W = 255


def _const_onehot():
    oh = np.zeros((33, 5, 256), np.float32)
    for ti, (dil, off) in enumerate(TYPES):
        for u in range(HANK_W):
            rel = u - 127 + off
            if abs(rel) <= 64:
                b = int(_t5_bucket_np(np.array([rel * dil]))[0])
                oh[b, ti, u] = 1.0
            else:
                oh[32, ti, u] = NEG
        oh[32, ti, 255] = NEG
    return oh.reshape(33, 5 * 256)


def _const_rope():
    half = 32
    inv = (np.float32(ROPE_BASE) ** (-np.arange(half, dtype=np.float32) / np.float32(half))).astype(np.float32)
    pos = np.arange(S, dtype=np.float32)
    ang = (pos[:, None] * inv[None, :]).astype(np.float32)
    cos = np.cos(ang).astype(np.float32)
    sin = np.sin(ang).astype(np.float32)
    cosT = np.zeros((128, S), np.float32)
    sinT = np.zeros((128, S), np.float32)
    for p in range(128):
        d = p % 64
        j = d % 32
        cosT[p] = cos[:, j]
        sinT[p] = -sin[:, j] if d < 32 else sin[:, j]
    return cosT, sinT


def _const_relm():
    BIG = 1.0e9
    t = np.zeros((128, 514), np.float32)
    c = np.arange(128, dtype=np.float32)
    p = np.arange(128, dtype=np.float32)
    t[:, 0:128] = (c + 1.0)[None, :]
    t[:, 128:256] = (128.0 - c)[None, :]
    rel = c[None, :] - p[:, None]
    t[:, 256:384] = np.where(rel >= 0, rel, BIG)
    t[:, 384:512] = np.where(rel < 0, -rel, BIG)
    t[:, 512] = 127.0 - p
    t[:, 513] = p
    return t


def build_program(dbg=None, stop_after=None):
    dbg = dbg or []
    nc = bass.Bass("TRN2", target_bir_lowering=False)

    def din(name, shape, dt=F32):
        return nc.dram_tensor(name, list(shape), dt, kind="ExternalInput").ap()

    x_d = din("x", [S, D])
    win_d = din("w_in", [D, PROJ_W + 1024])
    wout_d = din("w_out", [D, D])
    nmix_d = din("norm_mix_t", [128, 8])
    nffn_d = din("norm_ffn_t", [128, 8])
    nfin_d = din("norm_final", [1, D])
    again_d = din("attn_gain_t", [64, 8])
    relb_d = din("rel_bias", [32, 8])
    rdec_d = din("ret_decay", [1, 16])
    wr_d = din("w_router", [D, 20])
    br_d = din("b_router", [1, 20])
    w1_d = din("w1", [16, D, 512])
    w3_d = din("w3", [16, D, 512])
    w2_d = din("w2", [16, 512, D])
    c_ident_d = din("c_ident", [128, 128])
    c_oh_d = din("c_onehot", [33, 5 * 256])
    c_cos_d = din("c_cos", [128, S])
    c_sin_d = din("c_sin", [128, S])
    c_relm_d = din("c_relm", [128, 514])
    nffr_d = din("norm_ffn_row", [1, D])
    c_ustr_d = din("c_ustr", [128, 128])
    c_iota_d = din("c_iota16", [128, 17])
    out_d = nc.dram_tensor("out", [S, D], F32, kind="ExternalOutput").ap()
    hank_d = nc.dram_tensor("hank_scratch", [8, 5 * 256], F32, kind="Internal").ap()

    dbg_outs = {}

    with ExitStack() as es:
        tr = Tracker(nc, es)

        def sb(name, shape, dt, stack=es):
            return stack.enter_context(nc.sbuf_tensor(name, list(shape), dt))

        def ps(name, shape, dt, stack=es):
            return stack.enter_context(nc.psum_tensor(name, list(shape), dt))

        ident_f = sb("ident_f", [128, 128], F32)
        ident_b = sb("ident_b", [128, 128], BF16)
        ones_f = sb("ones_f", [128, 128], F32)
        ones_b = sb("ones_b", [128, 128], BF16)
        nmix = sb("nmix", [128, 8], F32)
        nffn = sb("nffn", [128, 8], F32)
        again = sb("again", [64, 8], F32)
        xnT = sb("xnT", [128, 8, SP_], BF16)
        mixT = sb("mixT", [128, 8, S], BF16)
        attnT = mixT
        ssh = sb("ssh", [128, NT, 8], F32)

        B_const = Buf("const")
        B_xnT = Buf("xnT")
        B_attnT = Buf("attnT")
        B_retT = Buf("retT")
        B_ssh = Buf("ssh")

        def dump(name, ap, shape, dt=F32):
            o = nc.dram_tensor("dbg_" + name, list(shape), dt, kind="ExternalOutput").ap()
            dbg_outs[name] = o
            sem = tr.dma_sem()
            tr.barrier()
            tr.dma("sp", sem, lambda e: [e.dma_start(out=o, in_=ap)])

        s_c = tr.dma_sem("c0")
        tr.dma("sp", s_c, lambda e: [
            e.dma_start(out=ident_f[:], in_=c_ident_d[:, :]),
            e.dma_start(out=nmix[:], in_=nmix_d[:, :]),
            e.dma_start(out=nffn[:], in_=nffn_d[:, :]),
            e.dma_start(out=again[:], in_=again_d[:, :]),
        ], writes=[B_const], n=4)
        tr.op("pool", lambda e: e.memset(ones_f[:], 1.0), writes=[B_const])
        tr.op("pool", lambda e: e.memset(ones_b[:], 1.0), writes=[B_const])
        tr.op("dve", lambda e: e.tensor_copy(ident_b[:], ident_f[:]), reads=[B_const], writes=[B_const])
        tr.op("pool", lambda e: e.memset(xnT[:, :, 0:PAD], 0.0), writes=[B_xnT])
        tr.op("pool", lambda e: e.memset(xnT[:, :, PAD + S:SP_], 0.0), writes=[B_xnT])

        def rms_to_featmajor(stack, src_loader, gain_t, dstT, B_dst, tagp):
            pass

        with ExitStack() as ph:
            xt = [sb("xt%d" % i, [128, D], F32, ph) for i in range(2)]
            xs = [sb("xs%d" % i, [128, D], BF16, ph) for i in range(2)]
            junk = sb("junkA", [128, D], BF16, ph)
            ssq = [sb("ssq%d" % i, [128, 1], F32, ph) for i in range(2)]
            rstd = [sb("rstd%d" % i, [128, 1], F32, ph) for i in range(2)]
            tp = [ps("tpA%d" % i, [128, 8, 128], BF16, ph) for i in range(2)]
            B_xt = [Buf() for _ in range(2)]
            B_xs = [Buf() for _ in range(2)]
            B_ssq = [Buf() for _ in range(2)]
            B_rstd = [Buf() for _ in range(2)]
            B_tp = [Buf() for _ in range(2)]
            B_junk = Buf()
            s_x = [tr.dma_sem() for _ in range(2)]
            for i in range(NT):
                sl = i % 2
                tr.dma("sp", s_x[sl], lambda e, i=i, sl=sl: [e.dma_start(out=xt[sl][:], in_=x_d[i * 128:(i + 1) * 128, :])],
                       writes=[B_xt[sl]])
                tr.op("act", lambda e, sl=sl: e.activation(out=junk[:], in_=xt[sl][:], func=AF.Square, accum_out=ssq[sl][:]),
                      reads=[B_xt[sl]], writes=[B_junk, B_ssq[sl]])
                tr.op("act", lambda e, sl=sl: e.activation(out=rstd[sl][:], in_=ssq[sl][:], func=AF.Sqrt, scale=1.0 / D, bias=EPS),
                      reads=[B_ssq[sl]], writes=[B_rstd[sl]])
                tr.op("dve", lambda e, sl=sl: e.reciprocal(rstd[sl][:], rstd[sl][:]), reads=[B_rstd[sl]], writes=[B_rstd[sl]])
                tr.op("dve", lambda e, sl=sl: e.tensor_scalar(xs[sl][:], xt[sl][:], rstd[sl][:, 0:1], None, ALU.mult),
                      reads=[B_xt[sl], B_rstd[sl]], writes=[B_xs[sl]])

                def tps(e, sl=sl):
                    last = None
                    for k in range(8):
                        last = e.transpose(tp[sl][:, k, :], xs[sl][:, k * 128:(k + 1) * 128], ident_b[:])
                    return last
                tr.op("pe", tps, reads=[B_xs[sl], B_const], writes=[B_tp[sl]])
                tr.op("dve", lambda e, i=i, sl=sl: e.tensor_tensor(
                    xnT[:, :, PAD + i * 128:PAD + (i + 1) * 128], tp[sl][:, :, :],
                    nmix[:, :].unsqueeze(2).broadcast_to([128, 8, 128]), ALU.mult),
                    reads=[B_tp[sl], B_const], writes=[B_xnT])
            if "xnT" in dbg:
                dump("xnT", xnT[:, :, PAD:PAD + S], [128, 8, S], BF16)
            tr.barrier()
            tr.emit()

        if stop_after == "A":
            tr.barrier(); tr.emit()
            return nc, dbg_outs

        pa = es.enter_context(ExitStack())
        Tt = sb("Tt", [128, 8, 5, 128], BF16, pa)
        B_Tt = Buf("Tt")
        with ExitStack() as ph:
            relb = sb("relb", [33, 8], F32, ph)
            oh = sb("oh", [33, 5 * 256], F32, ph)
            Fsb = sb("Fsb", [8, 5 * 256], F32, ph)
            Tp = sb("Tp", [128, 8, 5, 128], F32, ph)
            Fps = ps("Fps", [8, 3, 512], F32, ph)
            B_relb, B_oh, B_F, B_Fps, B_hank, B_Tp = Buf(), Buf(), Buf(), Buf(), Buf(), Buf()
            s_t = tr.dma_sem("t0")
            tr.op("pool", lambda e: e.memset(relb[32:33, :], 1.0), writes=[B_relb])
            tr.dma("sp", s_t, lambda e: [e.dma_start(out=relb[0:32, :], in_=relb_d[:, :]),
                                         e.dma_start(out=oh[:], in_=c_oh_d[:, :])], writes=[B_relb, B_oh], n=2)

            def fmm(e):
                last = None
                for c in range(3):
                    w = 512 if c < 2 else 256
                    last = e.matmul(Fps[0:8, c, 0:w], relb[0:33, 0:8], oh[0:33, c * 512:c * 512 + w], start=True, stop=True)
                return last
            tr.op("pe", fmm, reads=[B_relb, B_oh], writes=[B_Fps])
            tr.op("dve", lambda e: e.tensor_copy(Fsb[:, 0:1024], Fps[0:8, 0:2, :].rearrange("p a b -> p (a b)")),
                  reads=[B_Fps], writes=[B_F])
            tr.op("dve", lambda e: e.tensor_copy(Fsb[:, 1024:1280], Fps[0:8, 2, 0:256]), reads=[B_Fps], writes=[B_F])
            s_h = tr.dma_sem("hank")
            tr.dma("sp", s_h, lambda e: [e.dma_start(out=hank_d[:, :], in_=Fsb[:])], reads=[B_F], writes=[B_hank])
            tr.dma("sp", s_h, lambda e: [e.dma_start(out=Tp[:, h, :, :], in_=bass.AP(hank_d.tensor, h * 5 * 256, [[1, 128], [256, 5], [1, 128]]))
                                         for h in range(8)], reads=[B_hank], writes=[B_Tp], n=8)
            tr.op("dve", lambda e: e.tensor_copy(Tt[:], Tp[:, :, :, ::-1]), reads=[B_Tp], writes=[B_Tt])
            if "Tt" not in dbg:
                tr.op("act", lambda e: e.activation(out=Tt[:], in_=Tt[:], func=AF.Exp), reads=[B_Tt], writes=[B_Tt])
            if "Tt" in dbg:
                dump("Tt", Tt[:], [128, 8, 5, 128], BF16)
            tr.barrier()
            tr.emit()

        with ExitStack() as ph:
            wq = sb("wq", [128, 8, 256], BF16, ph)
            wk = sb("wk", [128, 8, 256], BF16, ph)
            wv = sb("wv", [128, 8, 256], BF16, ph)
            qT = sb("qT", [128, 2, S], BF16, ph)
            kT = sb("kT", [128, 2, SP_], BF16, ph)
            Vd1 = sb("Vd1", [128, 17, 4, 65], BF16, ph)
            Vd4 = sb("Vd4", [128, 20, 4, 65], BF16, ph)
            Vd16 = sb("Vd16", [128, 16, 4, 65], BF16, ph)
            acc = sb("acc", [128, 4, S], F32, ph)
            vT = acc[:, :, :].rearrange("p a b -> p (a b)")[:, 0:SP_].bitcast(BF16).rearrange("p (c t) -> p c t", c=2)
            Pt = [sb("Pt%d" % i, [128, 2, 2, 128], BF16, ph) for i in range(3)]
            Pt2 = [sb("Pt2_%d" % i, [128, 2, 2, 128], BF16, ph) for i in range(3)]
            rd = [sb("rd%d" % i, [64, 512], F32, ph) for i in range(2)]
            a32 = [sb("a32_%d" % i, [64, 512], F32, ph) for i in range(2)]
            sq = [sb("sq%d" % i, [64, 512], BF16, ph) for i in range(2)]
            S2 = [ps("S2_%d" % i, [128, 2, 512], F32, ph) for i in range(2)]
            pp = [S2[0][:, 0, :], S2[0][:, 1, :]]
            Ops = [ps("Ops%d" % i, [65, 4, 128], F32, ph) for i in range(2)]
            tpV = [S2[1][:, i, 0:128].bitcast(BF16).rearrange("p (c t) -> p c t", c=2) for i in range(2)]
            bcps = [Ops[i][0:64, :, :].rearrange("p a b -> p (a b)") for i in range(2)]
            ssps = ps("ssps", [128, 4], F32, ph)
            B_wq, B_wk, B_wv, B_qT, B_kT = Buf(), Buf(), Buf(), Buf(), Buf()
            B_V = {"d1": [Buf() for _ in range(17)], "d4": [Buf() for _ in range(20)], "d16": [Buf() for _ in range(16)]}
            B_acc = Buf()
            B_Pt = [Buf() for _ in range(3)]
            B_Pt2 = [Buf() for _ in range(3)]
            B_bank = [[Buf(), Buf()], [Buf(), Buf()]]
            B_pp = B_bank[0]
            B_Sps = B_bank[1]
            B_Ops = [Buf() for _ in range(2)]
            B_rd = [Buf() for _ in range(2)]
            B_ssps = Buf()
            B_a32 = [Buf() for _ in range(2)]
            B_sq = [Buf() for _ in range(2)]
            s_w = [tr.dma_sem() for _ in range(3)]

            tr.op("pool", lambda e: e.memset(kT[:, :, 0:PAD], 0.0), writes=[B_kT])
            tr.op("pool", lambda e: e.memset(kT[:, :, PAD + S:SP_], 0.0), writes=[B_kT])
            allV = B_V["d1"] + B_V["d4"] + B_V["d16"]
            tr.op("pool", lambda e: e.memset(Vd1[:, :, :, 64:65], 1.0), writes=B_V["d1"])
            tr.op("pool", lambda e: e.memset(Vd4[:, :, :, 64:65], 1.0), writes=B_V["d4"])
            tr.op("pool", lambda e: e.memset(Vd16[:, :, :, 64:65], 1.0), writes=B_V["d16"])
            tr.op("pool", lambda e: e.memset(Vd1[0:64, 0, :, 64:65], 0.0), writes=[B_V["d1"][0]])
            tr.op("pool", lambda e: e.memset(Vd1[64:128, 16, :, 64:65], 0.0), writes=[B_V["d1"][16]])
            for r in range(4):
                tr.op("pool", lambda e, r=r: e.memset(Vd4[0:64, r * 5, :, 64:65], 0.0), writes=[B_V["d4"][r * 5]])
                tr.op("pool", lambda e, r=r: e.memset(Vd4[64:128, r * 5 + 4, :, 64:65], 0.0), writes=[B_V["d4"][r * 5 + 4]])

            ppc = [0]
            evc = [0]

            def evac_copy(dst_fn, src_fn, reads, writes, scale=None):
                evc[0] += 1
                if scale is not None or evc[0] % 2 == 0:
                    sc = 1.0 if scale is None else scale
                    tr.op("act", lambda e: e.activation(out=dst_fn(), in_=src_fn(), func=AF.Copy, scale=sc), reads=reads, writes=writes)
                else:
                    tr.op("dve", lambda e: e.tensor_copy(dst_fn(), src_fn()), reads=reads, writes=writes)

            def attn_epilogue(hg):
                items = [(hh, tcq) for hh in range(4) for tcq in range(4)]

                def stage_a(ii):
                    hh, tcq = items[ii]
                    cs = slice(tcq * 512, (tcq + 1) * 512)
                    al = ii % 2
                    tr.op("pe", lambda e: e.matmul(bcps[al], ones_f[64:65, 0:64], acc[64:65, hh, cs], start=True, stop=True),
                          reads=[B_acc, B_const], writes=[B_Ops[al]])
                    tr.op("act", lambda e: e.activation(out=rd[al][:, :], in_=bcps[al], func=AF.Ln), reads=[B_Ops[al]], writes=[B_rd[al]])
                    tr.op("act", lambda e: e.activation(out=rd[al][:, :], in_=rd[al][:, :], func=AF.Exp, scale=-1.0), reads=[B_rd[al]], writes=[B_rd[al]])

                def stage_b(ii):
                    hh, tcq = items[ii]
                    h = 4 * hg + hh
                    cs = slice(tcq * 512, (tcq + 1) * 512)
                    al = ii % 2
                    tr.op("dve", lambda e: e.tensor_tensor(a32[al][:, :], acc[0:64, hh, cs], rd[al][:, :], ALU.mult),
                          reads=[B_acc, B_rd[al]], writes=[B_a32[al]])
                    tr.op("act", lambda e: e.activation(out=attnT[0:64, h, cs], in_=a32[al][:, :], func=AF.Copy, scale=again[0:64, h:h + 1]),
                          reads=[B_a32[al], B_const], writes=[B_attnT])
                    tr.op("act", lambda e: e.activation(out=sq[al][:, :], in_=a32[al][:, :], func=AF.Square),
                          reads=[B_a32[al]], writes=[B_sq[al]])

                    def ssmm(e):
                        last = None
                        for tt in range(4):
                            last = e.matmul(ssps[:, tt:tt + 1], sq[al][0:64, tt * 128:(tt + 1) * 128], ones_b[0:64, 0:1],
                                            start=True, stop=True)
                        return last
                    tr.op("pe", ssmm, reads=[B_sq[al], B_const], writes=[B_ssps])
                    tr.op("dve", lambda e: e.tensor_copy(ssh[:, 4 * tcq:4 * tcq + 4, h], ssps[:, 0:4]),
                          reads=[B_ssps], writes=[B_ssh])
                stage_a(0)
                for ii in range(len(items)):
                    if ii + 1 < len(items):
                        stage_a(ii + 1)
                    stage_b(ii)

            for hg in range(2):
                for wi, (wt, Bw, c0) in enumerate([(wq, B_wq, 0), (wk, B_wk, 512), (wv, B_wv, 1024)]):
                    src = win_d[:, c0 + hg * 256:c0 + hg * 256 + 256].rearrange("(k p) c -> p k c", p=128)
                    tr.dma("pool", s_w[wi], lambda e, wt=wt, src=src: [e.dma_start(out=wt[:], in_=src)], writes=[Bw])
                for (wt, Bw, dstT, Bd, off, scale) in [(wq, B_wq, qT, B_qT, 0, 0.125), (wk, B_wk, kT, B_kT, PAD, None)]:
                    for c2 in range(2):
                        for tc in range(4):
                            sl = ppc[0] % 2
                            ppc[0] += 1

                            def mm(e, wt=wt, c2=c2, tc=tc, sl=sl):
                                last = None
                                for k in range(8):
                                    last = e.matmul(pp[sl], wt[:, k, c2 * 128:(c2 + 1) * 128],
                                                    xnT[:, k, PAD + tc * 512:PAD + (tc + 1) * 512], start=(k == 0), stop=(k == 7))
                                return last
                            tr.op("pe", mm, reads=[Bw, B_xnT], writes=[B_pp[sl]])
                            evac_copy(lambda dstT=dstT, c2=c2, tc=tc, off=off: dstT[:, c2, off + tc * 512:off + (tc + 1) * 512],
                                      lambda sl=sl: pp[sl], [B_pp[sl]], [Bd], scale=scale)
                if hg > 0:
                    attn_epilogue(hg - 1)
                tr.op("pool", lambda e: e.memset(vT[:, :, 0:PAD], 0.0), writes=[B_acc])
                tr.op("pool", lambda e: e.memset(vT[:, :, PAD + S:SP_], 0.0), writes=[B_acc])
                for c2 in range(2):
                    for tc in range(4):
                        sl = ppc[0] % 2
                        ppc[0] += 1

                        def mmvt(e, c2=c2, tc=tc, sl=sl):
                            last = None
                            for k in range(8):
                                last = e.matmul(pp[sl], wv[:, k, c2 * 128:(c2 + 1) * 128],
                                                xnT[:, k, PAD + tc * 512:PAD + (tc + 1) * 512], start=(k == 0), stop=(k == 7))
                            return last
                        tr.op("pe", mmvt, reads=[B_wv, B_xnT], writes=[B_pp[sl]])
                        evac_copy(lambda c2=c2, tc=tc: vT[:, c2, PAD + tc * 512:PAD + (tc + 1) * 512],
                                  lambda sl=sl: pp[sl], [B_pp[sl]], [B_acc])
                vt = []
                for j in range(17):
                    vt.append(("d1", j, Vd1, (PAD + 128 * j - 64, 1)))
                for r in range(4):
                    for j in range(5):
                        vt.append(("d4", r * 5 + j, Vd4, (PAD + r + 4 * (128 * j - 64), 4)))
                for r in range(16):
                    vt.append(("d16", r, Vd16, (PAD + r, 16)))
                for vi, (dn, ti, Vt, (st, step)) in enumerate(vt):
                    tl = vi % 2

                    def tpv(e, st=st, step=step, tl=tl):
                        last = None
                        for c2 in range(2):
                            last = e.transpose(tpV[tl][:, c2, :], vT[:, c2, st:st + 127 * step + 1:step], ident_b[:])
                        return last
                    tr.op("pe", tpv, reads=[B_acc, B_const], writes=[B_Sps[tl]])
                    evac_copy(lambda Vt=Vt, ti=ti: Vt[:, ti, :, 0:64],
                              lambda tl=tl: tpV[tl][:, :, :].rearrange("p c (h d) -> p (c h) d", h=2),
                              [B_Sps[tl]], [B_V[dn][ti]])

                units = []
                for i in range(16):
                    units.append(dict(q=(128 * i, 1), acc=(128 * i, 1), first=True,
                                      keys=[("d1", i, Vd1, (PAD + 128 * i - 64, 1), 0), ("d1", i + 1, Vd1, (PAD + 128 * (i + 1) - 64, 1), 1)]))
                for r in range(4):
                    for i in range(4):
                        units.append(dict(q=(r + 512 * i, 4), acc=(r + 512 * i, 4), first=False,
                                          keys=[("d4", r * 5 + i, Vd4, (PAD + r + 4 * (128 * i - 64), 4), 2),
                                                ("d4", r * 5 + i + 1, Vd4, (PAD + r + 4 * (128 * (i + 1) - 64), 4), 3)]))
                for r in range(16):
                    units.append(dict(q=(r, 16), acc=(r, 16), first=False, keys=[("d16", r, Vd16, (PAD + r, 16), 4)]))
                steps = []
                for ui, u in enumerate(units):
                    for ki, key in enumerate(u["keys"]):
                        steps.append((ui, u, ki, key))

                def emit_qk(si, hg=hg):
                    ui, u, ki, (dn, ti, Vt, (kst, kstep), ty) = steps[si]
                    sl = si % 2
                    qst, qstep = u["q"]

                    def f(e):
                        last = None
                        for hh in range(4):
                            c2, par = hh // 2, hh % 2
                            base = 64 * par
                            last = e.matmul(S2[sl][:, par, c2 * 128:(c2 + 1) * 128], kT[base:base + 64, c2, kst:kst + 127 * kstep + 1:kstep],
                                            qT[base:base + 64, c2, qst:qst + 127 * qstep + 1:qstep], start=True, stop=True)
                        return last
                    tr.op("pe", f, reads=[B_kT, B_qT], writes=B_bank[sl])
                    pl = si % 3
                    tr.op("act", lambda e: e.activation(out=Pt[pl][:].rearrange("p a j c -> p a (j c)"), in_=S2[sl][:, :, 0:256], func=AF.Exp),
                          reads=B_bank[sl], writes=[B_Pt[pl]])
                    tr.op("pool" if si % 2 == 0 else "dve", lambda e: e.tensor_tensor(
                        Pt2[pl][:], Pt[pl][:], Tt[:, 4 * hg:4 * hg + 4, ty, :].rearrange("p (j a) c -> p a j c", a=2), ALU.mult),
                          reads=[B_Pt[pl], B_Tt], writes=[B_Pt2[pl]])

                def emit_pv(si, hg=hg):
                    ui, u, ki, (dn, ti, Vt, (kst, kstep), ty) = steps[si]
                    pl = si % 3
                    ol = ui % 2
                    nk = len(u["keys"])

                    def f(e):
                        last = None
                        for hh in range(4):
                            last = e.matmul(Ops[ol][0:65, hh, :], Vt[:, ti, hh, 0:65], Pt2[pl][:, hh % 2, hh // 2, :],
                                            start=(ki == 0 and hh == 0), stop=(ki == nk - 1), skip_group_check=True)
                        return last
                    tr.op("pe", f, reads=[B_V[dn][ti], B_Pt2[pl]], writes=[B_Ops[ol]])
                    if ki == nk - 1:
                        ast, astep = u["acc"]
                        dst = lambda: acc[0:65, :, ast:ast + 127 * astep + 1:astep]
                        if u["first"]:
                            tr.op("dve", lambda e: e.tensor_copy(dst(), Ops[ol][0:65, :, :]), reads=[B_Ops[ol]], writes=[B_acc])
                        else:
                            tr.op("dve", lambda e: e.tensor_tensor(dst(), dst(), Ops[ol][0:65, :, :], ALU.add),
                                  reads=[B_Ops[ol], B_acc], writes=[B_acc])

                emit_qk(0)
                emit_qk(1)
                for si in range(len(steps)):
                    if si + 2 < len(steps):
                        emit_qk(si + 2)
                    emit_pv(si)

                if "acc" in dbg and hg == 0:
                    dump("acc", acc[0:65, :, :], [65, 4, S], F32)
                if "qk" in dbg and hg == 0:
                    dump("qT", qT[:], [128, 2, S], BF16)
                    dump("kT", kT[:], [128, 2, SP_], BF16)
                if "v1" in dbg and hg == 0:
                    dump("Vd1", Vd1[:], [128, 17, 4, 65], BF16)
                if "v4" in dbg and hg == 0:
                    dump("Vd4", Vd4[:], [128, 20, 4, 65], BF16)
                    dump("Vd16", Vd16[:], [128, 16, 4, 65], BF16)

            attn_epilogue(1)
            if "attnT" in dbg:
                dump("attnT", mixT[0:64, :, :], [64, 8, S], BF16)
                dump("ssh", ssh[:], [128, NT, 8], F32)
            tr.barrier()
            tr.emit()
        pa.close()
        if stop_after == "C":
            tr.barrier(); tr.emit()
            return nc, dbg_outs
        B_retT = Buf("retT")

        with ExitStack() as ph:
            crel = sb("crel", [128, 514], F32, ph)
            dec = sb("dec", [128, 16], F32, ph)
            lg = sb("lg", [128, 16], F32, ph)
            lgPf = sb("lgPf", [128, 4], F32, ph)
            lgPb = sb("lgPb", [128, 4], F32, ph)
            QDF = sb("QDF", [128, 4, 128], F32, ph)
            QDB = sb("QDB", [128, 4, 128], F32, ph)
            DT = sb("DT", [128, 8, 128], F32, ph)
            DT2 = sb("DT2", [128, 8, 128], F32, ph)
            KDF = sb("KDF", [128, 8], F32, ph)
            KDB = sb("KDB", [128, 8], F32, ph)
            GCf = sb("GCf", [128, 4], F32, ph)
            GCb = sb("GCb", [128, 4], F32, ph)
            cosT = sb("cosT", [128, S], F32, ph)
            sinT = sb("sinT", [128, S], F32, ph)
            w2x = sb("w2x", [128, 8, 256], BF16, ph)
            qsw = [sb("qsw%d" % i, [128, 512], F32, ph) for i in range(2)]
            B_qsw = [Buf(), Buf()]
            wrv = sb("wrv", [128, 8, 256], BF16, ph)
            wrg = sb("wrg", [128, 8, 256], BF16, ph)
            rqT = sb("rqT", [128, 2, S], BF16, ph)
            rkT = sb("rkT", [128, 2, S], BF16, ph)
            Vt = sb("Vt", [128, NT, 256], BF16, ph)
            sgT = sb("sgT", [64, 4, S], BF16, ph)
            Ktok = sb("Ktok", [128, NT, 2, 128], BF16, ph)
            Sfb = sb("Sfb", [128, NT, 2, 64], BF16, ph)
            Sbb = sb("Sbb", [128, NT, 2, 64], BF16, ph)
            KV = sb("KV", [128, NT, 2, 64], F32, ph)
            B_KV = [Buf() for _ in range(NT)]
            Srf = sb("Srf", [128, 2, 64], F32, ph)
            Srb = sb("Srb", [128, 2, 64], F32, ph)
            t1 = [sb("t1_%d" % i, [128, 512], F32, ph) for i in range(2)]
            t2 = [sb("t2_%d" % i, [128, 512], F32, ph) for i in range(2)]
            Vs = [sb("Vs%d" % i, [128, 256], BF16, ph) for i in range(2)]
            Pm = [sb("Pm%d" % i, [128, 2, 2, 128], BF16, ph) for i in range(2)]
            Qf = [sb("Qf%d" % i, [128, 2, 128], BF16, ph) for i in range(2)]
            Qb = [sb("Qb%d" % i, [128, 2, 128], BF16, ph) for i in range(2)]
            sqyL = [sb("sqy%d" % i, [64, 512], BF16, ph) for i in range(2)]
            rs = sb("rs", [64, 512], F32, ph)
            rs2 = sb("rs2", [64, 512], F32, ph)
            ty = sb("ty", [64, 512], F32, ph)
            S4 = [ps("rS4_%d" % i, [128, 512], F32, ph) for i in range(4)]
            pp = S4[0:2]
            yps = ps("yps", [64, 4, 128], F32, ph)
            ssb = ps("ssb", [64, 512], F32, ph)
            G1 = ps("rG1", [128, 512], F32, ph)
            kvps = [ps("kvps0", [128, 2, 128], F32, ph), G1[:, 0:256].rearrange("p (a b) -> p a b", a=2)]
            ypsL = [yps, G1[0:64, :].rearrange("p (a b) -> p a b", a=4)]
            tpK = kvps[0][:, 0, :].bitcast(BF16).rearrange("p (c t) -> p c t", c=2)
            B_crel, B_dec, B_lg, B_tab, B_rope = Buf(), Buf(), Buf(), Buf(), Buf()
            B_w2x, B_wrv, B_wrg, B_rqT, B_rkT, B_sgT = Buf(), Buf(), Buf(), Buf(), Buf(), Buf()
            B_Vt = [Buf() for _ in range(NT)]
            B_Ktok = [Buf() for _ in range(NT)]
            B_Sfb = [Buf() for _ in range(NT)]
            B_Sbb = [Buf() for _ in range(NT)]
            B_Srf, B_Srb = Buf(), Buf()
            B_t1 = [Buf() for _ in range(2)]
            B_t2 = [Buf() for _ in range(2)]
            B_Vs = [Buf() for _ in range(2)]
            B_Pm = [Buf() for _ in range(2)]
            B_Qf = [Buf() for _ in range(2)]
            B_Qb = [Buf() for _ in range(2)]
            B_rs, B_ty, B_rs2 = Buf(), Buf(), Buf()
            B_sqyL = [Buf(), Buf()]
            B_S4 = [Buf() for _ in range(4)]
            B_pp = B_S4[0:2]
            B_yps, B_ssb = Buf(), Buf()
            B_kvps = [Buf(), Buf()]
            B_tpK = B_kvps[0]
            B_ypsL = [B_yps, B_kvps[1]]

            s_r = tr.dma_sem("r0")
            tr.dma("sp", s_r, lambda e: [
                e.dma_start(out=crel[:], in_=c_relm_d[:, :]),
                e.dma_start(out=dec[:], in_=rdec_d.partition_broadcast(128)),
                e.dma_start(out=cosT[:], in_=c_cos_d[:, :]),
                e.dma_start(out=sinT[:], in_=c_sin_d[:, :]),
            ], writes=[B_crel, B_dec, B_rope], n=4)
            tr.op("act", lambda e: e.activation(out=lg[:], in_=dec[:], func=AF.Exp), reads=[B_dec], writes=[B_lg])
            tr.op("dve", lambda e: e.tensor_scalar(lg[:], lg[:], -1.0, None, ALU.mult), reads=[B_lg], writes=[B_lg])
            tr.op("dve", lambda e: e.tensor_copy(lgPf[0:64, :], lg[0:64, 0:8:2]), reads=[B_lg], writes=[B_tab])
            tr.op("dve", lambda e: e.tensor_copy(lgPf[64:128, :], lg[64:128, 1:8:2]), reads=[B_lg], writes=[B_tab])
            tr.op("dve", lambda e: e.tensor_copy(lgPb[0:64, :], lg[0:64, 8:16:2]), reads=[B_lg], writes=[B_tab])
            tr.op("dve", lambda e: e.tensor_copy(lgPb[64:128, :], lg[64:128, 9:16:2]), reads=[B_lg], writes=[B_tab])
            for gp in range(4):
                tr.op("act", lambda e, gp=gp: e.activation(out=QDF[:, gp, :], in_=crel[:, 0:128], func=AF.Exp, scale=lgPf[:, gp:gp + 1]),
                      reads=[B_tab, B_crel], writes=[B_tab])
                tr.op("act", lambda e, gp=gp: e.activation(out=QDB[:, gp, :], in_=crel[:, 128:256], func=AF.Exp, scale=lgPb[:, gp:gp + 1]),
                      reads=[B_tab, B_crel], writes=[B_tab])
            for h in range(8):
                tr.op("act", lambda e, h=h: e.activation(out=DT[:, h, :], in_=crel[:, 256:384], func=AF.Exp, scale=lg[:, h:h + 1]),
                      reads=[B_lg, B_crel], writes=[B_tab])
                tr.op("act", lambda e, h=h: e.activation(out=DT2[:, h, :], in_=crel[:, 384:512], func=AF.Exp, scale=lg[:, 8 + h:9 + h]),
                      reads=[B_lg, B_crel], writes=[B_tab])
            tr.op("dve", lambda e: e.tensor_tensor(DT[:], DT[:], DT2[:], ALU.add), reads=[B_tab], writes=[B_tab])
            tr.op("act", lambda e: e.activation(out=KDF[:], in_=lg[:, 0:8], func=AF.Exp, scale=crel[:, 512:513]), reads=[B_lg, B_crel], writes=[B_tab])
            tr.op("act", lambda e: e.activation(out=KDB[:], in_=lg[:, 8:16], func=AF.Exp, scale=crel[:, 513:514]), reads=[B_lg, B_crel], writes=[B_tab])
            tr.op("act", lambda e: e.activation(out=GCf[:], in_=lgPf[:], func=AF.Exp, scale=128.0), reads=[B_tab], writes=[B_tab])
            tr.op("act", lambda e: e.activation(out=GCb[:], in_=lgPb[:], func=AF.Exp, scale=128.0), reads=[B_tab], writes=[B_tab])

            s_rw = [tr.dma_sem() for _ in range(3)]
            ppc = [0]
            if "rtab" in dbg:
                dump("DT", DT[:], [128, 8, 128], F32)
                dump("QDF", QDF[:], [128, 4, 128], F32)
                dump("KDF", KDF[:], [128, 8], F32)
                dump("GCf", GCf[:], [128, 4], F32)
            CUT = int(stop_after[1]) if (stop_after and stop_after[0] == "D" and len(stop_after) == 2) else 9
            for hg in (range(2) if stop_after != "D0" else []):
                for (c_plain, dstT, Bd, kscale) in [(1536, rqT, B_rqT, 1.0), (2048, rkT, B_rkT, 0.125)]:
                    srcA = win_d[:, c_plain + hg * 256:c_plain + hg * 256 + 256].rearrange("(k p) c -> p k c", p=128)
                    tr.dma("pool", s_rw[0], lambda e, srcA=srcA: [e.dma_start(out=w2x[:], in_=srcA)], writes=[B_w2x])
                    for pr in range(2):
                        for tc in range(4):
                            cs = slice(tc * 512, (tc + 1) * 512)
                            tl = ppc[0] % 2
                            ppc[0] += 1

                            def mmq(e, pr=pr, tc=tc, tl=tl):
                                last = None
                                for k in range(8):
                                    last = e.matmul(pp[tl][:, :], w2x[:, k, pr * 128:(pr + 1) * 128],
                                                    xnT[:, k, PAD + tc * 512:PAD + (tc + 1) * 512], start=(k == 0), stop=(k == 7))
                                return last
                            tr.op("pe", mmq, reads=[B_w2x, B_xnT], writes=[B_pp[tl]])
                            for (dst0, src0) in [(0, 32), (32, 0), (64, 96), (96, 64)]:
                                tr.op("act", lambda e, tl=tl, dst0=dst0, src0=src0: e.activation(
                                    out=qsw[tl][dst0:dst0 + 32, :], in_=pp[tl][src0:src0 + 32, :], func=AF.Copy),
                                    reads=[B_pp[tl]], writes=[B_qsw[tl]])
                            tr.op("dve", lambda e, cs=cs, tl=tl, kscale=kscale: e.scalar_tensor_tensor(t1[tl][:], pp[tl][:, :], kscale, cosT[:, cs], ALU.mult, ALU.mult),
                                  reads=[B_pp[tl], B_rope, B_qsw[tl]], writes=[B_t1[tl]])
                            tr.op("dve", lambda e, cs=cs, tl=tl, kscale=kscale: e.scalar_tensor_tensor(t2[tl][:], qsw[tl][:, :], kscale, sinT[:, cs], ALU.mult, ALU.mult),
                                  reads=[B_qsw[tl], B_rope], writes=[B_t2[tl]])
                            tr.op("pool", lambda e, cs=cs, tl=tl, dstT=dstT, pr=pr: e.tensor_tensor(dstT[:, pr, cs], t1[tl][:], t2[tl][:], ALU.add),
                                  reads=[B_t1[tl], B_t2[tl]], writes=[Bd])
                if CUT < 2:
                    continue
                srcV = win_d[:, 2560 + hg * 256:2560 + hg * 256 + 256].rearrange("(k p) c -> p k c", p=128)
                srcG = win_d[:, 3072 + hg * 256:3072 + hg * 256 + 256].rearrange("(k p) c -> p k c", p=128)
                tr.dma("pool", s_rw[1], lambda e, srcV=srcV: [e.dma_start(out=wrv[:], in_=srcV)], writes=[B_wrv])
                tr.dma("pool", s_rw[2], lambda e, srcG=srcG: [e.dma_start(out=wrg[:], in_=srcG)], writes=[B_wrg])
                for n in range(NT):
                    sl = ppc[0] % 2
                    ppc[0] += 1

                    def mmv(e, n=n, sl=sl):
                        last = None
                        for k in range(8):
                            last = e.matmul(pp[sl][:, 0:256], xnT[:, k, PAD + n * 128:PAD + (n + 1) * 128], wrv[:, k, :],
                                            start=(k == 0), stop=(k == 7))
                        return last
                    tr.op("pe", mmv, reads=[B_wrv, B_xnT], writes=[B_pp[sl]])
                    tr.op("act", lambda e, n=n, sl=sl: e.activation(out=Vt[:, n, :], in_=pp[sl][:, 0:256], func=AF.Copy),
                          reads=[B_pp[sl]], writes=[B_Vt[n]])
                for hh in range(4):
                    for tc in range(4):
                        sl = ppc[0] % 2
                        ppc[0] += 1

                        def mmg(e, hh=hh, tc=tc, sl=sl):
                            last = None
                            for k in range(8):
                                last = e.matmul(pp[sl][0:64, :], wrg[:, k, hh * 64:(hh + 1) * 64],
                                                xnT[:, k, PAD + tc * 512:PAD + (tc + 1) * 512], start=(k == 0), stop=(k == 7))
                            return last
                        tr.op("pe", mmg, reads=[B_wrg, B_xnT], writes=[B_pp[sl]])
                        tr.op("act", lambda e, hh=hh, tc=tc, sl=sl: e.activation(out=sgT[0:64, hh, tc * 512:(tc + 1) * 512], in_=pp[sl][0:64, :], func=AF.Silu),
                              reads=[B_pp[sl]], writes=[B_sgT])
                if CUT < 3:
                    continue
                for n in range(NT):
                    def tpk(e, n=n):
                        last = None
                        for pr in range(2):
                            last = e.transpose(tpK[:, pr, :], rkT[:, pr, n * 128:(n + 1) * 128], ident_b[:])
                        return last
                    tr.op("pe", tpk, reads=[B_rkT, B_const], writes=[B_tpK])
                    tr.op("dve", lambda e, n=n: e.tensor_copy(Ktok[:, n, :, :], tpK[:, :, :]), reads=[B_tpK], writes=[B_Ktok[n]])
                if CUT < 4:
                    continue
                dirs = [(list(range(NT)), KDF, GCf, Srf, B_Srf, Sfb, B_Sfb), (list(range(NT - 1, -1, -1)), KDB, GCb, Srb, B_Srb, Sbb, B_Sbb)]
                for di, (order, KD, GC, Srun, B_Srun, Sbf, B_Sbf) in enumerate(dirs):
                    for n in order:
                        vl = ppc[0] % 2
                        ppc[0] += 1
                        tr.op("pool", lambda e, n=n, vl=vl, KD=KD, hg=hg: e.tensor_tensor(
                            Vs[vl][:, :].rearrange("p (h d) -> p h d", h=4), Vt[:, n, :].rearrange("p (h d) -> p h d", h=4),
                            KD[:, 4 * hg:4 * hg + 4].unsqueeze(2).broadcast_to([128, 4, 64]), ALU.mult),
                            reads=[B_Vt[n], B_tab], writes=[B_Vs[vl]])

                        def mmkv(e, n=n, vl=vl):
                            last = None
                            for pr in range(2):
                                last = e.matmul(kvps[vl][:, pr, :], Ktok[:, n, pr, :], Vs[vl][:, pr * 128:(pr + 1) * 128], start=True, stop=True)
                            return last
                        tr.op("pe", mmkv, reads=[B_Ktok[n], B_Vs[vl]], writes=[B_kvps[vl]])
                        tr.op("act", lambda e, n=n, vl=vl: e.activation(out=KV[0:64, n, :, :], in_=kvps[vl][0:64, :, 0:64], func=AF.Copy),
                              reads=[B_kvps[vl]], writes=[B_KV[n]])
                        tr.op("dve", lambda e, n=n, vl=vl: e.tensor_copy(KV[64:128, n, :, :], kvps[vl][64:128, :, 64:128]),
                              reads=[B_kvps[vl]], writes=[B_KV[n]])
                    tr.op("pool", lambda e, Srun=Srun: e.memset(Srun[:], 0.0), writes=[B_Srun])
                    for n in order:
                        tr.op("act", lambda e, n=n, Srun=Srun, Sbf=Sbf: e.activation(out=Sbf[:, n, :, :], in_=Srun[:, :, :], func=AF.Copy),
                              reads=[B_Srun], writes=[B_Sbf[n]])
                        for pr in range(2):
                            tr.op("dve", lambda e, n=n, pr=pr, Srun=Srun, GC=GC, hg=hg: e.scalar_tensor_tensor(
                                Srun[:, pr, :], Srun[:, pr, :], GC[:, 2 * hg + pr:2 * hg + pr + 1], KV[:, n, pr, :], ALU.mult, ALU.add),
                                reads=[B_KV[n], B_Srun, B_tab], writes=[B_Srun])
                if CUT < 5:
                    continue
                def stage_a(n, hg=hg):
                    cs = slice(n * 128, (n + 1) * 128)
                    sl = n % 2
                    tr.op("pool", lambda e: e.tensor_tensor(Qf[sl][:], rqT[:, :, cs], QDF[:, 2 * hg:2 * hg + 2, :], ALU.mult),
                          reads=[B_rqT, B_tab], writes=[B_Qf[sl]])
                    tr.op("pool", lambda e: e.tensor_tensor(Qb[sl][:], rqT[:, :, cs], QDB[:, 2 * hg:2 * hg + 2, :], ALU.mult),
                          reads=[B_rqT, B_tab], writes=[B_Qb[sl]])

                    def mms(e):
                        last = None
                        for hh in range(4):
                            pr, par = hh // 2, hh % 2
                            base = 64 * par
                            last = e.matmul(S4[2 * sl + par][:, pr * 128:(pr + 1) * 128], rkT[base:base + 64, pr, cs],
                                            rqT[base:base + 64, pr, cs], start=True, stop=True)
                        return last
                    tr.op("pe", mms, reads=[B_rkT, B_rqT], writes=[B_S4[2 * sl], B_S4[2 * sl + 1]])
                    for par in range(2):
                        tr.op("dve", lambda e, par=par: e.tensor_tensor(
                            Pm[sl][:, par, :, :], S4[2 * sl + par][:, 0:256].rearrange("p (j c) -> p j c", j=2),
                            DT[:, 4 * hg + par:4 * hg + 4:2, :], ALU.mult),
                            reads=[B_S4[2 * sl + par], B_tab], writes=[B_Pm[sl]])

                def stage_b(n, hg=hg):
                    sl = n % 2
                    yb = ypsL[sl]

                    def mmy(e):
                        last = None
                        for hh in range(4):
                            pr, base = hh // 2, 64 * (hh % 2)
                            e.matmul(yb[0:64, hh, :], Vt[:, n, hh * 64:(hh + 1) * 64], Pm[sl][:, hh % 2, hh // 2, :], start=True, stop=False)
                            e.matmul(yb[0:64, hh, :], Sfb[base:base + 64, n, pr, :], Qf[sl][base:base + 64, pr, :], start=False, stop=False)
                            last = e.matmul(yb[0:64, hh, :], Sbb[base:base + 64, n, pr, :], Qb[sl][base:base + 64, pr, :], start=False, stop=True)
                        return last
                    tr.op("pe", mmy, reads=[B_Vt[n], B_Pm[sl], B_Sfb[n], B_Sbb[n], B_Qf[sl], B_Qb[sl]], writes=[B_ypsL[sl]])
                    tr.op("act", lambda e: e.activation(out=sqyL[sl][:], in_=yb[0:64, :, :].rearrange("p a b -> p (a b)"), func=AF.Square),
                          reads=[B_ypsL[sl]], writes=[B_sqyL[sl]])

                def stage_c(n, hg=hg):
                    cs = slice(n * 128, (n + 1) * 128)
                    sl = n % 2
                    yb = ypsL[sl]
                    tr.op("pe", lambda e: e.matmul(ssb[:, :], ones_b[0:64, 0:64], sqyL[sl][:, :], start=True, stop=True),
                          reads=[B_sqyL[sl], B_const], writes=[B_ssb])
                    tr.op("act", lambda e: e.activation(out=rs[:], in_=ssb[:, :], func=AF.Ln, scale=1.0 / 64, bias=EPS),
                          reads=[B_ssb], writes=[B_rs])
                    tr.op("act", lambda e: e.activation(out=rs2[:], in_=rs[:], func=AF.Exp, scale=-0.5), reads=[B_rs], writes=[B_rs2])
                    tr.op("dve", lambda e: e.tensor_tensor(ty[:], yb[0:64, :, :].rearrange("p a b -> p (a b)"), rs2[:], ALU.mult),
                          reads=[B_ypsL[sl], B_rs2], writes=[B_ty])
                    tr.op("dve", lambda e: e.tensor_tensor(mixT[64:128, 4 * hg:4 * hg + 4, cs], ty[:, :].rearrange("p (a b) -> p a b", a=4),
                                                           sgT[0:64, :, cs], ALU.mult),
                          reads=[B_ty, B_sgT], writes=[B_retT])
                if CUT >= 9:
                    stage_a(0)
                    stage_a(1)
                    stage_b(0)
                    for n in range(NT):
                        if n + 2 < NT:
                            stage_a(n + 2)
                        if n + 1 < NT:
                            stage_b(n + 1)
                        stage_c(n)
            if "retT" in dbg:
                dump("retT", mixT[64:128, :, :], [64, 8, S], BF16)
            tr.barrier()
            tr.emit()
        if stop_after and stop_after[0] == "D":
            tr.barrier(); tr.emit()
            return nc, dbg_outs

        CAP = MOE_CAP
        NSL = 16 * CAP
        BIGIDX = 1.0e6
        xe_d = nc.dram_tensor("xe_scratch", [NSL + 128, D], BF16, kind="Internal").ap()
        y_d = nc.dram_tensor("y_scratch", [NSL + 128, D], F32, kind="Internal").ap()
        hres = sb("hres", [128, NT, D], F32)
        gates = sb("gates", [128, NT, 16], F32)
        OH1 = sb("OH1", [128, NT, 16], F32)
        OH2 = sb("OH2", [128, NT, 16], F32)
        A12 = sb("A12", [128, 2, NT], F32)
        AV = sb("AV", [128, 2, NT], F32)
        DI = sb("DI", [128, 2, NT], mybir.dt.int32)
        flag = sb("flag", [1, 2], mybir.dt.int32)
        B_hres = [Buf() for _ in range(NT)]
        B_gates = Buf()
        B_OH, B_A, B_AV, B_DI, B_flag, B_xe, B_yd = Buf(), Buf(), Buf(), Buf(), Buf(), Buf(), Buf()
        hnT = xnT
        BIGM = 1.0e4
        with ExitStack() as ph:
            hn_tok = [sb("hn_tok%d" % i, [128, D], BF16, ph) for i in range(2)]
            Mk2 = [sb("Mk2_%d" % i, [128, 16], F32, ph) for i in range(2)]
            Pos2 = [sb("Pos2_%d" % i, [128, 16], F32, ph) for i in range(2)]
            V2 = [sb("V2_%d" % i, [128, 16], F32, ph) for i in range(2)]
            Q2 = [sb("Q2_%d" % i, [128, 16], F32, ph) for i in range(2)]
            tmp2 = [sb("tmp2_%d" % i, [128, 16], F32, ph) for i in range(2)]
            Run = sb("Run", [128, 16], F32, ph)
            iotaC = sb("iotaC", [128, 16], F32, ph)
            B_rt = [Buf(), Buf()]
            B_run = Buf()
            B_DIt = [Buf() for _ in range(NT)]
            s_sc = tr.dma_sem("scat")
            wo = sb("wo", [128, 8, D], BF16, ph)
            wr32 = sb("wr32", [128, 8, 20], F32, ph)
            brB = sb("brB", [128, 20], F32, ph)
            nffnB = sb("nffnB", [128, D], F32, ph)
            ustr = sb("ustr", [128, 128], F32, ph)
            iota16 = sb("iota16", [128, 17], F32, ph)
            zt = sb("zt", [128, D], BF16, ph)
            xt = [sb("ext%d" % i, [128, D], F32, ph) for i in range(2)]
            hsL = [sb("hs%d" % i, [128, D], F32, ph) for i in range(2)]
            junk = sb("junkE", [128, D], BF16, ph)
            hn32L = [sb("hn32_%d" % i, [128, 8, 128], F32, ph) for i in range(2)]
            sm = [sb("smallE%d" % i, [128, 24], F32, ph) for i in range(2)]
            Lg = [sb("Lg%d" % i, [128, 20], F32, ph) for i in range(2)]
            elm = [sb("elm%d" % i, [128, 16], F32, ph) for i in range(2)]
            elm2 = [sb("elm2%d" % i, [128, 16], F32, ph) for i in range(2)]
            ohg = [sb("ohg%d" % i, [128, 4], F32, ph) for i in range(2)]
            eg = [sb("eg%d" % i, [128, 4], F32, ph) for i in range(2)]
            Mk = sb("Mk", [128, NT, 16], F32, ph)
            Pos = sb("Pos", [128, NT, 16], F32, ph)
            Tot = sb("Tot", [128, NT, 16], F32, ph)
            Off = sb("Off", [128, NT, 16], F32, ph)
            tmpR = sb("tmpR", [128, NT, 16], F32, ph)
            SL = sb("SL", [128, 2, NT], F32, ph)
            EI = sb("EI", [128, 2, NT], F32, ph)
            VL = sb("VL", [128, 2, NT], F32, ph)
            DF = sb("DF", [128, 2, NT], F32, ph)
            ovs = sb("ovs", [128, 4], F32, ph)
            oA = [ps("oA%d" % i, [128, 512], F32, ph) for i in range(2)]
            oR = [ps("oR%d" % i, [128, 512], F32, ph) for i in range(2)]
            tp32 = ps("tp32", [128, 8, 128], F32, ph)
            bankX = [ps("bankX%d" % i, [128, 512], F32, ph) for i in range(2)]
            lgpsL = [bankX[i][:, 0:20] for i in range(2)]
            B_wo, B_wr, B_junk = Buf(), Buf(), Buf()
            B_hsL = [Buf(), Buf()]
            B_hn32L = [Buf(), Buf()]
            B_lgpsL = [Buf(), Buf()]
            B_sm = [Buf() for _ in range(2)]
            B_g = [Buf() for _ in range(2)]
            B_hnt = [Buf() for _ in range(2)]
            B_xt = [Buf() for _ in range(2)]
            B_oA = [Buf() for _ in range(2)]
            B_oR = [Buf() for _ in range(2)]
            B_tp32, B_r = Buf(), Buf()
            B_zt = Buf()
            s_e = tr.dma_sem("e0")
            s_ex = [tr.dma_sem() for _ in range(2)]
            tr.dma("pool", s_e, lambda e: [
                e.dma_start(out=wo[0:64, :, :], in_=wout_d[0:512, :].rearrange("(h p) c -> p h c", p=64)),
                e.dma_start(out=wo[64:128, :, :], in_=wout_d[512:1024, :].rearrange("(h p) c -> p h c", p=64))], writes=[B_wo], n=2)
            s_e2 = tr.dma_sem("e1")
            tr.dma("sp", s_e2, lambda e: [
                e.dma_start(out=wr32[:], in_=wr_d[:, :].rearrange("(k p) c -> p k c", p=128)),
                e.dma_start(out=brB[:], in_=br_d.partition_broadcast(128)),
                e.dma_start(out=nffnB[:], in_=nffr_d.partition_broadcast(128)),
                e.dma_start(out=ustr[:], in_=c_ustr_d[:, :]),
                e.dma_start(out=iota16[:], in_=c_iota_d[:, :])], writes=[B_wr], n=5)
            tr.op("pool", lambda e: e.memset(Run[:], 0.0), writes=[B_run])
            tr.op("dve", lambda e: e.tensor_scalar(iotaC[:], iota16[:, 0:16], float(CAP), None, ALU.mult), reads=[B_wr], writes=[B_run])
            tr.op("dve", lambda e: e.tensor_scalar(ovs[:, 3:4], iota16[:, 16:17], float(NSL), None, ALU.add), reads=[B_wr], writes=[B_run])
            tr.op("pool", lambda e: e.memset(zt[:], 0.0), writes=[B_zt])
            s_z = tr.dma_sem("z0")
            nz = NSL // 128 + 1
            def tile_stages(i):
                cs = slice(i * 128, (i + 1) * 128)
                sl = i % 2
                SM = sm[sl]
                hs = hsL[sl]
                hn32 = hn32L[sl]
                lgps = lgpsL[sl]
                B_hs, B_hn32, B_lgps = B_hsL[sl], B_hn32L[sl], B_lgpsL[sl]
                tr.begin_capture()
                tr.dma("sp", s_ex[sl], lambda e, i=i, sl=sl: [e.dma_start(out=xt[sl][:], in_=x_d[i * 128:(i + 1) * 128, :])], writes=[B_xt[sl]])
                for half in range(2):
                    def mmo(e, cs=cs, half=half):
                        last = None
                        for h in range(8):
                            e.matmul(oA[half][:, :], mixT[0:64, h, cs], wo[0:64, h, half * 512:(half + 1) * 512], start=(h == 0), stop=(h == 7))
                            last = e.matmul(oR[half][:, :], mixT[64:128, h, cs], wo[64:128, h, half * 512:(half + 1) * 512], start=(h == 0), stop=(h == 7))
                        return last
                    tr.op("pe", mmo, reads=[B_attnT, B_retT, B_wo], writes=[B_oA[half], B_oR[half]])
                S_ = [B_sm[sl]]
                tr.op("dve", lambda e, i=i, SM=SM: e.tensor_reduce(SM[:, 0:1], ssh[:, i, :], AX.X, ALU.add), reads=[B_ssh], writes=S_)
                tr.op("act", lambda e, SM=SM: e.activation(out=SM[:, 1:2], in_=SM[:, 0:1], func=AF.Sqrt, scale=1.0 / 512, bias=EPS), reads=S_, writes=S_)
                tr.op("dve", lambda e, SM=SM: e.reciprocal(SM[:, 2:3], SM[:, 1:2]), reads=S_, writes=S_)
                for half in range(2):
                    hsl = slice(half * 512, (half + 1) * 512)
                    tr.op("dve", lambda e, i=i, half=half, hsl=hsl, sl=sl, SM=SM: e.scalar_tensor_tensor(
                        hres[:, i, hsl], oA[half][:, :], SM[:, 2:3], xt[sl][:, hsl], ALU.mult, ALU.add),
                        reads=[B_oA[half], B_sm[sl], B_xt[sl]], writes=[B_hres[i]])
                    tr.op("dve", lambda e, i=i, half=half, hsl=hsl: e.tensor_tensor(hres[:, i, hsl], hres[:, i, hsl], oR[half][:, :], ALU.add),
                          reads=[B_oR[half], B_hres[i]], writes=[B_hres[i]])
                tr.op("act", lambda e, i=i, SM=SM: e.activation(out=junk[:], in_=hres[:, i, :], func=AF.Square, accum_out=SM[:, 3:4]),
                      reads=[B_hres[i]], writes=[B_junk] + S_)
                tr.op("act", lambda e, SM=SM: e.activation(out=SM[:, 4:5], in_=SM[:, 3:4], func=AF.Sqrt, scale=1.0 / D, bias=EPS), reads=S_, writes=S_)
                tr.op("dve", lambda e, SM=SM: e.reciprocal(SM[:, 5:6], SM[:, 4:5]), reads=S_, writes=S_)
                tr.op("dve", lambda e, i=i, SM=SM: e.tensor_scalar(hs[:], hres[:, i, :], SM[:, 5:6], None, ALU.mult), reads=[B_hres[i]] + S_, writes=[B_hs])
                tr.op("pool", lambda e, sl=sl: e.tensor_tensor(hn_tok[sl][:], hs[:], nffnB[:], ALU.mult), reads=[B_hs, B_wr], writes=[B_hnt[sl]])

                st1 = tr.end_capture()
                tr.begin_capture()

                def tps32(e):
                    last = None
                    for k in range(8):
                        last = e.transpose(tp32[:, k, :], hs[:, k * 128:(k + 1) * 128], ident_f[:])
                    return last
                tr.op("pe", tps32, reads=[B_hs, B_const], writes=[B_tp32])
                tr.op("dve", lambda e: e.tensor_tensor(hn32[:], tp32[:, :, :], nffn[:, :].unsqueeze(2).broadcast_to([128, 8, 128]), ALU.mult),
                      reads=[B_tp32, B_const], writes=[B_hn32])
                tr.op("pool", lambda e, cs=cs: e.tensor_copy(hnT[:, :, PAD + cs.start:PAD + cs.stop], hn32[:]), reads=[B_hn32], writes=[B_xnT])

                def mmr(e):
                    last = None
                    for k in range(8):
                        last = e.matmul(lgps[:, :], hn32[:, k, :], wr32[:, k, :], start=(k == 0), stop=(k == 7))
                    return last
                tr.op("pe", mmr, reads=[B_hn32, B_wr], writes=[B_lgps])
                G = [B_g[sl]]
                L_, E1, E2, OG, EG = Lg[sl], elm[sl], elm2[sl], ohg[sl], eg[sl]
                o1 = lambda i=i: OH1[:, i, :]
                o2 = lambda i=i: OH2[:, i, :]
                tr.op("dve", lambda e, L_=L_: e.tensor_tensor(L_[:], lgps[:, :], brB[:], ALU.add), reads=[B_lgps, B_wr], writes=G)
                tr.op("dve", lambda e, L_=L_, SM=SM: e.tensor_reduce(SM[:, 8:9], L_[:, 0:4], AX.X, ALU.max), reads=G, writes=G)
                tr.op("dve", lambda e, SM=SM: e.tensor_scalar(SM[:, 9:10], SM[:, 8:9], -1.0, None, ALU.mult), reads=G, writes=G)
                tr.op("act", lambda e, L_=L_, SM=SM, EG=EG: e.activation(out=EG[:], in_=L_[:, 0:4], func=AF.Exp, bias=SM[:, 9:10], scale=1.0, accum_out=SM[:, 10:11]),
                      reads=G, writes=G)
                tr.op("dve", lambda e, SM=SM: e.reciprocal(SM[:, 11:12], SM[:, 10:11]), reads=G, writes=G)
                tr.op("dve", lambda e, L_=L_, SM=SM, OG=OG: e.tensor_tensor(OG[:], L_[:, 0:4], SM[:, 8:9].broadcast_to([128, 4]), ALU.is_equal), reads=G, writes=G)
                tr.op("dve", lambda e, OG=OG: e.tensor_scalar(OG[:], OG[:], BIGM, -BIGM, ALU.mult, ALU.add), reads=G, writes=G)
                tr.op("dve", lambda e, L_=L_, OG=OG, E1=E1: e.tensor_tensor(E1[:, :].rearrange("p (g j) -> p g j", g=4), L_[:, 4:20].rearrange("p (g j) -> p g j", g=4),
                                                                           OG[:, :].unsqueeze(2).broadcast_to([128, 4, 4]), ALU.add), reads=G, writes=G)
                tr.op("dve", lambda e, SM=SM, E1=E1: e.tensor_reduce(SM[:, 12:13], E1[:], AX.X, ALU.max), reads=G, writes=G)
                tr.op("dve", lambda e, SM=SM, E1=E1, o1=o1: e.tensor_tensor(o1(), E1[:], SM[:, 12:13].broadcast_to([128, 16]), ALU.is_equal), reads=G, writes=G + [B_OH])
                tr.op("dve", lambda e, E1=E1, E2=E2, o1=o1: e.scalar_tensor_tensor(E2[:], o1(), -BIGM, E1[:], ALU.mult, ALU.add), reads=G + [B_OH], writes=G)
                tr.op("dve", lambda e, SM=SM, E2=E2: e.tensor_reduce(SM[:, 13:14], E2[:], AX.X, ALU.max), reads=G, writes=G)
                tr.op("dve", lambda e, SM=SM, E2=E2, o2=o2: e.tensor_tensor(o2(), E2[:], SM[:, 13:14].broadcast_to([128, 16]), ALU.is_equal), reads=G, writes=G + [B_OH])
                tr.op("dve", lambda e, SM=SM: e.tensor_tensor(SM[:, 14:15], SM[:, 13:14], SM[:, 12:13], ALU.subtract), reads=G, writes=G)
                tr.op("act", lambda e, SM=SM: e.activation(out=SM[:, 15:16], in_=SM[:, 14:15], func=AF.Exp), reads=G, writes=G)
                tr.op("dve", lambda e, SM=SM: e.tensor_scalar(SM[:, 16:17], SM[:, 15:16], 1.0, None, ALU.add), reads=G, writes=G)
                tr.op("dve", lambda e, SM=SM: e.reciprocal(SM[:, 17:18], SM[:, 16:17]), reads=G, writes=G)
                tr.op("dve", lambda e, SM=SM, i=i: e.tensor_tensor(A12[:, 0, i:i + 1], SM[:, 17:18], SM[:, 11:12], ALU.mult), reads=G, writes=G + [B_A])
                tr.op("dve", lambda e, SM=SM, i=i: e.tensor_tensor(A12[:, 1, i:i + 1], A12[:, 0, i:i + 1], SM[:, 15:16], ALU.mult), reads=G + [B_A], writes=[B_A])
                st2 = tr.end_capture()
                tr.begin_capture()
                RR = [B_rt[sl]]
                Mki, Posi, Vi, Qi, tmpi = Mk2[sl], Pos2[sl], V2[sl], Q2[sl], tmp2[sl]
                rps = bankX[sl][:, 32:64]
                tr.op("dve", lambda e: e.tensor_tensor(Mki[:], OH1[:, i, :], OH2[:, i, :], ALU.add), reads=[B_OH] + G, writes=RR)

                def rmm(e):
                    e.matmul(rps[:, 0:16], ustr[:, :], Mki[:, :], start=True, stop=True)
                    return e.matmul(rps[:, 16:32], ones_f[:, :], Mki[:, :], start=True, stop=True)
                tr.op("pe", rmm, reads=RR + [B_wr, B_const], writes=[B_lgps])
                tr.op("dve", lambda e: e.tensor_tensor(Posi[:], rps[:, 0:16], Run[:], ALU.add), reads=[B_lgps, B_run], writes=RR)
                tr.op("dve", lambda e: e.tensor_tensor(Run[:], Run[:], rps[:, 16:32], ALU.add), reads=[B_lgps, B_run], writes=[B_run])
                tr.op("dve", lambda e: e.tensor_scalar(Vi[:], Posi[:], float(CAP), None, ALU.is_lt), reads=RR, writes=RR)
                tr.op("dve", lambda e: e.tensor_tensor(Qi[:], Posi[:], iotaC[:], ALU.add), reads=RR + [B_run], writes=RR)
                for a, OHa in enumerate([OH1, OH2]):
                    tr.op("dve", lambda e, OHa=OHa: e.tensor_tensor(tmpi[:], OHa[:, i, :], Qi[:], ALU.mult), reads=RR + [B_OH], writes=RR)
                    tr.op("dve", lambda e, a=a: e.tensor_reduce(DF[:, a, i:i + 1], tmpi[:], AX.X, ALU.add), reads=RR, writes=RR + [B_r])
                    tr.op("dve", lambda e, OHa=OHa: e.tensor_tensor(tmpi[:], OHa[:, i, :], Vi[:], ALU.mult), reads=RR + [B_OH], writes=RR)
                    tr.op("dve", lambda e, a=a: e.tensor_reduce(VL[:, a, i:i + 1], tmpi[:], AX.X, ALU.add), reads=RR, writes=RR + [B_r])
                dfi = lambda: DF[:, :, i:i + 1]
                tr.op("dve", lambda e: e.tensor_scalar(dfi(), dfi(), ovs[:, 3:4], None, ALU.subtract), reads=RR + [B_r, B_run], writes=RR + [B_r])
                tr.op("dve", lambda e: e.tensor_tensor(dfi(), dfi(), VL[:, :, i:i + 1], ALU.mult), reads=RR + [B_r], writes=RR + [B_r])
                tr.op("dve", lambda e: e.tensor_scalar(dfi(), dfi(), ovs[:, 3:4], None, ALU.add), reads=RR + [B_r], writes=RR + [B_r])
                tr.op("dve", lambda e: e.tensor_copy(DI[:, :, i:i + 1], dfi()), reads=RR + [B_r], writes=[B_DIt[i]])
                for a in range(2):
                    tr.dma("pool", s_sc, lambda e, a=a: [e.indirect_dma_start(
                        out=xe_d[:, :], out_offset=bass.IndirectOffsetOnAxis(ap=DI[:, a, i:i + 1], axis=0),
                        in_=hn_tok[sl][:, :], in_offset=None)],
                        reads=[B_DIt[i], B_hnt[sl]], writes=[B_xe])
                st3 = tr.end_capture()
                return st1, st2 + st3

            stages = [tile_stages(i) for i in range(NT)]
            tr.replay(stages[0][0])
            for i in range(NT):
                if i + 1 < NT:
                    tr.replay(stages[i + 1][0], stages[i][1])
                else:
                    tr.replay(stages[i][1])

            R = [B_r]
            with ExitStack() as ph2:
                ovp = oR[0]
                B_ovp = B_oR[0]
                fl = lambda t: t[:, :, :].rearrange("p a b -> p (a b)")
                tr.op("dve", lambda e: e.tensor_tensor(fl(AV), fl(A12), fl(VL), ALU.mult), reads=R + [B_A], writes=[B_AV])
                tr.op("dve", lambda e: e.tensor_tensor(fl(DF), fl(A12), fl(AV), ALU.subtract), reads=R + [B_A, B_AV] + B_DIt, writes=R)
                tr.op("dve", lambda e: e.tensor_tensor(gates[:], OH1[:], DF[:, 0, :].unsqueeze(2).broadcast_to([128, NT, 16]), ALU.mult),
                      reads=R + [B_OH], writes=[B_gates])
                tr.op("dve", lambda e: e.tensor_tensor(tmpR[:], OH2[:], DF[:, 1, :].unsqueeze(2).broadcast_to([128, NT, 16]), ALU.mult),
                      reads=R + [B_OH], writes=R)
                tr.op("dve", lambda e: e.tensor_tensor(gates[:], gates[:], tmpR[:], ALU.add), reads=R + [B_gates], writes=[B_gates])
                tr.op("dve", lambda e: e.tensor_scalar(fl(VL), fl(VL), -1.0, 1.0, ALU.mult, ALU.add), reads=R + [B_AV], writes=R)
                tr.op("dve", lambda e: e.tensor_reduce(ovs[:, 0:1], fl(VL), AX.X, ALU.add), reads=R, writes=R)
                tr.op("pe", lambda e: e.matmul(ovp[0:1, 0:1], ovs[:, 0:1], ones_f[:, 0:1], start=True, stop=True), reads=R + [B_const], writes=[B_ovp])
                tr.op("dve", lambda e: e.tensor_copy(ovs[0:1, 2:3], ovp[0:1, 0:1]), reads=[B_ovp], writes=R)
                if FORCE_FALLBACK:
                    tr.op("dve", lambda e: e.tensor_scalar(ovs[0:1, 2:3], ovs[0:1, 2:3], 1.0, None, ALU.add), reads=R, writes=R)
                tr.op("dve", lambda e: e.tensor_copy(flag[0:1, 0:1], ovs[0:1, 2:3]), reads=R, writes=[B_flag])
                if "route" in dbg:
                    dump("DI", DI[:], [128, 2, NT], mybir.dt.int32)
                    dump("AV", AV[:], [128, 2, NT], F32)
                    dump("gres", gates[:], [128, NT, 16], F32)
                    dump("flag", flag[:], [1, 2], mybir.dt.int32)
                if "hres" in dbg:
                    dump("hres", hres[:], [128, NT, D], F32)
                tr.barrier()
                tr.emit()
        if stop_after == "E":
            tr.barrier(); tr.emit()
            return nc, dbg_outs

        NS3 = CAP // 128
        with ExitStack() as ph:
            w1b = [sb("w1b%d" % i, [128, 8, 512], BF16, ph) for i in range(2)]
            w3b = [sb("w3b%d" % i, [128, 8, 512], BF16, ph) for i in range(2)]
            w2b = [sb("w2b%d" % i, [128, 4, D], BF16, ph) for i in range(2)]
            Xt = [mixT[:, 0:NS3, 0:D], mixT[:, NS3:2 * NS3, 0:D]]
            XT = [mixT[:, :, D:D + CAP], sb("XT1", [128, 8, CAP], BF16, ph)]
            hidT = mixT[:, 0:4, D + CAP:D + 2 * CAP]
            sil = [sb("sil%d" % i, [128, CAP], F32, ph) for i in range(2)]
            ysb = [sb("ysb%d" % i, [128, D], F32, ph) for i in range(2)]
            tpx = [ps("tpx%d" % i, [128, 8, 128], BF16, ph) for i in range(2)]
            h1p = [ps("h1p%d" % i, [128, 512], F32, ph)[:, 0:CAP] for i in range(2)]
            h3p = [ps("h3p%d" % i, [128, 512], F32, ph)[:, 0:CAP] for i in range(2)]
            yp = [ps("yp%d" % i, [128, 512], F32, ph) for i in range(2)]
            B_w = [Buf() for _ in range(2)]
            B_Xt = [Buf() for _ in range(2)]
            B_XT = [Buf() for _ in range(2)]
            B_hidT = Buf()
            B_sil = [Buf() for _ in range(2)]
            B_ysb = [Buf() for _ in range(2)]
            B_tpx = [Buf() for _ in range(2)]
            B_h1 = [Buf() for _ in range(2)]
            B_h3 = [Buf() for _ in range(2)]
            B_yp = [Buf() for _ in range(2)]
            s_m = [tr.dma_sem() for _ in range(2)]
            s_xl = [tr.dma_sem() for _ in range(2)]
            s_ys = [tr.dma_sem() for _ in range(2)]
            cnt = [0, 0, 0, 0]

            def load_w(ex):
                ws = ex % 2
                tr.dma("pool", s_m[ws], lambda e: [
                    e.dma_start(out=w1b[ws][:], in_=w1_d[ex].rearrange("(p k) f -> p k f", k=8)),
                    e.dma_start(out=w3b[ws][:], in_=w3_d[ex].rearrange("(p k) f -> p k f", k=8)),
                    e.dma_start(out=w2b[ws][:], in_=w2_d[ex].rearrange("(p k) d -> p k d", k=4))], writes=[B_w[ws]], n=3)

            def load_x(ex):
                xs_ = ex % 2
                tr.dma("sp", s_xl[xs_], lambda e: [e.dma_start(out=Xt[xs_], in_=xe_d[ex * CAP:(ex + 1) * CAP, :].rearrange("(s p) d -> p s d", p=128))],
                       reads=[B_xe], writes=[B_Xt[xs_]])

            def transposes(ex):
                xs_ = ex % 2
                for s3 in range(NS3):
                    tl = cnt[0] % 2
                    cnt[0] += 1

                    def tpf(e, s3=s3, tl=tl):
                        last = None
                        for k in range(8):
                            last = e.transpose(tpx[tl][:, k, :], Xt[xs_][:, s3, k:D:8], ident_b[:])
                        return last
                    tr.op("pe", tpf, reads=[B_Xt[xs_], B_const], writes=[B_tpx[tl]])
                    if cnt[0] % 2 == 0:
                        tr.op("act", lambda e, s3=s3, tl=tl: e.activation(out=XT[xs_][:, :, s3 * 128:(s3 + 1) * 128], in_=tpx[tl][:, :, :], func=AF.Copy),
                              reads=[B_tpx[tl]], writes=[B_XT[xs_]])
                    else:
                        tr.op("dve", lambda e, s3=s3, tl=tl: e.tensor_copy(XT[xs_][:, :, s3 * 128:(s3 + 1) * 128], tpx[tl][:, :, :]),
                              reads=[B_tpx[tl]], writes=[B_XT[xs_]])

            load_w(0)
            load_x(0)
            load_x(1)
            transposes(0)
            for ex in range(16):
                ws = ex % 2
                xs_ = ex % 2
                if ex + 1 < 16:
                    load_w(ex + 1)
                for ffc in range(4):
                    bl = cnt[1] % 2
                    sl2 = cnt[1] % 2
                    cnt[1] += 1

                    def mm13(e, ffc=ffc, wt=None, dst=None, ws=ws, xs_=xs_):
                        last = None
                        for k in range(8):
                            last = e.matmul(dst[:, :], wt[ws][:, k, ffc:512:4], XT[xs_][:, k, :], start=(k == 0), stop=(k == 7))
                        return last
                    tr.op("pe", lambda e, ffc=ffc, bl=bl, ws=ws, xs_=xs_, f=mm13: f(e, ffc, w1b, h1p[bl], ws, xs_), reads=[B_w[ws], B_XT[xs_]], writes=[B_h1[bl]])
                    tr.op("pe", lambda e, ffc=ffc, bl=bl, ws=ws, xs_=xs_, f=mm13: f(e, ffc, w3b, h3p[bl], ws, xs_), reads=[B_w[ws], B_XT[xs_]], writes=[B_h3[bl]])
                    tr.op("act", lambda e, sl2=sl2, bl=bl: e.activation(out=sil[sl2][:], in_=h1p[bl][:, :], func=AF.Silu), reads=[B_h1[bl]], writes=[B_sil[sl2]])
                    tr.op("dve", lambda e, sl2=sl2, ffc=ffc, bl=bl: e.tensor_tensor(hidT[:, ffc, :], sil[sl2][:], h3p[bl][:, :], ALU.mult),
                          reads=[B_sil[sl2], B_h3[bl]], writes=[B_hidT])
                if ex + 1 < 16:
                    transposes(ex + 1)
                if ex + 2 < 16:
                    load_x(ex + 2)
                for s3 in range(NS3):
                    yl = cnt[2] % 2
                    cnt[2] += 1
                    for half in range(2):
                        ol = cnt[3] % 2
                        cnt[3] += 1

                        def mm2(e, s3=s3, half=half, ol=ol, ws=ws):
                            last = None
                            for ffc in range(4):
                                last = e.matmul(yp[ol][:, :], hidT[:, ffc, s3 * 128:(s3 + 1) * 128],
                                                w2b[ws][:, ffc, half * 512:(half + 1) * 512], start=(ffc == 0), stop=(ffc == 3))
                            return last
                        tr.op("pe", mm2, reads=[B_hidT, B_w[ws]], writes=[B_yp[ol]])
                        if half == 0:
                            tr.op("act", lambda e, yl=yl, ol=ol: e.activation(out=ysb[yl][:, 0:512], in_=yp[ol][:, :], func=AF.Copy),
                                  reads=[B_yp[ol]], writes=[B_ysb[yl]])
                        else:
                            tr.op("dve", lambda e, yl=yl, ol=ol: e.tensor_copy(ysb[yl][:, 512:1024], yp[ol][:, :]),
                                  reads=[B_yp[ol]], writes=[B_ysb[yl]])
                    r0 = ex * CAP + s3 * 128
                    tr.dma("sp", s_ys[yl], lambda e, yl=yl, r0=r0: [e.dma_start(out=y_d[r0:r0 + 128, :], in_=ysb[yl][:])],
                           reads=[B_ysb[yl]], writes=[B_yd])
            tr.barrier()
            tr.emit()

        with ExitStack() as ph:
            tr2 = Tracker(nc, ph, prefix="fb_")
            w1b = [sb("fw1b%d" % i, [128, 8, 512], BF16, ph) for i in range(2)]
            w3b = [sb("fw3b%d" % i, [128, 8, 512], BF16, ph) for i in range(2)]
            w2b = [sb("fw2b%d" % i, [128, 4, D], BF16, ph) for i in range(2)]
            hid = [sb("fhid%d" % i, [128, 4, 512], BF16, ph) for i in range(2)]
            sil = [sb("fsil%d" % i, [128, 512], F32, ph) for i in range(2)]
            h1p = [ps("fh1p%d" % i, [128, 512], F32, ph) for i in range(2)]
            h3p = [ps("fh3p%d" % i, [128, 512], F32, ph) for i in range(2)]
            op_ = [ps("fop%d" % i, [128, 512], F32, ph) for i in range(4)]
            B_w = [Buf() for _ in range(2)]
            B_hid = [Buf() for _ in range(2)]
            B_sil = [Buf() for _ in range(2)]
            B_h1 = [Buf() for _ in range(2)]
            B_h3 = [Buf() for _ in range(2)]
            B_op = [Buf() for _ in range(4)]
            F_hres = [Buf() for _ in range(NT)]
            s_m = [tr2.dma_sem() for _ in range(2)]
            cnt = [0, 0, 0]
            for ex in range(16):
                ws = ex % 2
                tr2.dma("pool", s_m[ws], lambda e, ex=ex, ws=ws: [
                    e.dma_start(out=w1b[ws][:], in_=w1_d[ex].rearrange("(k p) f -> p k f", p=128)),
                    e.dma_start(out=w3b[ws][:], in_=w3_d[ex].rearrange("(k p) f -> p k f", p=128)),
                    e.dma_start(out=w2b[ws][:], in_=w2_d[ex].rearrange("(k p) d -> p k d", p=128))], writes=[B_w[ws]], n=3)
                for tc in range(4):
                    hsl = cnt[0] % 2
                    cnt[0] += 1
                    for ffc in range(4):
                        bl = cnt[1] % 2
                        cnt[1] += 1

                        def mm13(e, ws=ws, tc=tc, ffc=ffc, wt=None, dst=None):
                            last = None
                            for k in range(8):
                                last = e.matmul(dst[:, :], wt[ws][:, k, ffc * 128:(ffc + 1) * 128],
                                                hnT[:, k, PAD + tc * 512:PAD + (tc + 1) * 512], start=(k == 0), stop=(k == 7))
                            return last
                        tr2.op("pe", lambda e, ws=ws, tc=tc, ffc=ffc, bl=bl: mm13(e, ws, tc, ffc, w1b, h1p[bl]), reads=[B_w[ws]], writes=[B_h1[bl]])
                        tr2.op("pe", lambda e, ws=ws, tc=tc, ffc=ffc, bl=bl: mm13(e, ws, tc, ffc, w3b, h3p[bl]), reads=[B_w[ws]], writes=[B_h3[bl]])
                        tr2.op("act", lambda e, bl=bl: e.activation(out=sil[bl][:], in_=h1p[bl][:, :], func=AF.Silu), reads=[B_h1[bl]], writes=[B_sil[bl]])
                        tr2.op("dve", lambda e, bl=bl, hsl=hsl, ffc=ffc: e.tensor_tensor(hid[hsl][:, ffc, :], sil[bl][:], h3p[bl][:, :], ALU.mult),
                               reads=[B_sil[bl], B_h3[bl]], writes=[B_hid[hsl]])
                    for tt in range(4):
                        tile_i = tc * 4 + tt
                        for half in range(2):
                            ol = cnt[2] % 4
                            cnt[2] += 1

                            def mm2(e, ws=ws, hsl=hsl, tt=tt, half=half, ol=ol):
                                last = None
                                for ffc in range(4):
                                    last = e.matmul(op_[ol][:, :], hid[hsl][:, ffc, tt * 128:(tt + 1) * 128],
                                                    w2b[ws][:, ffc, half * 512:(half + 1) * 512], start=(ffc == 0), stop=(ffc == 3))
                                return last
                            tr2.op("pe", mm2, reads=[B_hid[hsl], B_w[ws]], writes=[B_op[ol]])
                            tr2.op("dve", lambda e, tile_i=tile_i, half=half, ol=ol, ex=ex: e.scalar_tensor_tensor(
                                hres[:, tile_i, half * 512:(half + 1) * 512], op_[ol][:, :], gates[:, tile_i, ex:ex + 1],
                                hres[:, tile_i, half * 512:(half + 1) * 512], ALU.mult, ALU.add),
                                reads=[B_op[ol], F_hres[tile_i]], writes=[F_hres[tile_i]])
            tr2.barrier()
            tr.barrier()
            tr.emit()
            tr2.emit(cond_ap=flag[0:1, 0:1])

        with ExitStack() as ph:
            nfB = sb("nfB", [128, D], F32, ph)
            y12 = [[sb("y%d_%d" % (a, i), [128, D], F32, ph) for a in range(2)] for i in range(2)]
            ot = [sb("ot%d" % i, [128, D], F32, ph) for i in range(2)]
            junk = sb("junkG", [128, D], BF16, ph)
            smg = sb("smG", [128, 2, 4], F32, ph)
            B_nf, B_junk = Buf(), Buf()
            B_y = [[Buf() for a in range(2)] for i in range(2)]
            B_ot = [Buf() for _ in range(2)]
            B_smg = [Buf() for _ in range(2)]
            s_g = tr.dma_sem("g0")
            s_o = [tr.dma_sem() for _ in range(2)]
            s_ga = [[tr.dma_sem() for a in range(2)] for i in range(2)]
            tr.dma("sp", s_g, lambda e: [e.dma_start(out=nfB[:], in_=nfin_d.partition_broadcast(128))], writes=[B_nf])
            for sl in range(2):
                for a in range(2):
                    tr.op("pool", lambda e, sl=sl, a=a: e.memset(y12[sl][a][:], 0.0), writes=[B_y[sl][a]])
            s_yz = tr.dma_sem("yz")
            tr.dma("sp", s_yz, lambda e: [e.dma_start(out=y_d[NSL:NSL + 128, :], in_=y12[0][0][:])], reads=[B_y[0][0]], writes=[B_yd])
            for i in range(NT):
                sl = i % 2
                for a in range(2):
                    tr.dma("pool", s_ga[sl][a], lambda e, i=i, a=a, sl=sl: [e.indirect_dma_start(
                        out=y12[sl][a][:, :], out_offset=None, in_=y_d[:, :],
                        in_offset=bass.IndirectOffsetOnAxis(ap=DI[:, a, i:i + 1], axis=0))],
                        reads=B_DIt + [B_yd], writes=[B_y[sl][a]])
                    tr.op("dve", lambda e, i=i, a=a, sl=sl: e.scalar_tensor_tensor(hres[:, i, :], y12[sl][a][:], AV[:, a, i:i + 1], hres[:, i, :], ALU.mult, ALU.add),
                          reads=[B_y[sl][a], B_AV, B_hres[i]], writes=[B_hres[i]])
                tr.op("act", lambda e, i=i, sl=sl: e.activation(out=junk[:], in_=hres[:, i, :], func=AF.Square, accum_out=smg[:, sl, 0:1]),
                      reads=[B_hres[i]], writes=[B_junk, B_smg[sl]])
                tr.op("act", lambda e, sl=sl: e.activation(out=smg[:, sl, 1:2], in_=smg[:, sl, 0:1], func=AF.Sqrt, scale=1.0 / D, bias=EPS),
                      reads=[B_smg[sl]], writes=[B_smg[sl]])
                tr.op("dve", lambda e, sl=sl: e.reciprocal(smg[:, sl, 2:3], smg[:, sl, 1:2]), reads=[B_smg[sl]], writes=[B_smg[sl]])
                tr.op("dve", lambda e, i=i, sl=sl: e.scalar_tensor_tensor(ot[sl][:], hres[:, i, :], smg[:, sl, 2:3], nfB[:], ALU.mult, ALU.mult),
                      reads=[B_hres[i], B_smg[sl], B_nf], writes=[B_ot[sl]])
                tr.dma("sp", s_o[sl], lambda e, i=i, sl=sl: [e.dma_start(out=out_d[i * 128:(i + 1) * 128, :], in_=ot[sl][:])], reads=[B_ot[sl]])
            tr.barrier()
            tr.emit()

        tr.barrier()
        tr.emit()
    return nc, dbg_outs


def make_inputs(inputs):
    w_in = np.asarray(inputs["w_in"][0], np.float32)
    a = 512

    def swap_halves(w):
        w4 = w.reshape(D, 8, 2, 32)
        return np.ascontiguousarray(w4[:, :, ::-1, :]).reshape(D, 512)
    rq = w_in[:, 3 * a:3 * a + 512]
    rk = w_in[:, 3 * a + 512:3 * a + 1024]
    w_in_ext = np.ascontiguousarray(np.concatenate([w_in, swap_halves(rq), swap_halves(rk)], axis=1))
    cosT, sinT = _const_rope()
    shared = {
        "w_in": w_in_ext,
        "w_out": np.ascontiguousarray(inputs["w_out"][0], np.float32),
        "norm_mix_t": np.ascontiguousarray(np.asarray(inputs["norm_mix"][0], np.float32).reshape(8, 128).T),
        "norm_ffn_t": np.ascontiguousarray(np.asarray(inputs["norm_ffn"][0], np.float32).reshape(8, 128).T),
        "norm_final": np.ascontiguousarray(np.asarray(inputs["norm_final"], np.float32).reshape(1, D)),
        "attn_gain_t": np.ascontiguousarray(np.asarray(inputs["attn_out_gain"][0], np.float32).reshape(8, 64).T),
        "rel_bias": np.ascontiguousarray(inputs["rel_bias"], np.float32),
        "ret_decay": np.ascontiguousarray(np.concatenate([np.asarray(inputs["ret_decay_fwd"][0], np.float32),
                                                          np.asarray(inputs["ret_decay_bwd"][0], np.float32)]).reshape(1, 16)),
        "w_router": np.ascontiguousarray(np.concatenate([
            np.asarray(inputs["router_group_w"][0], np.float32),
            np.asarray(inputs["router_expert_w"][0], np.float32).transpose(1, 0, 2).reshape(D, 16)], axis=1)),
        "b_router": np.ascontiguousarray(np.concatenate([
            np.asarray(inputs["router_group_b"][0], np.float32).reshape(4),
            np.asarray(inputs["router_expert_b"][0], np.float32).reshape(16)]).reshape(1, 20)),
        "w1": np.ascontiguousarray(inputs["expert_w1"][0], np.float32),
        "w3": np.ascontiguousarray(inputs["expert_w3"][0], np.float32),
        "w2": np.ascontiguousarray(inputs["expert_w2"][0], np.float32),
        "c_ident": np.eye(128, dtype=np.float32),
        "c_onehot": _const_onehot(),
        "c_cos": cosT,
        "c_sin": sinT,
        "c_relm": _const_relm(),
        "norm_ffn_row": np.ascontiguousarray(np.asarray(inputs["norm_ffn"][0], np.float32).reshape(1, D)),
        "c_ustr": np.triu(np.ones((128, 128), np.float32), k=1),
        "c_iota16": np.ascontiguousarray(np.concatenate([np.broadcast_to(np.arange(16, dtype=np.float32)[None, :], (128, 16)),
                                                         np.arange(128, dtype=np.float32)[:, None]], axis=1)),
    }
    x = np.asarray(inputs["x"], np.float32)
    in_maps = []
    for b in range(8):
        m = dict(shared)
        m["x"] = np.ascontiguousarray(x[b])
        in_maps.append(m)
    return in_maps


def kernel(**inputs):
    nc, _ = build_program()
    in_maps = make_inputs(inputs)
    res = run_bass_kernel_spmd(nc, in_maps, core_ids=list(range(8)))
    return np.stack([np.asarray(r["out"], np.float32) for r in res.results], axis=0)
```

```python
import math
from contextlib import ExitStack

import numpy as np
import concourse.bass as bass
import concourse.mybir as mybir
from concourse.bass_utils import run_bass_kernel_spmd

F32, BF16 = mybir.dt.float32, mybir.dt.bfloat16
AF = mybir.ActivationFunctionType
ALU = mybir.AluOpType
AX = mybir.AxisListType

S = 2048
D = 1024
NT = S // 128
PAD = 256
SP_ = S + 2 * PAD
EPS = 1e-6
NEG = -30000.0
PROJ_W = 3584
N_BUCKETS = 32
REL_MAX_DIST = 1024
ROPE_BASE = 10000.0

SAME_SYNC = True
MOE_CAP = 384
FORCE_FALLBACK = False


class Buf:
    __slots__ = ("name", "w", "r")

    def __init__(self, name=""):
        self.name = name
        self.w = None
        self.r = {}


class Eng:
    def __init__(self, name, sem):
        self.name = name
        self.sem = sem
        self.count = 0
        self.waited = {}
        self.queue = []


class Tracker:
    def __init__(self, nc, es, prefix=""):
        self.nc = nc
        self.es = es
        self.prefix = prefix
        self.sems = {}
        self.engs = {}
        for name in ("pe", "act", "dve", "pool", "sp"):
            sem = es.enter_context(nc.semaphore("sem_" + prefix + name))
            self.sems[name] = sem
            self.engs[name] = Eng(name, sem)
        self.dma_counts = {}
        self.ndma = 0
        self.capture = None

    def dma_sem(self, name=None):
        name = name or ("dma%d" % self.ndma)
        self.ndma += 1
        sem = self.es.enter_context(self.nc.semaphore("sem_" + self.prefix + name))
        self.sems[name] = sem
        self.dma_counts[name] = 0
        return name

    def _deps(self, eng, reads, writes):
        deps = {}

        def add(k, v):
            if v > deps.get(k, 0):
                deps[k] = v
        for b in reads:
            if b.w is not None:
                add(*b.w)
        for b in writes:
            if b.w is not None:
                add(*b.w)
            for k, v in b.r.items():
                add(k, v)
        waits = []
        for k, v in deps.items():
            if k == eng.name and (eng.name in ("pe", "sp") or not SAME_SYNC):
                continue
            if eng.waited.get(k, 0) >= v:
                continue
            eng.waited[k] = v
            waits.append((k, v))
        return waits

    def op(self, engname, fn, reads=(), writes=()):
        if self.capture is not None:
            self.capture.append(("op", engname, fn, list(reads), list(writes)))
            return None
        eng = self.engs[engname]
        waits = self._deps(eng, reads, writes)
        eng.count += 1
        tok = (engname, eng.count)
        eng.queue.append((waits, fn, (engname, 1)))
        for b in reads:
            b.r[engname] = eng.count
        for b in writes:
            b.w = tok
            b.r = {}
        return tok

    def dma(self, qname, semname, fn, reads=(), writes=(), n=1):
        if self.capture is not None:
            self.capture.append(("dma", qname, semname, fn, list(reads), list(writes), n))
            return None
        eng = self.engs[qname]
        waits = self._deps(eng, reads, writes)
        self.dma_counts[semname] += 16 * n
        val = self.dma_counts[semname]
        eng.queue.append((waits, fn, (semname, 16)))
        for b in reads:
            b.r[semname] = val
        for b in writes:
            b.w = (semname, val)
            b.r = {}
        return (semname, val)

    def begin_capture(self):
        self.capture = []

    def end_capture(self):
        items, self.capture = self.capture, None
        return items

    def replay(self, *lists):
        merged = []
        for li, items in enumerate(lists):
            n = len(items)
            for k, it in enumerate(items):
                merged.append(((k + 0.5) / max(n, 1), li, k, it))
        merged.sort(key=lambda t: (t[0], t[1], t[2]))
        for _, _, _, it in merged:
            if it[0] == "op":
                self.op(it[1], it[2], it[3], it[4])
            else:
                self.dma(it[1], it[2], it[3], it[4], it[5], it[6])

    def barrier(self):
        for eng in self.engs.values():
            for k, other in self.engs.items():
                if k == eng.name:
                    continue
                if other.count > eng.waited.get(k, 0):
                    eng.waited[k] = other.count
                    eng.queue.append(([(k, other.count)], None, None))
            for k, v in self.dma_counts.items():
                if v > eng.waited.get(k, 0):
                    eng.waited[k] = v
                    eng.queue.append(([(k, v)], None, None))

    def emit(self, cond_ap=None):
        nc = self.nc
        tr = self
        with nc.Block() as block:
            def run(engname):
                def body(e):
                    if cond_ap is None:
                        inner(e)
                    else:
                        with e.register("r_cond_" + engname) as r:
                            e.reg_load(r, cond_ap)
                            with e.If_ne(r, 0):
                                inner(e)

                def inner(e):
                    eng = tr.engs[engname]
                    for waits, fn, inc in eng.queue:
                        for k, v in waits:
                            e.wait_ge(tr.sems[k], v)
                        if fn is None:
                            continue
                        res = fn(e)
                        if inc[1] == 16:
                            for ins in res:
                                ins.then_inc(tr.sems[inc[0]], 16)
                        else:
                            res.then_inc(tr.sems[inc[0]], 1)
                    eng.queue = []
                return body
            block.tensor(run("pe"))
            block.scalar(run("act"))
            block.vector(run("dve"))
            block.gpsimd(run("pool"))
            block.sync(run("sp"))


def _t5_bucket_np(rel):
    half = N_BUCKETS // 2
    max_exact = half // 2
    offset = np.where(rel > 0, half, 0)
    n = np.abs(rel)
    nf = np.maximum(n, 1).astype(np.float32)
    large = max_exact + (np.log(nf / np.float32(max_exact)) / np.float32(math.log(REL_MAX_DIST / max_exact))
                         * np.float32(half - max_exact)).astype(np.int32)
    large = np.minimum(large, half - 1)
    return offset + np.where(n < max_exact, n, large)


TYPES = [(1, -64), (1, 64), (4, -64), (4, 64), (16, 0)]
HANK_W = 255


def _const_onehot():
    oh = np.zeros((33, 5, 256), np.float32)
    for ti, (dil, off) in enumerate(TYPES):
        for u in range(HANK_W):
            rel = u - 127 + off
            if abs(rel) <= 64:
                b = int(_t5_bucket_np(np.array([rel * dil]))[0])
                oh[b, ti, u] = 1.0
            else:
                oh[32, ti, u] = NEG
        oh[32, ti, 255] = NEG
    return oh.reshape(33, 5 * 256)


def _const_rope():
    half = 32
    inv = (np.float32(ROPE_BASE) ** (-np.arange(half, dtype=np.float32) / np.float32(half))).astype(np.float32)
    pos = np.arange(S, dtype=np.float32)
    ang = (pos[:, None] * inv[None, :]).astype(np.float32)
    cos = np.cos(ang).astype(np.float32)
    sin = np.sin(ang).astype(np.float32)
    cosT = np.zeros((128, S), np.float32)
    sinT = np.zeros((128, S), np.float32)
    for p in range(128):
        d = p % 64
        j = d % 32
        cosT[p] = cos[:, j]
        sinT[p] = -sin[:, j] if d < 32 else sin[:, j]
    return cosT, sinT


def _const_relm():
    BIG = 1.0e9
    t = np.zeros((128, 514), np.float32)
    c = np.arange(128, dtype=np.float32)
    p = np.arange(128, dtype=np.float32)
    t[:, 0:128] = (c + 1.0)[None, :]
    t[:, 128:256] = (128.0 - c)[None, :]
    rel = c[None, :] - p[:, None]
    t[:, 256:384] = np.where(rel >= 0, rel, BIG)
    t[:, 384:512] = np.where(rel < 0, -rel, BIG)
    t[:, 512] = 127.0 - p
    t[:, 513] = p
    return t


def build_program(dbg=None, stop_after=None):
    dbg = dbg or []
    nc = bass.Bass("TRN2", target_bir_lowering=False)

    def din(name, shape, dt=F32):
        return nc.dram_tensor(name, list(shape), dt, kind="ExternalInput").ap()

    x_d = din("x", [S, D])
    win_d = din("w_in", [D, PROJ_W + 1024])
    wout_d = din("w_out", [D, D])
    nmix_d = din("norm_mix_t", [128, 8])
    nffn_d = din("norm_ffn_t", [128, 8])
    nfin_d = din("norm_final", [1, D])
    again_d = din("attn_gain_t", [64, 8])
    relb_d = din("rel_bias", [32, 8])
    rdec_d = din("ret_decay", [1, 16])
    wr_d = din("w_router", [D, 20])
    br_d = din("b_router", [1, 20])
    w1_d = din("w1", [16, D, 512])
    w3_d = din("w3", [16, D, 512])
    w2_d = din("w2", [16, 512, D])
    c_ident_d = din("c_ident", [128, 128])
    c_oh_d = din("c_onehot", [33, 5 * 256])
    c_cos_d = din("c_cos", [128, S])
    c_sin_d = din("c_sin", [128, S])
    c_relm_d = din("c_relm", [128, 514])
    nffr_d = din("norm_ffn_row", [1, D])
    c_ustr_d = din("c_ustr", [128, 128])
    c_iota_d = din("c_iota16", [128, 17])
    out_d = nc.dram_tensor("out", [S, D], F32, kind="ExternalOutput").ap()
    hank_d = nc.dram_tensor("hank_scratch", [8, 5 * 256], F32, kind="Internal").ap()

    dbg_outs = {}

    with ExitStack() as es:
        tr = Tracker(nc, es)

        def sb(name, shape, dt, stack=es):
            return stack.enter_context(nc.sbuf_tensor(name, list(shape), dt))

        def ps(name, shape, dt, stack=es):
            return stack.enter_context(nc.psum_tensor(name, list(shape), dt))

        ident_f = sb("ident_f", [128, 128], F32)
        ident_b = sb("ident_b", [128, 128], BF16)
        ones_f = sb("ones_f", [128, 128], F32)
        ones_b = sb("ones_b", [128, 128], BF16)
        nmix = sb("nmix", [128, 8], F32)
        nffn = sb("nffn", [128, 8], F32)
        again = sb("again", [64, 8], F32)
        xnT = sb("xnT", [128, 8, SP_], BF16)
        mixT = sb("mixT", [128, 8, S], BF16)
        attnT = mixT
        ssh = sb("ssh", [128, NT, 8], F32)

        B_const = Buf("const")
        B_xnT = Buf("xnT")
        B_attnT = Buf("attnT")
        B_retT = Buf("retT")
        B_ssh = Buf("ssh")

        def dump(name, ap, shape, dt=F32):
            o = nc.dram_tensor("dbg_" + name, list(shape), dt, kind="ExternalOutput").ap()
            dbg_outs[name] = o
            sem = tr.dma_sem()
            tr.barrier()
            tr.dma("sp", sem, lambda e: [e.dma_start(out=o, in_=ap)])

        s_c = tr.dma_sem("c0")
        tr.dma("sp", s_c, lambda e: [
            e.dma_start(out=ident_f[:], in_=c_ident_d[:, :]),
            e.dma_start(out=nmix[:], in_=nmix_d[:, :]),
            e.dma_start(out=nffn[:], in_=nffn_d[:, :]),
            e.dma_start(out=again[:], in_=again_d[:, :]),
        ], writes=[B_const], n=4)
        tr.op("pool", lambda e: e.memset(ones_f[:], 1.0), writes=[B_const])
        tr.op("pool", lambda e: e.memset(ones_b[:], 1.0), writes=[B_const])
        tr.op("dve", lambda e: e.tensor_copy(ident_b[:], ident_f[:]), reads=[B_const], writes=[B_const])
        tr.op("pool", lambda e: e.memset(xnT[:, :, 0:PAD], 0.0), writes=[B_xnT])
        tr.op("pool", lambda e: e.memset(xnT[:, :, PAD + S:SP_], 0.0), writes=[B_xnT])

        def rms_to_featmajor(stack, src_loader, gain_t, dstT, B_dst, tagp):
            pass

        with ExitStack() as ph:
            xt = [sb("xt%d" % i, [128, D], F32, ph) for i in range(2)]
            xs = [sb("xs%d" % i, [128, D], BF16, ph) for i in range(2)]
            junk = sb("junkA", [128, D], BF16, ph)
            ssq = [sb("ssq%d" % i, [128, 1], F32, ph) for i in range(2)]
            rstd = [sb("rstd%d" % i, [128, 1], F32, ph) for i in range(2)]
            tp = [ps("tpA%d" % i, [128, 8, 128], BF16, ph) for i in range(2)]
            B_xt = [Buf() for _ in range(2)]
            B_xs = [Buf() for _ in range(2)]
            B_ssq = [Buf() for _ in range(2)]
            B_rstd = [Buf() for _ in range(2)]
            B_tp = [Buf() for _ in range(2)]
            B_junk = Buf()
            s_x = [tr.dma_sem() for _ in range(2)]
            for i in range(NT):
                sl = i % 2
                tr.dma("sp", s_x[sl], lambda e, i=i, sl=sl: [e.dma_start(out=xt[sl][:], in_=x_d[i * 128:(i + 1) * 128, :])],
                       writes=[B_xt[sl]])
                tr.op("act", lambda e, sl=sl: e.activation(out=junk[:], in_=xt[sl][:], func=AF.Square, accum_out=ssq[sl][:]),
                      reads=[B_xt[sl]], writes=[B_junk, B_ssq[sl]])
                tr.op("act", lambda e, sl=sl: e.activation(out=rstd[sl][:], in_=ssq[sl][:], func=AF.Sqrt, scale=1.0 / D, bias=EPS),
                      reads=[B_ssq[sl]], writes=[B_rstd[sl]])
                tr.op("dve", lambda e, sl=sl: e.reciprocal(rstd[sl][:], rstd[sl][:]), reads=[B_rstd[sl]], writes=[B_rstd[sl]])
                tr.op("dve", lambda e, sl=sl: e.tensor_scalar(xs[sl][:], xt[sl][:], rstd[sl][:, 0:1], None, ALU.mult),
                      reads=[B_xt[sl], B_rstd[sl]], writes=[B_xs[sl]])

                def tps(e, sl=sl):
                    last = None
                    for k in range(8):
                        last = e.transpose(tp[sl][:, k, :], xs[sl][:, k * 128:(k + 1) * 128], ident_b[:])
                    return last
                tr.op("pe", tps, reads=[B_xs[sl], B_const], writes=[B_tp[sl]])
                tr.op("dve", lambda e, i=i, sl=sl: e.tensor_tensor(
                    xnT[:, :, PAD + i * 128:PAD + (i + 1) * 128], tp[sl][:, :, :],
                    nmix[:, :].unsqueeze(2).broadcast_to([128, 8, 128]), ALU.mult),
                    reads=[B_tp[sl], B_const], writes=[B_xnT])
            if "xnT" in dbg:
                dump("xnT", xnT[:, :, PAD:PAD + S], [128, 8, S], BF16)
            tr.barrier()
            tr.emit()

        if stop_after == "A":
            tr.barrier(); tr.emit()
            return nc, dbg_outs

        pa = es.enter_context(ExitStack())
        Tt = sb("Tt", [128, 8, 5, 128], BF16, pa)
        B_Tt = Buf("Tt")
        with ExitStack() as ph:
            relb = sb("relb", [33, 8], F32, ph)
            oh = sb("oh", [33, 5 * 256], F32, ph)
            Fsb = sb("Fsb", [8, 5 * 256], F32, ph)
            Tp = sb("Tp", [128, 8, 5, 128], F32, ph)
            Fps = ps("Fps", [8, 3, 512], F32, ph)
            B_relb, B_oh, B_F, B_Fps, B_hank, B_Tp = Buf(), Buf(), Buf(), Buf(), Buf(), Buf()
            s_t = tr.dma_sem("t0")
            tr.op("pool", lambda e: e.memset(relb[32:33, :], 1.0), writes=[B_relb])
            tr.dma("sp", s_t, lambda e: [e.dma_start(out=relb[0:32, :], in_=relb_d[:, :]),
                                         e.dma_start(out=oh[:], in_=c_oh_d[:, :])], writes=[B_relb, B_oh], n=2)

            def fmm(e):
                last = None
                for c in range(3):
                    w = 512 if c < 2 else 256
                    last = e.matmul(Fps[0:8, c, 0:w], relb[0:33, 0:8], oh[0:33, c * 512:c * 512 + w], start=True, stop=True)
                return last
            tr.op("pe", fmm, reads=[B_relb, B_oh], writes=[B_Fps])
            tr.op("dve", lambda e: e.tensor_copy(Fsb[:, 0:1024], Fps[0:8, 0:2, :].rearrange("p a b -> p (a b)")),
                  reads=[B_Fps], writes=[B_F])
            tr.op("dve", lambda e: e.tensor_copy(Fsb[:, 1024:1280], Fps[0:8, 2, 0:256]), reads=[B_Fps], writes=[B_F])
            s_h = tr.dma_sem("hank")
            tr.dma("sp", s_h, lambda e: [e.dma_start(out=hank_d[:, :], in_=Fsb[:])], reads=[B_F], writes=[B_hank])
            tr.dma("sp", s_h, lambda e: [e.dma_start(out=Tp[:, h, :, :], in_=bass.AP(hank_d.tensor, h * 5 * 256, [[1, 128], [256, 5], [1, 128]]))
                                         for h in range(8)], reads=[B_hank], writes=[B_Tp], n=8)
            tr.op("dve", lambda e: e.tensor_copy(Tt[:], Tp[:, :, :, ::-1]), reads=[B_Tp], writes=[B_Tt])
            if "Tt" not in dbg:
                tr.op("act", lambda e: e.activation(out=Tt[:], in_=Tt[:], func=AF.Exp), reads=[B_Tt], writes=[B_Tt])
            if "Tt" in dbg:
                dump("Tt", Tt[:], [128, 8, 5, 128], BF16)
            tr.barrier()
            tr.emit()

        with ExitStack() as ph:
            wq = sb("wq", [128, 8, 256], BF16, ph)
            wk = sb("wk", [128, 8, 256], BF16, ph)
            wv = sb("wv", [128, 8, 256], BF16, ph)
            qT = sb("qT", [128, 2, S], BF16, ph)
            kT = sb("kT", [128, 2, SP_], BF16, ph)
            Vd1 = sb("Vd1", [128, 17, 4, 65], BF16, ph)
            Vd4 = sb("Vd4", [128, 20, 4, 65], BF16, ph)
            Vd16 = sb("Vd16", [128, 16, 4, 65], BF16, ph)
            acc = sb("acc", [128, 4, S], F32, ph)
            vT = acc[:, :, :].rearrange("p a b -> p (a b)")[:, 0:SP_].bitcast(BF16).rearrange("p (c t) -> p c t", c=2)
            Pt = [sb("Pt%d" % i, [128, 2, 2, 128], BF16, ph) for i in range(3)]
            Pt2 = [sb("Pt2_%d" % i, [128, 2, 2, 128], BF16, ph) for i in range(3)]
            rd = [sb("rd%d" % i, [64, 512], F32, ph) for i in range(2)]
            a32 = [sb("a32_%d" % i, [64, 512], F32, ph) for i in range(2)]
            sq = [sb("sq%d" % i, [64, 512], BF16, ph) for i in range(2)]
            S2 = [ps("S2_%d" % i, [128, 2, 512], F32, ph) for i in range(2)]
            pp = [S2[0][:, 0, :], S2[0][:, 1, :]]
            Ops = [ps("Ops%d" % i, [65, 4, 128], F32, ph) for i in range(2)]
            tpV = [S2[1][:, i, 0:128].bitcast(BF16).rearrange("p (c t) -> p c t", c=2) for i in range(2)]
            bcps = [Ops[i][0:64, :, :].rearrange("p a b -> p (a b)") for i in range(2)]
            ssps = ps("ssps", [128, 4], F32, ph)
            B_wq, B_wk, B_wv, B_qT, B_kT = Buf(), Buf(), Buf(), Buf(), Buf()
            B_V = {"d1": [Buf() for _ in range(17)], "d4": [Buf() for _ in range(20)], "d16": [Buf() for _ in range(16)]}
            B_acc = Buf()
            B_Pt = [Buf() for _ in range(3)]
            B_Pt2 = [Buf() for _ in range(3)]
            B_bank = [[Buf(), Buf()], [Buf(), Buf()]]
            B_pp = B_bank[0]
            B_Sps = B_bank[1]
            B_Ops = [Buf() for _ in range(2)]
            B_rd = [Buf() for _ in range(2)]
            B_ssps = Buf()
            B_a32 = [Buf() for _ in range(2)]
            B_sq = [Buf() for _ in range(2)]
            s_w = [tr.dma_sem() for _ in range(3)]

            tr.op("pool", lambda e: e.memset(kT[:, :, 0:PAD], 0.0), writes=[B_kT])
            tr.op("pool", lambda e: e.memset(kT[:, :, PAD + S:SP_], 0.0), writes=[B_kT])
            allV = B_V["d1"] + B_V["d4"] + B_V["d16"]
            tr.op("pool", lambda e: e.memset(Vd1[:, :, :, 64:65], 1.0), writes=B_V["d1"])
            tr.op("pool", lambda e: e.memset(Vd4[:, :, :, 64:65], 1.0), writes=B_V["d4"])
            tr.op("pool", lambda e: e.memset(Vd16[:, :, :, 64:65], 1.0), writes=B_V["d16"])
            tr.op("pool", lambda e: e.memset(Vd1[0:64, 0, :, 64:65], 0.0), writes=[B_V["d1"][0]])
            tr.op("pool", lambda e: e.memset(Vd1[64:128, 16, :, 64:65], 0.0), writes=[B_V["d1"][16]])
            for r in range(4):
                tr.op("pool", lambda e, r=r: e.memset(Vd4[0:64, r * 5, :, 64:65], 0.0), writes=[B_V["d4"][r * 5]])
                tr.op("pool", lambda e, r=r: e.memset(Vd4[64:128, r * 5 + 4, :, 64:65], 0.0), writes=[B_V["d4"][r * 5 + 4]])

            ppc = [0]
            evc = [0]

            def evac_copy(dst_fn, src_fn, reads, writes, scale=None):
                evc[0] += 1
                if scale is not None or evc[0] % 2 == 0:
                    sc = 1.0 if scale is None else scale
                    tr.op("act", lambda e: e.activation(out=dst_fn(), in_=src_fn(), func=AF.Copy, scale=sc), reads=reads, writes=writes)
                else:
                    tr.op("dve", lambda e: e.tensor_copy(dst_fn(), src_fn()), reads=reads, writes=writes)

            def attn_epilogue(hg):
                items = [(hh, tcq) for hh in range(4) for tcq in range(4)]

                def stage_a(ii):
                    hh, tcq = items[ii]
                    cs = slice(tcq * 512, (tcq + 1) * 512)
                    al = ii % 2
                    tr.op("pe", lambda e: e.matmul(bcps[al], ones_f[64:65, 0:64], acc[64:65, hh, cs], start=True, stop=True),
                          reads=[B_acc, B_const], writes=[B_Ops[al]])
                    tr.op("act", lambda e: e.activation(out=rd[al][:, :], in_=bcps[al], func=AF.Ln), reads=[B_Ops[al]], writes=[B_rd[al]])
                    tr.op("act", lambda e: e.activation(out=rd[al][:, :], in_=rd[al][:, :], func=AF.Exp, scale=-1.0), reads=[B_rd[al]], writes=[B_rd[al]])

                def stage_b(ii):
                    hh, tcq = items[ii]
                    h = 4 * hg + hh
                    cs = slice(tcq * 512, (tcq + 1) * 512)
                    al = ii % 2
                    tr.op("dve", lambda e: e.tensor_tensor(a32[al][:, :], acc[0:64, hh, cs], rd[al][:, :], ALU.mult),
                          reads=[B_acc, B_rd[al]], writes=[B_a32[al]])
                    tr.op("act", lambda e: e.activation(out=attnT[0:64, h, cs], in_=a32[al][:, :], func=AF.Copy, scale=again[0:64, h:h + 1]),
                          reads=[B_a32[al], B_const], writes=[B_attnT])
                    tr.op("act", lambda e: e.activation(out=sq[al][:, :], in_=a32[al][:, :], func=AF.Square),
                          reads=[B_a32[al]], writes=[B_sq[al]])

                    def ssmm(e):
                        last = None
                        for tt in range(4):
                            last = e.matmul(ssps[:, tt:tt + 1], sq[al][0:64, tt * 128:(tt + 1) * 128], ones_b[0:64, 0:1],
                                            start=True, stop=True)
                        return last
                    tr.op("pe", ssmm, reads=[B_sq[al], B_const], writes=[B_ssps])
                    tr.op("dve", lambda e: e.tensor_copy(ssh[:, 4 * tcq:4 * tcq + 4, h], ssps[:, 0:4]),
                          reads=[B_ssps], writes=[B_ssh])
                stage_a(0)
                for ii in range(len(items)):
                    if ii + 1 < len(items):
                        stage_a(ii + 1)
                    stage_b(ii)

            for hg in range(2):
                for wi, (wt, Bw, c0) in enumerate([(wq, B_wq, 0), (wk, B_wk, 512), (wv, B_wv, 1024)]):
                    src = win_d[:, c0 + hg * 256:c0 + hg * 256 + 256].rearrange("(k p) c -> p k c", p=128)
                    tr.dma("pool", s_w[wi], lambda e, wt=wt, src=src: [e.dma_start(out=wt[:], in_=src)], writes=[Bw])
                for (wt, Bw, dstT, Bd, off, scale) in [(wq, B_wq, qT, B_qT, 0, 0.125), (wk, B_wk, kT, B_kT, PAD, None)]:
                    for c2 in range(2):
                        for tc in range(4):
                            sl = ppc[0] % 2
                            ppc[0] += 1

                            def mm(e, wt=wt, c2=c2, tc=tc, sl=sl):
                                last = None
                                for k in range(8):
                                    last = e.matmul(pp[sl], wt[:, k, c2 * 128:(c2 + 1) * 128],
                                                    xnT[:, k, PAD + tc * 512:PAD + (tc + 1) * 512], start=(k == 0), stop=(k == 7))
                                return last
                            tr.op("pe", mm, reads=[Bw, B_xnT], writes=[B_pp[sl]])
                            evac_copy(lambda dstT=dstT, c2=c2, tc=tc, off=off: dstT[:, c2, off + tc * 512:off + (tc + 1) * 512],
                                      lambda sl=sl: pp[sl], [B_pp[sl]], [Bd], scale=scale)
                if hg > 0:
                    attn_epilogue(hg - 1)
                tr.op("pool", lambda e: e.memset(vT[:, :, 0:PAD], 0.0), writes=[B_acc])
                tr.op("pool", lambda e: e.memset(vT[:, :, PAD + S:SP_], 0.0), writes=[B_acc])
                for c2 in range(2):
                    for tc in range(4):
                        sl = ppc[0] % 2
                        ppc[0] += 1

                        def mmvt(e, c2=c2, tc=tc, sl=sl):
                            last = None
                            for k in range(8):
                                last = e.matmul(pp[sl], wv[:, k, c2 * 128:(c2 + 1) * 128],
                                                xnT[:, k, PAD + tc * 512:PAD + (tc + 1) * 512], start=(k == 0), stop=(k == 7))
                            return last
                        tr.op("pe", mmvt, reads=[B_wv, B_xnT], writes=[B_pp[sl]])
                        evac_copy(lambda c2=c2, tc=tc: vT[:, c2, PAD + tc * 512:PAD + (tc + 1) * 512],
                                  lambda sl=sl: pp[sl], [B_pp[sl]], [B_acc])
                vt = []
                for j in range(17):
                    vt.append(("d1", j, Vd1, (PAD + 128 * j - 64, 1)))
                for r in range(4):
                    for j in range(5):
                        vt.append(("d4", r * 5 + j, Vd4, (PAD + r + 4 * (128 * j - 64), 4)))
                for r in range(16):
                    vt.append(("d16", r, Vd16, (PAD + r, 16)))
                for vi, (dn, ti, Vt, (st, step)) in enumerate(vt):
                    tl = vi % 2

                    def tpv(e, st=st, step=step, tl=tl):
                        last = None
                        for c2 in range(2):
                            last = e.transpose(tpV[tl][:, c2, :], vT[:, c2, st:st + 127 * step + 1:step], ident_b[:])
                        return last
                    tr.op("pe", tpv, reads=[B_acc, B_const], writes=[B_Sps[tl]])
                    evac_copy(lambda Vt=Vt, ti=ti: Vt[:, ti, :, 0:64],
                              lambda tl=tl: tpV[tl][:, :, :].rearrange("p c (h d) -> p (c h) d", h=2),
                              [B_Sps[tl]], [B_V[dn][ti]])

                units = []
                for i in range(16):
                    units.append(dict(q=(128 * i, 1), acc=(128 * i, 1), first=True,
                                      keys=[("d1", i, Vd1, (PAD + 128 * i - 64, 1), 0), ("d1", i + 1, Vd1, (PAD + 128 * (i + 1) - 64, 1), 1)]))
                for r in range(4):
                    for i in range(4):
                        units.append(dict(q=(r + 512 * i, 4), acc=(r + 512 * i, 4), first=False,
                                          keys=[("d4", r * 5 + i, Vd4, (PAD + r + 4 * (128 * i - 64), 4), 2),
                                                ("d4", r * 5 + i + 1, Vd4, (PAD + r + 4 * (128 * (i + 1) - 64), 4), 3)]))
                for r in range(16):
                    units.append(dict(q=(r, 16), acc=(r, 16), first=False, keys=[("d16", r, Vd16, (PAD + r, 16), 4)]))
                steps = []
                for ui, u in enumerate(units):
                    for ki, key in enumerate(u["keys"]):
                        steps.append((ui, u, ki, key))

                def emit_qk(si, hg=hg):
                    ui, u, ki, (dn, ti, Vt, (kst, kstep), ty) = steps[si]
                    sl = si % 2
                    qst, qstep = u["q"]

                    def f(e):
                        last = None
                        for hh in range(4):
                            c2, par = hh // 2, hh % 2
                            base = 64 * par
                            last = e.matmul(S2[sl][:, par, c2 * 128:(c2 + 1) * 128], kT[base:base + 64, c2, kst:kst + 127 * kstep + 1:kstep],
                                            qT[base:base + 64, c2, qst:qst + 127 * qstep + 1:qstep], start=True, stop=True)
                        return last
                    tr.op("pe", f, reads=[B_kT, B_qT], writes=B_bank[sl])
                    pl = si % 3
                    tr.op("act", lambda e: e.activation(out=Pt[pl][:].rearrange("p a j c -> p a (j c)"), in_=S2[sl][:, :, 0:256], func=AF.Exp),
                          reads=B_bank[sl], writes=[B_Pt[pl]])
                    tr.op("pool" if si % 2 == 0 else "dve", lambda e: e.tensor_tensor(
                        Pt2[pl][:], Pt[pl][:], Tt[:, 4 * hg:4 * hg + 4, ty, :].rearrange("p (j a) c -> p a j c", a=2), ALU.mult),
                          reads=[B_Pt[pl], B_Tt], writes=[B_Pt2[pl]])

                def emit_pv(si, hg=hg):
                    ui, u, ki, (dn, ti, Vt, (kst, kstep), ty) = steps[si]
                    pl = si % 3
                    ol = ui % 2
                    nk = len(u["keys"])

                    def f(e):
                        last = None
                        for hh in range(4):
                            last = e.matmul(Ops[ol][0:65, hh, :], Vt[:, ti, hh, 0:65], Pt2[pl][:, hh % 2, hh // 2, :],
                                            start=(ki == 0 and hh == 0), stop=(ki == nk - 1), skip_group_check=True)
                        return last
                    tr.op("pe", f, reads=[B_V[dn][ti], B_Pt2[pl]], writes=[B_Ops[ol]])
                    if ki == nk - 1:
                        ast, astep = u["acc"]
                        dst = lambda: acc[0:65, :, ast:ast + 127 * astep + 1:astep]
                        if u["first"]:
                            tr.op("dve", lambda e: e.tensor_copy(dst(), Ops[ol][0:65, :, :]), reads=[B_Ops[ol]], writes=[B_acc])
                        else:
                            tr.op("dve", lambda e: e.tensor_tensor(dst(), dst(), Ops[ol][0:65, :, :], ALU.add),
                                  reads=[B_Ops[ol], B_acc], writes=[B_acc])

                emit_qk(0)
                emit_qk(1)
                for si in range(len(steps)):
                    if si + 2 < len(steps):
                        emit_qk(si + 2)
                    emit_pv(si)

                if "acc" in dbg and hg == 0:
                    dump("acc", acc[0:65, :, :], [65, 4, S], F32)
                if "qk" in dbg and hg == 0:
                    dump("qT", qT[:], [128, 2, S], BF16)
                    dump("kT", kT[:], [128, 2, SP_], BF16)
                if "v1" in dbg and hg == 0:
                    dump("Vd1", Vd1[:], [128, 17, 4, 65], BF16)
                if "v4" in dbg and hg == 0:
                    dump("Vd4", Vd4[:], [128, 20, 4, 65], BF16)
                    dump("Vd16", Vd16[:], [128, 16, 4, 65], BF16)

            attn_epilogue(1)
            if "attnT" in dbg:
                dump("attnT", mixT[0:64, :, :], [64, 8, S], BF16)
                dump("ssh", ssh[:], [128, NT, 8], F32)
            tr.barrier()
            tr.emit()
        pa.close()
        if stop_after == "C":
            tr.barrier(); tr.emit()
            return nc, dbg_outs
        B_retT = Buf("retT")

        with ExitStack() as ph:
            crel = sb("crel", [128, 514], F32, ph)
            dec = sb("dec", [128, 16], F32, ph)
            lg = sb("lg", [128, 16], F32, ph)
            lgPf = sb("lgPf", [128, 4], F32, ph)
            lgPb = sb("lgPb", [128, 4], F32, ph)
            QDF = sb("QDF", [128, 4, 128], F32, ph)
            QDB = sb("QDB", [128, 4, 128], F32, ph)
            DT = sb("DT", [128, 8, 128], F32, ph)
            DT2 = sb("DT2", [128, 8, 128], F32, ph)
            KDF = sb("KDF", [128, 8], F32, ph)
            KDB = sb("KDB", [128, 8], F32, ph)
            GCf = sb("GCf", [128, 4], F32, ph)
            GCb = sb("GCb", [128, 4], F32, ph)
            cosT = sb("cosT", [128, S], F32, ph)
            sinT = sb("sinT", [128, S], F32, ph)
            w2x = sb("w2x", [128, 8, 256], BF16, ph)
            qsw = [sb("qsw%d" % i, [128, 512], F32, ph) for i in range(2)]
            B_qsw = [Buf(), Buf()]
            wrv = sb("wrv", [128, 8, 256], BF16, ph)
            wrg = sb("wrg", [128, 8, 256], BF16, ph)
            rqT = sb("rqT", [128, 2, S], BF16, ph)
            rkT = sb("rkT", [128, 2, S], BF16, ph)
            Vt = sb("Vt", [128, NT, 256], BF16, ph)
            sgT = sb("sgT", [64, 4, S], BF16, ph)
            Ktok = sb("Ktok", [128, NT, 2, 128], BF16, ph)
            Sfb = sb("Sfb", [128, NT, 2, 64], BF16, ph)
            Sbb = sb("Sbb", [128, NT, 2, 64], BF16, ph)
            KV = sb("KV", [128, NT, 2, 64], F32, ph)
            B_KV = [Buf() for _ in range(NT)]
            Srf = sb("Srf", [128, 2, 64], F32, ph)
            Srb = sb("Srb", [128, 2, 64], F32, ph)
            t1 = [sb("t1_%d" % i, [128, 512], F32, ph) for i in range(2)]
            t2 = [sb("t2_%d" % i, [128, 512], F32, ph) for i in range(2)]
            Vs = [sb("Vs%d" % i, [128, 256], BF16, ph) for i in range(2)]
            Pm = [sb("Pm%d" % i, [128, 2, 2, 128], BF16, ph) for i in range(2)]
            Qf = [sb("Qf%d" % i, [128, 2, 128], BF16, ph) for i in range(2)]
            Qb = [sb("Qb%d" % i, [128, 2, 128], BF16, ph) for i in range(2)]
            sqyL = [sb("sqy%d" % i, [64, 512], BF16, ph) for i in range(2)]
            rs = sb("rs", [64, 512], F32, ph)
            rs2 = sb("rs2", [64, 512], F32, ph)
            ty = sb("ty", [64, 512], F32, ph)
            S4 = [ps("rS4_%d" % i, [128, 512], F32, ph) for i in range(4)]
            pp = S4[0:2]
            yps = ps("yps", [64, 4, 128], F32, ph)
            ssb = ps("ssb", [64, 512], F32, ph)
            G1 = ps("rG1", [128, 512], F32, ph)
            kvps = [ps("kvps0", [128, 2, 128], F32, ph), G1[:, 0:256].rearrange("p (a b) -> p a b", a=2)]
            ypsL = [yps, G1[0:64, :].rearrange("p (a b) -> p a b", a=4)]
            tpK = kvps[0][:, 0, :].bitcast(BF16).rearrange("p (c t) -> p c t", c=2)
            B_crel, B_dec, B_lg, B_tab, B_rope = Buf(), Buf(), Buf(), Buf(), Buf()
            B_w2x, B_wrv, B_wrg, B_rqT, B_rkT, B_sgT = Buf(), Buf(), Buf(), Buf(), Buf(), Buf()
            B_Vt = [Buf() for _ in range(NT)]
            B_Ktok = [Buf() for _ in range(NT)]
            B_Sfb = [Buf() for _ in range(NT)]
            B_Sbb = [Buf() for _ in range(NT)]
            B_Srf, B_Srb = Buf(), Buf()
            B_t1 = [Buf() for _ in range(2)]
            B_t2 = [Buf() for _ in range(2)]
            B_Vs = [Buf() for _ in range(2)]
            B_Pm = [Buf() for _ in range(2)]
            B_Qf = [Buf() for _ in range(2)]
            B_Qb = [Buf() for _ in range(2)]
            B_rs, B_ty, B_rs2 = Buf(), Buf(), Buf()
            B_sqyL = [Buf(), Buf()]
            B_S4 = [Buf() for _ in range(4)]
            B_pp = B_S4[0:2]
            B_yps, B_ssb = Buf(), Buf()
            B_kvps = [Buf(), Buf()]
            B_tpK = B_kvps[0]
            B_ypsL = [B_yps, B_kvps[1]]

            s_r = tr.dma_sem("r0")
            tr.dma("sp", s_r, lambda e: [
                e.dma_start(out=crel[:], in_=c_relm_d[:, :]),
                e.dma_start(out=dec[:], in_=rdec_d.partition_broadcast(128)),
                e.dma_start(out=cosT[:], in_=c_cos_d[:, :]),
                e.dma_start(out=sinT[:], in_=c_sin_d[:, :]),
            ], writes=[B_crel, B_dec, B_rope], n=4)
            tr.op("act", lambda e: e.activation(out=lg[:], in_=dec[:], func=AF.Exp), reads=[B_dec], writes=[B_lg])
            tr.op("dve", lambda e: e.tensor_scalar(lg[:], lg[:], -1.0, None, ALU.mult), reads=[B_lg], writes=[B_lg])
            tr.op("dve", lambda e: e.tensor_copy(lgPf[0:64, :], lg[0:64, 0:8:2]), reads=[B_lg], writes=[B_tab])
            tr.op("dve", lambda e: e.tensor_copy(lgPf[64:128, :], lg[64:128, 1:8:2]), reads=[B_lg], writes=[B_tab])
            tr.op("dve", lambda e: e.tensor_copy(lgPb[0:64, :], lg[0:64, 8:16:2]), reads=[B_lg], writes=[B_tab])
            tr.op("dve", lambda e: e.tensor_copy(lgPb[64:128, :], lg[64:128, 9:16:2]), reads=[B_lg], writes=[B_tab])
            for gp in range(4):
                tr.op("act", lambda e, gp=gp: e.activation(out=QDF[:, gp, :], in_=crel[:, 0:128], func=AF.Exp, scale=lgPf[:, gp:gp + 1]),
                      reads=[B_tab, B_crel], writes=[B_tab])
                tr.op("act", lambda e, gp=gp: e.activation(out=QDB[:, gp, :], in_=crel[:, 128:256], func=AF.Exp, scale=lgPb[:, gp:gp + 1]),
                      reads=[B_tab, B_crel], writes=[B_tab])
            for h in range(8):
                tr.op("act", lambda e, h=h: e.activation(out=DT[:, h, :], in_=crel[:, 256:384], func=AF.Exp, scale=lg[:, h:h + 1]),
                      reads=[B_lg, B_crel], writes=[B_tab])
                tr.op("act", lambda e, h=h: e.activation(out=DT2[:, h, :], in_=crel[:, 384:512], func=AF.Exp, scale=lg[:, 8 + h:9 + h]),
                      reads=[B_lg, B_crel], writes=[B_tab])
            tr.op("dve", lambda e: e.tensor_tensor(DT[:], DT[:], DT2[:], ALU.add), reads=[B_tab], writes=[B_tab])
            tr.op("act", lambda e: e.activation(out=KDF[:], in_=lg[:, 0:8], func=AF.Exp, scale=crel[:, 512:513]), reads=[B_lg, B_crel], writes=[B_tab])
            tr.op("act", lambda e: e.activation(out=KDB[:], in_=lg[:, 8:16], func=AF.Exp, scale=crel[:, 513:514]), reads=[B_lg, B_crel], writes=[B_tab])
            tr.op("act", lambda e: e.activation(out=GCf[:], in_=lgPf[:], func=AF.Exp, scale=128.0), reads=[B_tab], writes=[B_tab])
            tr.op("act", lambda e: e.activation(out=GCb[:], in_=lgPb[:], func=AF.Exp, scale=128.0), reads=[B_tab], writes=[B_tab])

            s_rw = [tr.dma_sem() for _ in range(3)]
            ppc = [0]
            if "rtab" in dbg:
                dump("DT", DT[:], [128, 8, 128], F32)
                dump("QDF", QDF[:], [128, 4, 128], F32)
                dump("KDF", KDF[:], [128, 8], F32)
                dump("GCf", GCf[:], [128, 4], F32)
            CUT = int(stop_after[1]) if (stop_after and stop_after[0] == "D" and len(stop_after) == 2) else 9
            for hg in (range(2) if stop_after != "D0" else []):
                for (c_plain, dstT, Bd, kscale) in [(1536, rqT, B_rqT, 1.0), (2048, rkT, B_rkT, 0.125)]:
                    srcA = win_d[:, c_plain + hg * 256:c_plain + hg * 256 + 256].rearrange("(k p) c -> p k c", p=128)
                    tr.dma("pool", s_rw[0], lambda e, srcA=srcA: [e.dma_start(out=w2x[:], in_=srcA)], writes=[B_w2x])
                    for pr in range(2):
                        for tc in range(4):
                            cs = slice(tc * 512, (tc + 1) * 512)
                            tl = ppc[0] % 2
                            ppc[0] += 1

                            def mmq(e, pr=pr, tc=tc, tl=tl):
                                last = None
                                for k in range(8):
                                    last = e.matmul(pp[tl][:, :], w2x[:, k, pr * 128:(pr + 1) * 128],
                                                    xnT[:, k, PAD + tc * 512:PAD + (tc + 1) * 512], start=(k == 0), stop=(k == 7))
                                return last
                            tr.op("pe", mmq, reads=[B_w2x, B_xnT], writes=[B_pp[tl]])
                            for (dst0, src0) in [(0, 32), (32, 0), (64, 96), (96, 64)]:
                                tr.op("act", lambda e, tl=tl, dst0=dst0, src0=src0: e.activation(
                                    out=qsw[tl][dst0:dst0 + 32, :], in_=pp[tl][src0:src0 + 32, :], func=AF.Copy),
                                    reads=[B_pp[tl]], writes=[B_qsw[tl]])
                            tr.op("dve", lambda e, cs=cs, tl=tl, kscale=kscale: e.scalar_tensor_tensor(t1[tl][:], pp[tl][:, :], kscale, cosT[:, cs], ALU.mult, ALU.mult),
                                  reads=[B_pp[tl], B_rope, B_qsw[tl]], writes=[B_t1[tl]])
                            tr.op("dve", lambda e, cs=cs, tl=tl, kscale=kscale: e.scalar_tensor_tensor(t2[tl][:], qsw[tl][:, :], kscale, sinT[:, cs], ALU.mult, ALU.mult),
                                  reads=[B_qsw[tl], B_rope], writes=[B_t2[tl]])
                            tr.op("pool", lambda e, cs=cs, tl=tl, dstT=dstT, pr=pr: e.tensor_tensor(dstT[:, pr, cs], t1[tl][:], t2[tl][:], ALU.add),
                                  reads=[B_t1[tl], B_t2[tl]], writes=[Bd])
                if CUT < 2:
                    continue
                srcV = win_d[:, 2560 + hg * 256:2560 + hg * 256 + 256].rearrange("(k p) c -> p k c", p=128)
                srcG = win_d[:, 3072 + hg * 256:3072 + hg * 256 + 256].rearrange("(k p) c -> p k c", p=128)
                tr.dma("pool", s_rw[1], lambda e, srcV=srcV: [e.dma_start(out=wrv[:], in_=srcV)], writes=[B_wrv])
                tr.dma("pool", s_rw[2], lambda e, srcG=srcG: [e.dma_start(out=wrg[:], in_=srcG)], writes=[B_wrg])
                for n in range(NT):
                    sl = ppc[0] % 2
                    ppc[0] += 1

                    def mmv(e, n=n, sl=sl):
                        last = None
                        for k in range(8):
                            last = e.matmul(pp[sl][:, 0:256], xnT[:, k, PAD + n * 128:PAD + (n + 1) * 128], wrv[:, k, :],
                                            start=(k == 0), stop=(k == 7))
                        return last
                    tr.op("pe", mmv, reads=[B_wrv, B_xnT], writes=[B_pp[sl]])
                    tr.op("act", lambda e, n=n, sl=sl: e.activation(out=Vt[:, n, :], in_=pp[sl][:, 0:256], func=AF.Copy),
                          reads=[B_pp[sl]], writes=[B_Vt[n]])
                for hh in range(4):
                    for tc in range(4):
                        sl = ppc[0] % 2
                        ppc[0] += 1

                        def mmg(e, hh=hh, tc=tc, sl=sl):
                            last = None
                            for k in range(8):
                                last = e.matmul(pp[sl][0:64, :], wrg[:, k, hh * 64:(hh + 1) * 64],
                                                xnT[:, k, PAD + tc * 512:PAD + (tc + 1) * 512], start=(k == 0), stop=(k == 7))
                            return last
                        tr.op("pe", mmg, reads=[B_wrg, B_xnT], writes=[B_pp[sl]])
                        tr.op("act", lambda e, hh=hh, tc=tc, sl=sl: e.activation(out=sgT[0:64, hh, tc * 512:(tc + 1) * 512], in_=pp[sl][0:64, :], func=AF.Silu),
                              reads=[B_pp[sl]], writes=[B_sgT])
                if CUT < 3:
                    continue
                for n in range(NT):
                    def tpk(e, n=n):
                        last = None
                        for pr in range(2):
                            last = e.transpose(tpK[:, pr, :], rkT[:, pr, n * 128:(n + 1) * 128], ident_b[:])
                        return last
                    tr.op("pe", tpk, reads=[B_rkT, B_const], writes=[B_tpK])
                    tr.op("dve", lambda e, n=n: e.tensor_copy(Ktok[:, n, :, :], tpK[:, :, :]), reads=[B_tpK], writes=[B_Ktok[n]])
                if CUT < 4:
                    continue
                dirs = [(list(range(NT)), KDF, GCf, Srf, B_Srf, Sfb, B_Sfb), (list(range(NT - 1, -1, -1)), KDB, GCb, Srb, B_Srb, Sbb, B_Sbb)]
                for di, (order, KD, GC, Srun, B_Srun, Sbf, B_Sbf) in enumerate(dirs):
                    for n in order:
                        vl = ppc[0] % 2
                        ppc[0] += 1
                        tr.op("pool", lambda e, n=n, vl=vl, KD=KD, hg=hg: e.tensor_tensor(
                            Vs[vl][:, :].rearrange("p (h d) -> p h d", h=4), Vt[:, n, :].rearrange("p (h d) -> p h d", h=4),
                            KD[:, 4 * hg:4 * hg + 4].unsqueeze(2).broadcast_to([128, 4, 64]), ALU.mult),
                            reads=[B_Vt[n], B_tab], writes=[B_Vs[vl]])

                        def mmkv(e, n=n, vl=vl):
                            last = None
                            for pr in range(2):
                                last = e.matmul(kvps[vl][:, pr, :], Ktok[:, n, pr, :], Vs[vl][:, pr * 128:(pr + 1) * 128], start=True, stop=True)
                            return last
                        tr.op("pe", mmkv, reads=[B_Ktok[n], B_Vs[vl]], writes=[B_kvps[vl]])
                        if vl == 0:
                            tr.op("act", lambda e, n=n, vl=vl: e.activation(out=KV[0:64, n, :, :], in_=kvps[vl][0:64, :, 0:64], func=AF.Copy),
                                  reads=[B_kvps[vl]], writes=[B_KV[n]])
                            tr.op("act", lambda e, n=n, vl=vl: e.activation(out=KV[64:128, n, :, :], in_=kvps[vl][64:128, :, 64:128], func=AF.Copy),
                                  reads=[B_kvps[vl]], writes=[B_KV[n]])
                        else:
                            tr.op("dve", lambda e, n=n, vl=vl: e.tensor_copy(KV[0:64, n, :, :], kvps[vl][0:64, :, 0:64]),
                                  reads=[B_kvps[vl]], writes=[B_KV[n]])
                            tr.op("dve", lambda e, n=n, vl=vl: e.tensor_copy(KV[64:128, n, :, :], kvps[vl][64:128, :, 64:128]),
                                  reads=[B_kvps[vl]], writes=[B_KV[n]])
                    tr.op("pool", lambda e, Srun=Srun: e.memset(Srun[:], 0.0), writes=[B_Srun])
                    for n in order:
                        tr.op("act", lambda e, n=n, Srun=Srun, Sbf=Sbf: e.activation(out=Sbf[:, n, :, :], in_=Srun[:, :, :], func=AF.Copy),
                              reads=[B_Srun], writes=[B_Sbf[n]])
                        for pr in range(2):
                            tr.op("dve", lambda e, n=n, pr=pr, Srun=Srun, GC=GC, hg=hg: e.scalar_tensor_tensor(
                                Srun[:, pr, :], Srun[:, pr, :], GC[:, 2 * hg + pr:2 * hg + pr + 1], KV[:, n, pr, :], ALU.mult, ALU.add),
                                reads=[B_KV[n], B_Srun, B_tab], writes=[B_Srun])
                if CUT < 5:
                    continue
                def stage_a(n, hg=hg):
                    cs = slice(n * 128, (n + 1) * 128)
                    sl = n % 2
                    tr.op("pool", lambda e: e.tensor_tensor(Qf[sl][:], rqT[:, :, cs], QDF[:, 2 * hg:2 * hg + 2, :], ALU.mult),
                          reads=[B_rqT, B_tab], writes=[B_Qf[sl]])
                    tr.op("pool", lambda e: e.tensor_tensor(Qb[sl][:], rqT[:, :, cs], QDB[:, 2 * hg:2 * hg + 2, :], ALU.mult),
                          reads=[B_rqT, B_tab], writes=[B_Qb[sl]])

                    def mms(e):
                        last = None
                        for hh in range(4):
                            pr, par = hh // 2, hh % 2
                            base = 64 * par
                            last = e.matmul(S4[2 * sl + par][:, pr * 128:(pr + 1) * 128], rkT[base:base + 64, pr, cs],
                                            rqT[base:base + 64, pr, cs], start=True, stop=True)
                        return last
                    tr.op("pe", mms, reads=[B_rkT, B_rqT], writes=[B_S4[2 * sl], B_S4[2 * sl + 1]])
                    for par in range(2):
                        tr.op("dve", lambda e, par=par: e.tensor_tensor(
                            Pm[sl][:, par, :, :], S4[2 * sl + par][:, 0:256].rearrange("p (j c) -> p j c", j=2),
                            DT[:, 4 * hg + par:4 * hg + 4:2, :], ALU.mult),
                            reads=[B_S4[2 * sl + par], B_tab], writes=[B_Pm[sl]])

                def stage_b(n, hg=hg):
                    sl = n % 2
                    yb = ypsL[sl]

                    def mmy(e):
                        last = None
                        for hh in range(4):
                            pr, base = hh // 2, 64 * (hh % 2)
                            e.matmul(yb[0:64, hh, :], Vt[:, n, hh * 64:(hh + 1) * 64], Pm[sl][:, hh % 2, hh // 2, :], start=True, stop=False)
                            e.matmul(yb[0:64, hh, :], Sfb[base:base + 64, n, pr, :], Qf[sl][base:base + 64, pr, :], start=False, stop=False)
                            last = e.matmul(yb[0:64, hh, :], Sbb[base:base + 64, n, pr, :], Qb[sl][base:base + 64, pr, :], start=False, stop=True)
                        return last
                    tr.op("pe", mmy, reads=[B_Vt[n], B_Pm[sl], B_Sfb[n], B_Sbb[n], B_Qf[sl], B_Qb[sl]], writes=[B_ypsL[sl]])
                    tr.op("act", lambda e: e.activation(out=sqyL[sl][:], in_=yb[0:64, :, :].rearrange("p a b -> p (a b)"), func=AF.Square),
                          reads=[B_ypsL[sl]], writes=[B_sqyL[sl]])

                def stage_c(n, hg=hg):
                    cs = slice(n * 128, (n + 1) * 128)
                    sl = n % 2
                    yb = ypsL[sl]
                    tr.op("pe", lambda e: e.matmul(ssb[:, :], ones_b[0:64, 0:64], sqyL[sl][:, :], start=True, stop=True),
                          reads=[B_sqyL[sl], B_const], writes=[B_ssb])
                    tr.op("act", lambda e: e.activation(out=rs[:], in_=ssb[:, :], func=AF.Ln, scale=1.0 / 64, bias=EPS),
                          reads=[B_ssb], writes=[B_rs])
                    tr.op("act", lambda e: e.activation(out=rs2[:], in_=rs[:], func=AF.Exp, scale=-0.5), reads=[B_rs], writes=[B_rs2])
                    tr.op("dve", lambda e: e.tensor_tensor(ty[:], yb[0:64, :, :].rearrange("p a b -> p (a b)"), rs2[:], ALU.mult),
                          reads=[B_ypsL[sl], B_rs2], writes=[B_ty])
                    tr.op("dve", lambda e: e.tensor_tensor(mixT[64:128, 4 * hg:4 * hg + 4, cs], ty[:, :].rearrange("p (a b) -> p a b", a=4),
                                                           sgT[0:64, :, cs], ALU.mult),
                          reads=[B_ty, B_sgT], writes=[B_retT])
                if CUT >= 9:
                    stage_a(0)
                    stage_a(1)
                    stage_b(0)
                    for n in range(NT):
                        if n + 2 < NT:
                            stage_a(n + 2)
                        if n + 1 < NT:
                            stage_b(n + 1)
                        stage_c(n)
            if "retT" in dbg:
                dump("retT", mixT[64:128, :, :], [64, 8, S], BF16)
            tr.barrier()
            tr.emit()
        if stop_after and stop_after[0] == "D":
            tr.barrier(); tr.emit()
            return nc, dbg_outs

        CAP = MOE_CAP
        NSL = 16 * CAP
        BIGIDX = 1.0e6
        xe_d = nc.dram_tensor("xe_scratch", [NSL + 128, D], BF16, kind="Internal").ap()
        y_d = nc.dram_tensor("y_scratch", [NSL + 128, D], F32, kind="Internal").ap()
        hres = sb("hres", [128, NT, D], F32)
        gates = sb("gates", [128, NT, 16], F32)
        OH1 = sb("OH1", [128, NT, 16], F32)
        OH2 = sb("OH2", [128, NT, 16], F32)
        A12 = sb("A12", [128, 2, NT], F32)
        AV = sb("AV", [128, 2, NT], F32)
        DI = sb("DI", [128, 2, NT], mybir.dt.int32)
        flag = sb("flag", [1, 2], mybir.dt.int32)
        B_hres = [Buf() for _ in range(NT)]
        B_gates = Buf()
        B_OH, B_A, B_AV, B_DI, B_flag, B_xe, B_yd = Buf(), Buf(), Buf(), Buf(), Buf(), Buf(), Buf()
        hnT = xnT
        BIGM = 1.0e4
        with ExitStack() as ph:
            hn_tok = [sb("hn_tok%d" % i, [128, D], BF16, ph) for i in range(2)]
            Mk2 = [sb("Mk2_%d" % i, [128, 16], F32, ph) for i in range(2)]
            Pos2 = [sb("Pos2_%d" % i, [128, 16], F32, ph) for i in range(2)]
            V2 = [sb("V2_%d" % i, [128, 16], F32, ph) for i in range(2)]
            Q2 = [sb("Q2_%d" % i, [128, 16], F32, ph) for i in range(2)]
            tmp2 = [sb("tmp2_%d" % i, [128, 16], F32, ph) for i in range(2)]
            Run = sb("Run", [128, 16], F32, ph)
            iotaC = sb("iotaC", [128, 16], F32, ph)
            B_rt = [Buf(), Buf()]
            B_run = Buf()
            B_DIt = [Buf() for _ in range(NT)]
            s_sc = tr.dma_sem("scat")
            wo = sb("wo", [128, 8, D], BF16, ph)
            wr32 = sb("wr32", [128, 8, 20], F32, ph)
            brB = sb("brB", [128, 20], F32, ph)
            nffnB = sb("nffnB", [128, D], F32, ph)
            ustr = sb("ustr", [128, 128], F32, ph)
            iota16 = sb("iota16", [128, 17], F32, ph)
            zt = sb("zt", [128, D], BF16, ph)
            xt = [sb("ext%d" % i, [128, D], F32, ph) for i in range(2)]
            hsL = [sb("hs%d" % i, [128, D], F32, ph) for i in range(2)]
            junk = sb("junkE", [128, D], BF16, ph)
            hn32L = [sb("hn32_%d" % i, [128, 8, 128], F32, ph) for i in range(2)]
            sm = [sb("smallE%d" % i, [128, 24], F32, ph) for i in range(2)]
            Lg = [sb("Lg%d" % i, [128, 20], F32, ph) for i in range(2)]
            elm = [sb("elm%d" % i, [128, 16], F32, ph) for i in range(2)]
            elm2 = [sb("elm2%d" % i, [128, 16], F32, ph) for i in range(2)]
            ohg = [sb("ohg%d" % i, [128, 4], F32, ph) for i in range(2)]
            eg = [sb("eg%d" % i, [128, 4], F32, ph) for i in range(2)]
            Mk = sb("Mk", [128, NT, 16], F32, ph)
            Pos = sb("Pos", [128, NT, 16], F32, ph)
            Tot = sb("Tot", [128, NT, 16], F32, ph)
            Off = sb("Off", [128, NT, 16], F32, ph)
            tmpR = sb("tmpR", [128, NT, 16], F32, ph)
            SL = sb("SL", [128, 2, NT], F32, ph)
            EI = sb("EI", [128, 2, NT], F32, ph)
            VL = sb("VL", [128, 2, NT], F32, ph)
            DF = sb("DF", [128, 2, NT], F32, ph)
            ovs = sb("ovs", [128, 4], F32, ph)
            oA = [ps("oA%d" % i, [128, 512], F32, ph) for i in range(2)]
            oR = [ps("oR%d" % i, [128, 512], F32, ph) for i in range(2)]
            tp32 = ps("tp32", [128, 8, 128], F32, ph)
            bankX = [ps("bankX%d" % i, [128, 512], F32, ph) for i in range(2)]
            lgpsL = [bankX[i][:, 0:20] for i in range(2)]
            B_wo, B_wr, B_junk = Buf(), Buf(), Buf()
            B_hsL = [Buf(), Buf()]
            B_hn32L = [Buf(), Buf()]
            B_lgpsL = [Buf(), Buf()]
            B_sm = [Buf() for _ in range(2)]
            B_g = [Buf() for _ in range(2)]
            B_hnt = [Buf() for _ in range(2)]
            B_xt = [Buf() for _ in range(2)]
            B_oA = [Buf() for _ in range(2)]
            B_oR = [Buf() for _ in range(2)]
            B_tp32, B_r = Buf(), Buf()
            B_zt = Buf()
            s_e = tr.dma_sem("e0")
            s_ex = [tr.dma_sem() for _ in range(2)]
            tr.dma("pool", s_e, lambda e: [
                e.dma_start(out=wo[0:64, :, :], in_=wout_d[0:512, :].rearrange("(h p) c -> p h c", p=64)),
                e.dma_start(out=wo[64:128, :, :], in_=wout_d[512:1024, :].rearrange("(h p) c -> p h c", p=64))], writes=[B_wo], n=2)
            s_e2 = tr.dma_sem("e1")
            tr.dma("sp", s_e2, lambda e: [
                e.dma_start(out=wr32[:], in_=wr_d[:, :].rearrange("(k p) c -> p k c", p=128)),
                e.dma_start(out=brB[:], in_=br_d.partition_broadcast(128)),
                e.dma_start(out=nffnB[:], in_=nffr_d.partition_broadcast(128)),
                e.dma_start(out=ustr[:], in_=c_ustr_d[:, :]),
                e.dma_start(out=iota16[:], in_=c_iota_d[:, :])], writes=[B_wr], n=5)
            tr.op("pool", lambda e: e.memset(Run[:], 0.0), writes=[B_run])
            tr.op("dve", lambda e: e.tensor_scalar(iotaC[:], iota16[:, 0:16], float(CAP), None, ALU.mult), reads=[B_wr], writes=[B_run])
            tr.op("dve", lambda e: e.tensor_scalar(ovs[:, 3:4], iota16[:, 16:17], float(NSL), None, ALU.add), reads=[B_wr], writes=[B_run])
            tr.op("pool", lambda e: e.memset(zt[:], 0.0), writes=[B_zt])
            s_z = tr.dma_sem("z0")
            nz = NSL // 128 + 1
            def tile_stages(i):
                cs = slice(i * 128, (i + 1) * 128)
                sl = i % 2
                SM = sm[sl]
                hs = hsL[sl]
                hn32 = hn32L[sl]
                lgps = lgpsL[sl]
                B_hs, B_hn32, B_lgps = B_hsL[sl], B_hn32L[sl], B_lgpsL[sl]
                tr.begin_capture()
                tr.dma("sp", s_ex[sl], lambda e, i=i, sl=sl: [e.dma_start(out=xt[sl][:], in_=x_d[i * 128:(i + 1) * 128, :])], writes=[B_xt[sl]])
                for half in range(2):
                    def mmo(e, cs=cs, half=half):
                        last = None
                        for h in range(8):
                            e.matmul(oA[half][:, :], mixT[0:64, h, cs], wo[0:64, h, half * 512:(half + 1) * 512], start=(h == 0), stop=(h == 7))
                            last = e.matmul(oR[half][:, :], mixT[64:128, h, cs], wo[64:128, h, half * 512:(half + 1) * 512], start=(h == 0), stop=(h == 7))
                        return last
                    tr.op("pe", mmo, reads=[B_attnT, B_retT, B_wo], writes=[B_oA[half], B_oR[half]])
                S_ = [B_sm[sl]]
                tr.op("dve", lambda e, i=i, SM=SM: e.tensor_reduce(SM[:, 0:1], ssh[:, i, :], AX.X, ALU.add), reads=[B_ssh], writes=S_)
                tr.op("act", lambda e, SM=SM: e.activation(out=SM[:, 1:2], in_=SM[:, 0:1], func=AF.Sqrt, scale=1.0 / 512, bias=EPS), reads=S_, writes=S_)
                tr.op("dve", lambda e, SM=SM: e.reciprocal(SM[:, 2:3], SM[:, 1:2]), reads=S_, writes=S_)
                for half in range(2):
                    hsl = slice(half * 512, (half + 1) * 512)
                    tr.op("dve", lambda e, i=i, half=half, hsl=hsl, sl=sl, SM=SM: e.scalar_tensor_tensor(
                        hres[:, i, hsl], oA[half][:, :], SM[:, 2:3], xt[sl][:, hsl], ALU.mult, ALU.add),
                        reads=[B_oA[half], B_sm[sl], B_xt[sl]], writes=[B_hres[i]])
                    tr.op("dve", lambda e, i=i, half=half, hsl=hsl: e.tensor_tensor(hres[:, i, hsl], hres[:, i, hsl], oR[half][:, :], ALU.add),
                          reads=[B_oR[half], B_hres[i]], writes=[B_hres[i]])
                tr.op("act", lambda e, i=i, SM=SM: e.activation(out=junk[:], in_=hres[:, i, :], func=AF.Square, accum_out=SM[:, 3:4]),
                      reads=[B_hres[i]], writes=[B_junk] + S_)
                tr.op("act", lambda e, SM=SM: e.activation(out=SM[:, 4:5], in_=SM[:, 3:4], func=AF.Sqrt, scale=1.0 / D, bias=EPS), reads=S_, writes=S_)
                tr.op("dve", lambda e, SM=SM: e.reciprocal(SM[:, 5:6], SM[:, 4:5]), reads=S_, writes=S_)
                tr.op("dve", lambda e, i=i, SM=SM: e.tensor_scalar(hs[:], hres[:, i, :], SM[:, 5:6], None, ALU.mult), reads=[B_hres[i]] + S_, writes=[B_hs])
                tr.op("pool", lambda e, sl=sl: e.tensor_tensor(hn_tok[sl][:], hs[:], nffnB[:], ALU.mult), reads=[B_hs, B_wr], writes=[B_hnt[sl]])

                st1 = tr.end_capture()
                tr.begin_capture()

                def tps32(e):
                    last = None
                    for k in range(8):
                        last = e.transpose(tp32[:, k, :], hs[:, k * 128:(k + 1) * 128], ident_f[:])
                    return last
                tr.op("pe", tps32, reads=[B_hs, B_const], writes=[B_tp32])
                tr.op("dve", lambda e: e.tensor_tensor(hn32[:], tp32[:, :, :], nffn[:, :].unsqueeze(2).broadcast_to([128, 8, 128]), ALU.mult),
                      reads=[B_tp32, B_const], writes=[B_hn32])
                tr.op("pool", lambda e, cs=cs: e.tensor_copy(hnT[:, :, PAD + cs.start:PAD + cs.stop], hn32[:]), reads=[B_hn32], writes=[B_xnT])

                def mmr(e):
                    last = None
                    for k in range(8):
                        last = e.matmul(lgps[:, :], hn32[:, k, :], wr32[:, k, :], start=(k == 0), stop=(k == 7))
                    return last
                tr.op("pe", mmr, reads=[B_hn32, B_wr], writes=[B_lgps])
                G = [B_g[sl]]
                L_, E1, E2, OG, EG = Lg[sl], elm[sl], elm2[sl], ohg[sl], eg[sl]
                o1 = lambda i=i: OH1[:, i, :]
                o2 = lambda i=i: OH2[:, i, :]
                tr.op("dve", lambda e, L_=L_: e.tensor_tensor(L_[:], lgps[:, :], brB[:], ALU.add), reads=[B_lgps, B_wr], writes=G)
                tr.op("dve", lambda e, L_=L_, SM=SM: e.tensor_reduce(SM[:, 8:9], L_[:, 0:4], AX.X, ALU.max), reads=G, writes=G)
                tr.op("dve", lambda e, SM=SM: e.tensor_scalar(SM[:, 9:10], SM[:, 8:9], -1.0, None, ALU.mult), reads=G, writes=G)
                tr.op("act", lambda e, L_=L_, SM=SM, EG=EG: e.activation(out=EG[:], in_=L_[:, 0:4], func=AF.Exp, bias=SM[:, 9:10], scale=1.0, accum_out=SM[:, 10:11]),
                      reads=G, writes=G)
                tr.op("dve", lambda e, SM=SM: e.reciprocal(SM[:, 11:12], SM[:, 10:11]), reads=G, writes=G)
                tr.op("dve", lambda e, L_=L_, SM=SM, OG=OG: e.tensor_tensor(OG[:], L_[:, 0:4], SM[:, 8:9].broadcast_to([128, 4]), ALU.is_equal), reads=G, writes=G)
                tr.op("dve", lambda e, OG=OG: e.tensor_scalar(OG[:], OG[:], BIGM, -BIGM, ALU.mult, ALU.add), reads=G, writes=G)
                tr.op("dve", lambda e, L_=L_, OG=OG, E1=E1: e.tensor_tensor(E1[:, :].rearrange("p (g j) -> p g j", g=4), L_[:, 4:20].rearrange("p (g j) -> p g j", g=4),
                                                                           OG[:, :].unsqueeze(2).broadcast_to([128, 4, 4]), ALU.add), reads=G, writes=G)
                tr.op("dve", lambda e, SM=SM, E1=E1: e.tensor_reduce(SM[:, 12:13], E1[:], AX.X, ALU.max), reads=G, writes=G)
                tr.op("dve", lambda e, SM=SM, E1=E1, o1=o1: e.tensor_tensor(o1(), E1[:], SM[:, 12:13].broadcast_to([128, 16]), ALU.is_equal), reads=G, writes=G + [B_OH])
                tr.op("dve", lambda e, E1=E1, E2=E2, o1=o1: e.scalar_tensor_tensor(E2[:], o1(), -BIGM, E1[:], ALU.mult, ALU.add), reads=G + [B_OH], writes=G)
                tr.op("dve", lambda e, SM=SM, E2=E2: e.tensor_reduce(SM[:, 13:14], E2[:], AX.X, ALU.max), reads=G, writes=G)
                tr.op("dve", lambda e, SM=SM, E2=E2, o2=o2: e.tensor_tensor(o2(), E2[:], SM[:, 13:14].broadcast_to([128, 16]), ALU.is_equal), reads=G, writes=G + [B_OH])
                tr.op("dve", lambda e, SM=SM: e.tensor_tensor(SM[:, 14:15], SM[:, 13:14], SM[:, 12:13], ALU.subtract), reads=G, writes=G)
                tr.op("act", lambda e, SM=SM: e.activation(out=SM[:, 15:16], in_=SM[:, 14:15], func=AF.Exp), reads=G, writes=G)
                tr.op("dve", lambda e, SM=SM: e.tensor_scalar(SM[:, 16:17], SM[:, 15:16], 1.0, None, ALU.add), reads=G, writes=G)
                tr.op("dve", lambda e, SM=SM: e.reciprocal(SM[:, 17:18], SM[:, 16:17]), reads=G, writes=G)
                tr.op("dve", lambda e, SM=SM, i=i: e.tensor_tensor(A12[:, 0, i:i + 1], SM[:, 17:18], SM[:, 11:12], ALU.mult), reads=G, writes=G + [B_A])
                tr.op("dve", lambda e, SM=SM, i=i: e.tensor_tensor(A12[:, 1, i:i + 1], A12[:, 0, i:i + 1], SM[:, 15:16], ALU.mult), reads=G + [B_A], writes=[B_A])
                st2 = tr.end_capture()
                tr.begin_capture()
                RR = [B_rt[sl]]
                Mki, Posi, Vi, Qi, tmpi = Mk2[sl], Pos2[sl], V2[sl], Q2[sl], tmp2[sl]
                rps = bankX[sl][:, 32:64]
                tr.op("dve", lambda e: e.tensor_tensor(Mki[:], OH1[:, i, :], OH2[:, i, :], ALU.add), reads=[B_OH] + G, writes=RR)

                def rmm(e):
                    e.matmul(rps[:, 0:16], ustr[:, :], Mki[:, :], start=True, stop=True)
                    return e.matmul(rps[:, 16:32], ones_f[:, :], Mki[:, :], start=True, stop=True)
                tr.op("pe", rmm, reads=RR + [B_wr, B_const], writes=[B_lgps])
                tr.op("dve", lambda e: e.tensor_tensor(Posi[:], rps[:, 0:16], Run[:], ALU.add), reads=[B_lgps, B_run], writes=RR)
                tr.op("dve", lambda e: e.tensor_tensor(Run[:], Run[:], rps[:, 16:32], ALU.add), reads=[B_lgps, B_run], writes=[B_run])
                tr.op("dve", lambda e: e.tensor_scalar(Vi[:], Posi[:], float(CAP), None, ALU.is_lt), reads=RR, writes=RR)
                tr.op("dve", lambda e: e.tensor_tensor(Qi[:], Posi[:], iotaC[:], ALU.add), reads=RR + [B_run], writes=RR)
                for a, OHa in enumerate([OH1, OH2]):
                    tr.op("dve", lambda e, OHa=OHa: e.tensor_tensor(tmpi[:], OHa[:, i, :], Qi[:], ALU.mult), reads=RR + [B_OH], writes=RR)
                    tr.op("dve", lambda e, a=a: e.tensor_reduce(DF[:, a, i:i + 1], tmpi[:], AX.X, ALU.add), reads=RR, writes=RR + [B_r])
                    tr.op("dve", lambda e, OHa=OHa: e.tensor_tensor(tmpi[:], OHa[:, i, :], Vi[:], ALU.mult), reads=RR + [B_OH], writes=RR)
                    tr.op("dve", lambda e, a=a: e.tensor_reduce(VL[:, a, i:i + 1], tmpi[:], AX.X, ALU.add), reads=RR, writes=RR + [B_r])
                dfi = lambda: DF[:, :, i:i + 1]
                tr.op("dve", lambda e: e.tensor_scalar(dfi(), dfi(), ovs[:, 3:4], None, ALU.subtract), reads=RR + [B_r, B_run], writes=RR + [B_r])
                tr.op("dve", lambda e: e.tensor_tensor(dfi(), dfi(), VL[:, :, i:i + 1], ALU.mult), reads=RR + [B_r], writes=RR + [B_r])
                tr.op("dve", lambda e: e.tensor_scalar(dfi(), dfi(), ovs[:, 3:4], None, ALU.add), reads=RR + [B_r], writes=RR + [B_r])
                tr.op("dve", lambda e: e.tensor_copy(DI[:, :, i:i + 1], dfi()), reads=RR + [B_r], writes=[B_DIt[i]])
                for a in range(2):
                    tr.dma("pool", s_sc, lambda e, a=a: [e.indirect_dma_start(
                        out=xe_d[:, :], out_offset=bass.IndirectOffsetOnAxis(ap=DI[:, a, i:i + 1], axis=0),
                        in_=hn_tok[sl][:, :], in_offset=None)],
                        reads=[B_DIt[i], B_hnt[sl]], writes=[B_xe])
                st3 = tr.end_capture()
                return st1, st2 + st3

            stages = [tile_stages(i) for i in range(NT)]
            tr.replay(stages[0][0])
            for i in range(NT):
                if i + 1 < NT:
                    tr.replay(stages[i + 1][0], stages[i][1])
                else:
                    tr.replay(stages[i][1])

            R = [B_r]
            with ExitStack() as ph2:
                ovp = oR[0]
                B_ovp = B_oR[0]
                fl = lambda t: t[:, :, :].rearrange("p a b -> p (a b)")
                tr.op("dve", lambda e: e.tensor_tensor(fl(AV), fl(A12), fl(VL), ALU.mult), reads=R + [B_A], writes=[B_AV])
                tr.op("dve", lambda e: e.tensor_tensor(fl(DF), fl(A12), fl(AV), ALU.subtract), reads=R + [B_A, B_AV] + B_DIt, writes=R)
                tr.op("dve", lambda e: e.tensor_tensor(gates[:], OH1[:], DF[:, 0, :].unsqueeze(2).broadcast_to([128, NT, 16]), ALU.mult),
                      reads=R + [B_OH], writes=[B_gates])
                tr.op("dve", lambda e: e.tensor_tensor(tmpR[:], OH2[:], DF[:, 1, :].unsqueeze(2).broadcast_to([128, NT, 16]), ALU.mult),
                      reads=R + [B_OH], writes=R)
                tr.op("dve", lambda e: e.tensor_tensor(gates[:], gates[:], tmpR[:], ALU.add), reads=R + [B_gates], writes=[B_gates])
                tr.op("dve", lambda e: e.tensor_scalar(fl(VL), fl(VL), -1.0, 1.0, ALU.mult, ALU.add), reads=R + [B_AV], writes=R)
                tr.op("dve", lambda e: e.tensor_reduce(ovs[:, 0:1], fl(VL), AX.X, ALU.add), reads=R, writes=R)
                tr.op("pe", lambda e: e.matmul(ovp[0:1, 0:1], ovs[:, 0:1], ones_f[:, 0:1], start=True, stop=True), reads=R + [B_const], writes=[B_ovp])
                tr.op("dve", lambda e: e.tensor_copy(ovs[0:1, 2:3], ovp[0:1, 0:1]), reads=[B_ovp], writes=R)
                if FORCE_FALLBACK:
                    tr.op("dve", lambda e: e.tensor_scalar(ovs[0:1, 2:3], ovs[0:1, 2:3], 1.0, None, ALU.add), reads=R, writes=R)
                tr.op("dve", lambda e: e.tensor_copy(flag[0:1, 0:1], ovs[0:1, 2:3]), reads=R, writes=[B_flag])
                if "route" in dbg:
                    dump("DI", DI[:], [128, 2, NT], mybir.dt.int32)
                    dump("AV", AV[:], [128, 2, NT], F32)
                    dump("gres", gates[:], [128, NT, 16], F32)
                    dump("flag", flag[:], [1, 2], mybir.dt.int32)
                if "hres" in dbg:
                    dump("hres", hres[:], [128, NT, D], F32)
                tr.barrier()
                tr.emit()
        if stop_after == "E":
            tr.barrier(); tr.emit()
            return nc, dbg_outs

        NS3 = CAP // 128
        with ExitStack() as ph:
            w1b = [sb("w1b%d" % i, [128, 8, 512], BF16, ph) for i in range(2)]
            w3b = [sb("w3b%d" % i, [128, 8, 512], BF16, ph) for i in range(2)]
            w2b = [sb("w2b%d" % i, [128, 4, D], BF16, ph) for i in range(2)]
            Xt = [mixT[:, 0:NS3, 0:D], mixT[:, NS3:2 * NS3, 0:D]]
            XT = [mixT[:, :, D:D + CAP], sb("XT1", [128, 8, CAP], BF16, ph)]
            hidT = mixT[:, 0:4, D + CAP:D + 2 * CAP]
            sil = [sb("sil%d" % i, [128, CAP], F32, ph) for i in range(2)]
            ysb = [sb("ysb%d" % i, [128, D], F32, ph) for i in range(2)]
            tpx = [ps("tpx%d" % i, [128, 8, 128], BF16, ph) for i in range(2)]
            h1p = [ps("h1p%d" % i, [128, 512], F32, ph)[:, 0:CAP] for i in range(2)]
            h3p = [ps("h3p%d" % i, [128, 512], F32, ph)[:, 0:CAP] for i in range(2)]
            yp = [ps("yp%d" % i, [128, 512], F32, ph) for i in range(2)]
            B_w = [Buf() for _ in range(2)]
            B_Xt = [Buf() for _ in range(2)]
            B_XT = [Buf() for _ in range(2)]
            B_hidT = Buf()
            B_sil = [Buf() for _ in range(2)]
            B_ysb = [Buf() for _ in range(2)]
            B_tpx = [Buf() for _ in range(2)]
            B_h1 = [Buf() for _ in range(2)]
            B_h3 = [Buf() for _ in range(2)]
            B_yp = [Buf() for _ in range(2)]
            s_m = [tr.dma_sem() for _ in range(2)]
            s_xl = [tr.dma_sem() for _ in range(2)]
            s_ys = [tr.dma_sem() for _ in range(2)]
            cnt = [0, 0, 0, 0]

            def load_w(ex):
                ws = ex % 2
                tr.dma("pool", s_m[ws], lambda e: [
                    e.dma_start(out=w1b[ws][:], in_=w1_d[ex].rearrange("(p k) f -> p k f", k=8)),
                    e.dma_start(out=w3b[ws][:], in_=w3_d[ex].rearrange("(p k) f -> p k f", k=8)),
                    e.dma_start(out=w2b[ws][:], in_=w2_d[ex].rearrange("(p k) d -> p k d", k=4))], writes=[B_w[ws]], n=3)

            def load_x(ex):
                xs_ = ex % 2
                tr.dma("sp", s_xl[xs_], lambda e: [e.dma_start(out=Xt[xs_], in_=xe_d[ex * CAP:(ex + 1) * CAP, :].rearrange("(s p) d -> p s d", p=128))],
                       reads=[B_xe], writes=[B_Xt[xs_]])

            def transposes(ex):
                xs_ = ex % 2
                for s3 in range(NS3):
                    tl = cnt[0] % 2
                    cnt[0] += 1

                    def tpf(e, s3=s3, tl=tl):
                        last = None
                        for k in range(8):
                            last = e.transpose(tpx[tl][:, k, :], Xt[xs_][:, s3, k:D:8], ident_b[:])
                        return last
                    tr.op("pe", tpf, reads=[B_Xt[xs_], B_const], writes=[B_tpx[tl]])
                    if cnt[0] % 2 == 0:
                        tr.op("act", lambda e, s3=s3, tl=tl: e.activation(out=XT[xs_][:, :, s3 * 128:(s3 + 1) * 128], in_=tpx[tl][:, :, :], func=AF.Copy),
                              reads=[B_tpx[tl]], writes=[B_XT[xs_]])
                    else:
                        tr.op("dve", lambda e, s3=s3, tl=tl: e.tensor_copy(XT[xs_][:, :, s3 * 128:(s3 + 1) * 128], tpx[tl][:, :, :]),
                              reads=[B_tpx[tl]], writes=[B_XT[xs_]])

            load_w(0)
            load_x(0)
            load_x(1)
            transposes(0)
            for ex in range(16):
                ws = ex % 2
                xs_ = ex % 2
                if ex + 1 < 16:
                    load_w(ex + 1)
                for ffc in range(4):
                    bl = cnt[1] % 2
                    sl2 = cnt[1] % 2
                    cnt[1] += 1

                    def mm13(e, ffc=ffc, wt=None, dst=None, ws=ws, xs_=xs_):
                        last = None
                        for k in range(8):
                            last = e.matmul(dst[:, :], wt[ws][:, k, ffc:512:4], XT[xs_][:, k, :], start=(k == 0), stop=(k == 7))
                        return last
                    tr.op("pe", lambda e, ffc=ffc, bl=bl, ws=ws, xs_=xs_, f=mm13: f(e, ffc, w1b, h1p[bl], ws, xs_), reads=[B_w[ws], B_XT[xs_]], writes=[B_h1[bl]])
                    tr.op("pe", lambda e, ffc=ffc, bl=bl, ws=ws, xs_=xs_, f=mm13: f(e, ffc, w3b, h3p[bl], ws, xs_), reads=[B_w[ws], B_XT[xs_]], writes=[B_h3[bl]])
                    tr.op("act", lambda e, sl2=sl2, bl=bl: e.activation(out=sil[sl2][:], in_=h1p[bl][:, :], func=AF.Silu), reads=[B_h1[bl]], writes=[B_sil[sl2]])
                    tr.op("dve", lambda e, sl2=sl2, ffc=ffc, bl=bl: e.tensor_tensor(hidT[:, ffc, :], sil[sl2][:], h3p[bl][:, :], ALU.mult),
                          reads=[B_sil[sl2], B_h3[bl]], writes=[B_hidT])
                if ex + 1 < 16:
                    transposes(ex + 1)
                if ex + 2 < 16:
                    load_x(ex + 2)
                for s3 in range(NS3):
                    yl = cnt[2] % 2
                    cnt[2] += 1
                    for half in range(2):
                        ol = cnt[3] % 2
                        cnt[3] += 1

                        def mm2(e, s3=s3, half=half, ol=ol, ws=ws):
                            last = None
                            for ffc in range(4):
                                last = e.matmul(yp[ol][:, :], hidT[:, ffc, s3 * 128:(s3 + 1) * 128],
                                                w2b[ws][:, ffc, half * 512:(half + 1) * 512], start=(ffc == 0), stop=(ffc == 3))
                            return last
                        tr.op("pe", mm2, reads=[B_hidT, B_w[ws]], writes=[B_yp[ol]])
                        if half == 0:
                            tr.op("act", lambda e, yl=yl, ol=ol: e.activation(out=ysb[yl][:, 0:512], in_=yp[ol][:, :], func=AF.Copy),
                                  reads=[B_yp[ol]], writes=[B_ysb[yl]])
                        else:
                            tr.op("dve", lambda e, yl=yl, ol=ol: e.tensor_copy(ysb[yl][:, 512:1024], yp[ol][:, :]),
                                  reads=[B_yp[ol]], writes=[B_ysb[yl]])
                    r0 = ex * CAP + s3 * 128
                    tr.dma("sp", s_ys[yl], lambda e, yl=yl, r0=r0: [e.dma_start(out=y_d[r0:r0 + 128, :], in_=ysb[yl][:])],
                           reads=[B_ysb[yl]], writes=[B_yd])
            tr.barrier()
            tr.emit()

        with ExitStack() as ph:
            tr2 = Tracker(nc, ph, prefix="fb_")
            w1b = [sb("fw1b%d" % i, [128, 8, 512], BF16, ph) for i in range(2)]
            w3b = [sb("fw3b%d" % i, [128, 8, 512], BF16, ph) for i in range(2)]
            w2b = [sb("fw2b%d" % i, [128, 4, D], BF16, ph) for i in range(2)]
            hid = [sb("fhid%d" % i, [128, 4, 512], BF16, ph) for i in range(2)]
            sil = [sb("fsil%d" % i, [128, 512], F32, ph) for i in range(2)]
            h1p = [ps("fh1p%d" % i, [128, 512], F32, ph) for i in range(2)]
            h3p = [ps("fh3p%d" % i, [128, 512], F32, ph) for i in range(2)]
            op_ = [ps("fop%d" % i, [128, 512], F32, ph) for i in range(4)]
            B_w = [Buf() for _ in range(2)]
            B_hid = [Buf() for _ in range(2)]
            B_sil = [Buf() for _ in range(2)]
            B_h1 = [Buf() for _ in range(2)]
            B_h3 = [Buf() for _ in range(2)]
            B_op = [Buf() for _ in range(4)]
            F_hres = [Buf() for _ in range(NT)]
            s_m = [tr2.dma_sem() for _ in range(2)]
            cnt = [0, 0, 0]
            for ex in range(16):
                ws = ex % 2
                tr2.dma("pool", s_m[ws], lambda e, ex=ex, ws=ws: [
                    e.dma_start(out=w1b[ws][:], in_=w1_d[ex].rearrange("(k p) f -> p k f", p=128)),
                    e.dma_start(out=w3b[ws][:], in_=w3_d[ex].rearrange("(k p) f -> p k f", p=128)),
                    e.dma_start(out=w2b[ws][:], in_=w2_d[ex].rearrange("(k p) d -> p k d", p=128))], writes=[B_w[ws]], n=3)
                for tc in range(4):
                    hsl = cnt[0] % 2
                    cnt[0] += 1
                    for ffc in range(4):
                        bl = cnt[1] % 2
                        cnt[1] += 1

                        def mm13(e, ws=ws, tc=tc, ffc=ffc, wt=None, dst=None):
                            last = None
                            for k in range(8):
                                last = e.matmul(dst[:, :], wt[ws][:, k, ffc * 128:(ffc + 1) * 128],
                                                hnT[:, k, PAD + tc * 512:PAD + (tc + 1) * 512], start=(k == 0), stop=(k == 7))
                            return last
                        tr2.op("pe", lambda e, ws=ws, tc=tc, ffc=ffc, bl=bl: mm13(e, ws, tc, ffc, w1b, h1p[bl]), reads=[B_w[ws]], writes=[B_h1[bl]])
                        tr2.op("pe", lambda e, ws=ws, tc=tc, ffc=ffc, bl=bl: mm13(e, ws, tc, ffc, w3b, h3p[bl]), reads=[B_w[ws]], writes=[B_h3[bl]])
                        tr2.op("act", lambda e, bl=bl: e.activation(out=sil[bl][:], in_=h1p[bl][:, :], func=AF.Silu), reads=[B_h1[bl]], writes=[B_sil[bl]])
                        tr2.op("dve", lambda e, bl=bl, hsl=hsl, ffc=ffc: e.tensor_tensor(hid[hsl][:, ffc, :], sil[bl][:], h3p[bl][:, :], ALU.mult),
                               reads=[B_sil[bl], B_h3[bl]], writes=[B_hid[hsl]])
                    for tt in range(4):
                        tile_i = tc * 4 + tt
                        for half in range(2):
                            ol = cnt[2] % 4
                            cnt[2] += 1

                            def mm2(e, ws=ws, hsl=hsl, tt=tt, half=half, ol=ol):
                                last = None
                                for ffc in range(4):
                                    last = e.matmul(op_[ol][:, :], hid[hsl][:, ffc, tt * 128:(tt + 1) * 128],
                                                    w2b[ws][:, ffc, half * 512:(half + 1) * 512], start=(ffc == 0), stop=(ffc == 3))
                                return last
                            tr2.op("pe", mm2, reads=[B_hid[hsl], B_w[ws]], writes=[B_op[ol]])
                            tr2.op("dve", lambda e, tile_i=tile_i, half=half, ol=ol, ex=ex: e.scalar_tensor_tensor(
                                hres[:, tile_i, half * 512:(half + 1) * 512], op_[ol][:, :], gates[:, tile_i, ex:ex + 1],
                                hres[:, tile_i, half * 512:(half + 1) * 512], ALU.mult, ALU.add),
                                reads=[B_op[ol], F_hres[tile_i]], writes=[F_hres[tile_i]])
            tr2.barrier()
            tr.barrier()
            tr.emit()
            tr2.emit(cond_ap=flag[0:1, 0:1])

        with ExitStack() as ph:
            nfB = sb("nfB", [128, D], F32, ph)
            y12 = [[sb("y%d_%d" % (a, i), [128, D], F32, ph) for a in range(2)] for i in range(2)]
            ot = [sb("ot%d" % i, [128, D], F32, ph) for i in range(2)]
            junk = sb("junkG", [128, D], BF16, ph)
            smg = sb("smG", [128, 2, 4], F32, ph)
            B_nf, B_junk = Buf(), Buf()
            B_y = [[Buf() for a in range(2)] for i in range(2)]
            B_ot = [Buf() for _ in range(2)]
            B_smg = [Buf() for _ in range(2)]
            s_g = tr.dma_sem("g0")
            s_o = [tr.dma_sem() for _ in range(2)]
            s_ga = [[tr.dma_sem() for a in range(2)] for i in range(2)]
            tr.dma("sp", s_g, lambda e: [e.dma_start(out=nfB[:], in_=nfin_d.partition_broadcast(128))], writes=[B_nf])
            for sl in range(2):
                for a in range(2):
                    tr.op("pool", lambda e, sl=sl, a=a: e.memset(y12[sl][a][:], 0.0), writes=[B_y[sl][a]])
            s_yz = tr.dma_sem("yz")
            tr.dma("sp", s_yz, lambda e: [e.dma_start(out=y_d[NSL:NSL + 128, :], in_=y12[0][0][:])], reads=[B_y[0][0]], writes=[B_yd])
            for i in range(NT):
                sl = i % 2
                for a in range(2):
                    tr.dma("pool", s_ga[sl][a], lambda e, i=i, a=a, sl=sl: [e.indirect_dma_start(
                        out=y12[sl][a][:, :], out_offset=None, in_=y_d[:, :],
                        in_offset=bass.IndirectOffsetOnAxis(ap=DI[:, a, i:i + 1], axis=0))],
                        reads=B_DIt + [B_yd], writes=[B_y[sl][a]])
                    tr.op("dve", lambda e, i=i, a=a, sl=sl: e.scalar_tensor_tensor(hres[:, i, :], y12[sl][a][:], AV[:, a, i:i + 1], hres[:, i, :], ALU.mult, ALU.add),
                          reads=[B_y[sl][a], B_AV, B_hres[i]], writes=[B_hres[i]])
                tr.op("act", lambda e, i=i, sl=sl: e.activation(out=junk[:], in_=hres[:, i, :], func=AF.Square, accum_out=smg[:, sl, 0:1]),
                      reads=[B_hres[i]], writes=[B_junk, B_smg[sl]])
                tr.op("act", lambda e, sl=sl: e.activation(out=smg[:, sl, 1:2], in_=smg[:, sl, 0:1], func=AF.Sqrt, scale=1.0 / D, bias=EPS),
                      reads=[B_smg[sl]], writes=[B_smg[sl]])
                tr.op("dve", lambda e, sl=sl: e.reciprocal(smg[:, sl, 2:3], smg[:, sl, 1:2]), reads=[B_smg[sl]], writes=[B_smg[sl]])
                tr.op("dve", lambda e, i=i, sl=sl: e.scalar_tensor_tensor(ot[sl][:], hres[:, i, :], smg[:, sl, 2:3], nfB[:], ALU.mult, ALU.mult),
                      reads=[B_hres[i], B_smg[sl], B_nf], writes=[B_ot[sl]])
                tr.dma("sp", s_o[sl], lambda e, i=i, sl=sl: [e.dma_start(out=out_d[i * 128:(i + 1) * 128, :], in_=ot[sl][:])], reads=[B_ot[sl]])
            tr.barrier()
            tr.emit()

        tr.barrier()
        tr.emit()
    return nc, dbg_outs


def make_inputs(inputs):
    w_in = np.asarray(inputs["w_in"][0], np.float32)
    a = 512

    def swap_halves(w):
        w4 = w.reshape(D, 8, 2, 32)
        return np.ascontiguousarray(w4[:, :, ::-1, :]).reshape(D, 512)
    rq = w_in[:, 3 * a:3 * a + 512]
    rk = w_in[:, 3 * a + 512:3 * a + 1024]
    w_in_ext = np.ascontiguousarray(np.concatenate([w_in, swap_halves(rq), swap_halves(rk)], axis=1))
    cosT, sinT = _const_rope()
    shared = {
        "w_in": w_in_ext,
        "w_out": np.ascontiguousarray(inputs["w_out"][0], np.float32),
        "norm_mix_t": np.ascontiguousarray(np.asarray(inputs["norm_mix"][0], np.float32).reshape(8, 128).T),
        "norm_ffn_t": np.ascontiguousarray(np.asarray(inputs["norm_ffn"][0], np.float32).reshape(8, 128).T),
        "norm_final": np.ascontiguousarray(np.asarray(inputs["norm_final"], np.float32).reshape(1, D)),
        "attn_gain_t": np.ascontiguousarray(np.asarray(inputs["attn_out_gain"][0], np.float32).reshape(8, 64).T),
        "rel_bias": np.ascontiguousarray(inputs["rel_bias"], np.float32),
        "ret_decay": np.ascontiguousarray(np.concatenate([np.asarray(inputs["ret_decay_fwd"][0], np.float32),
                                                          np.asarray(inputs["ret_decay_bwd"][0], np.float32)]).reshape(1, 16)),
        "w_router": np.ascontiguousarray(np.concatenate([
            np.asarray(inputs["router_group_w"][0], np.float32),
            np.asarray(inputs["router_expert_w"][0], np.float32).transpose(1, 0, 2).reshape(D, 16)], axis=1)),
        "b_router": np.ascontiguousarray(np.concatenate([
            np.asarray(inputs["router_group_b"][0], np.float32).reshape(4),
            np.asarray(inputs["router_expert_b"][0], np.float32).reshape(16)]).reshape(1, 20)),
        "w1": np.ascontiguousarray(inputs["expert_w1"][0], np.float32),
        "w3": np.ascontiguousarray(inputs["expert_w3"][0], np.float32),
        "w2": np.ascontiguousarray(inputs["expert_w2"][0], np.float32),
        "c_ident": np.eye(128, dtype=np.float32),
        "c_onehot": _const_onehot(),
        "c_cos": cosT,
        "c_sin": sinT,
        "c_relm": _const_relm(),
        "norm_ffn_row": np.ascontiguousarray(np.asarray(inputs["norm_ffn"][0], np.float32).reshape(1, D)),
        "c_ustr": np.triu(np.ones((128, 128), np.float32), k=1),
        "c_iota16": np.ascontiguousarray(np.concatenate([np.broadcast_to(np.arange(16, dtype=np.float32)[None, :], (128, 16)),
                                                         np.arange(128, dtype=np.float32)[:, None]], axis=1)),
    }
    x = np.asarray(inputs["x"], np.float32)
    in_maps = []
    for b in range(8):
        m = dict(shared)
        m["x"] = np.ascontiguousarray(x[b])
        in_maps.append(m)
    return in_maps


def kernel(**inputs):
    nc, _ = build_program()
    in_maps = make_inputs(inputs)
    res = run_bass_kernel_spmd(nc, in_maps, core_ids=list(range(8)))
    return np.stack([np.asarray(r["out"], np.float32) for r in res.results], axis=0)
```
